# Optimizing a Trainium2 kernel written in Bass

```python
import math
import jax, jax.numpy as jnp
from jax import lax
import numpy as np

D_MODEL = 1024
BATCH = 8
SEQ = 4096
DEPTH = 1

HEAD_DIM = 64
SB_HEADS = 8
SB_WIDTH = SB_HEADS * HEAD_DIM
DIFF_HEADS = 4
DIFF_QK_DIM = 64
DIFF_V_DIM = 2 * DIFF_QK_DIM
DIFF_QK_WIDTH = DIFF_HEADS * 2 * DIFF_QK_DIM
DIFF_V_WIDTH = DIFF_HEADS * DIFF_V_DIM
N_BRANCHES = 2
IN_SPLITS = (SB_WIDTH, SB_WIDTH, SB_WIDTH, DIFF_QK_WIDTH, DIFF_QK_WIDTH, DIFF_V_WIDTH, D_MODEL, D_MODEL)
IN_WIDTH = 3 * SB_WIDTH + 2 * DIFF_QK_WIDTH + DIFF_V_WIDTH + N_BRANCHES * D_MODEL
Q_BLOCK = 128
ROPE_THETA = 10000.0
N_GROUPS = 4
EXPERTS_PER_GROUP = 8
N_EXPERTS = N_GROUPS * EXPERTS_PER_GROUP
TOP_K = 2
D_EXPERT = 256
EXPERT_BLOCK = 128
EPS = 1e-6

kernel_name = "hybrid_stickbreak_diffattn_hiermoe"


def rmsnorm(x, g):
    xf = x.astype(jnp.float32)
    y = xf * lax.rsqrt(jnp.mean(xf * xf, axis=-1, keepdims=True) + EPS)
    return (y * g.astype(jnp.float32)).astype(x.dtype)


def rope_tables(seq_len, dim):
    pos = jnp.arange(seq_len, dtype=jnp.float32)
    inv_freq = 1.0 / (ROPE_THETA ** (jnp.arange(0, dim, 2, dtype=jnp.float32) / dim))
    freqs = pos[:, None] * inv_freq[None, :]
    emb = jnp.concatenate([freqs, freqs], axis=-1)
    return jnp.cos(emb), jnp.sin(emb)


def apply_rope(x, cos, sin):
    xf = x.astype(jnp.float32)
    half = xf.shape[-1] // 2
    rot = jnp.concatenate([-xf[..., half:], xf[..., :half]], axis=-1)
    return (xf * cos + rot * sin).astype(x.dtype)


def split_heads(t, n_heads, d):
    b, s, _ = t.shape
    return t.reshape(b, s, n_heads, d).transpose(0, 2, 1, 3)


def merge_heads(t):
    b, h, s, d = t.shape
    return t.transpose(0, 2, 1, 3).reshape(b, s, h * d)


def stick_breaking_attention(q, k, v):
    seq_len = q.shape[2]
    scale = HEAD_DIM ** -0.5
    outs = []
    for blk in range(seq_len // Q_BLOCK):
        q0 = blk * Q_BLOCK
        kv_len = q0 + Q_BLOCK
        qb = q[:, :, q0:kv_len]
        kb = k[:, :, :kv_len]
        vb = v[:, :, :kv_len]
        z = jnp.einsum('bhqd,bhkd->bhqk', qb, kb, preferred_element_type=jnp.float32) * scale
        qpos = q0 + jnp.arange(Q_BLOCK)[:, None]
        kpos = jnp.arange(kv_len)[None, :]
        strict = kpos < qpos
        log_1m_beta = jnp.where(strict, jax.nn.log_sigmoid(-z), 0.0)
        after = lax.cumsum(log_1m_beta, axis=3, reverse=True) - log_1m_beta
        a = jnp.where(strict, jnp.exp(jax.nn.log_sigmoid(z) + after), 0.0)
        outs.append(jnp.einsum('bhqk,bhkd->bhqd', a.astype(v.dtype), vb))
    return jnp.concatenate(outs, axis=2)


def differential_attention(q1, q2, k1, k2, v, lam):
    seq_len = q1.shape[2]
    scale = DIFF_QK_DIM ** -0.5
    outs = []
    for blk in range(seq_len // Q_BLOCK):
        q0 = blk * Q_BLOCK
        kv_len = q0 + Q_BLOCK
        qpos = q0 + jnp.arange(Q_BLOCK)[:, None]
        kpos = jnp.arange(kv_len)[None, :]
        causal = kpos <= qpos

        def probs(qx, kx):
            s = jnp.einsum('bhqd,bhkd->bhqk', qx[:, :, q0:kv_len], kx[:, :, :kv_len],
                           preferred_element_type=jnp.float32) * scale
            return jax.nn.softmax(jnp.where(causal, s, -jnp.inf), axis=-1)

        p = probs(q1, k1) - lam.astype(jnp.float32) * probs(q2, k2)
        outs.append(jnp.einsum('bhqk,bhkd->bhqd', p.astype(v.dtype), v[:, :, :kv_len]))
    return jnp.concatenate(outs, axis=2)


def hierarchical_moe(h, w_rg, b_rg, w_re, b_re, w_gate, w_up, w_down):
    b, s, d = h.shape
    n_tok = b * s
    t = h.reshape(n_tok, d)
    g_logits = (t @ w_rg + b_rg).astype(jnp.float32)
    grp = jnp.argmax(g_logits, axis=-1)
    p_grp = jnp.take_along_axis(jax.nn.softmax(g_logits, axis=-1), grp[:, None], axis=-1)
    e_logits = (t @ w_re + b_re).astype(jnp.float32).reshape(n_tok, N_GROUPS, EXPERTS_PER_GROUP)
    e_in = jnp.take_along_axis(e_logits, grp[:, None, None], axis=1)[:, 0]
    top_w, top_i = lax.top_k(jax.nn.softmax(e_in, axis=-1), TOP_K)
    top_w = top_w / jnp.sum(top_w, axis=-1, keepdims=True) * p_grp
    expert_id = grp[:, None] * EXPERTS_PER_GROUP + top_i

    n_assign = n_tok * TOP_K
    flat_e = expert_id.reshape(n_assign)
    flat_w = top_w.reshape(n_assign)
    flat_tok = jnp.arange(n_assign) // TOP_K
    order = jnp.argsort(flat_e)
    s_e, s_tok, s_w = flat_e[order], flat_tok[order], flat_w[order]
    counts = jnp.bincount(flat_e, length=N_EXPERTS)
    starts = jnp.cumsum(counts) - counts
    padded = (counts + EXPERT_BLOCK - 1) // EXPERT_BLOCK * EXPERT_BLOCK
    pad_end = jnp.cumsum(padded)
    pad_start = pad_end - padded
    dest = pad_start[s_e] + (jnp.arange(n_assign) - starts[s_e])
    n_rows = ((n_assign + EXPERT_BLOCK - 1) // EXPERT_BLOCK + N_EXPERTS) * EXPERT_BLOCK
    n_blocks = n_rows // EXPERT_BLOCK
    xd = jnp.zeros((n_rows, d), t.dtype).at[dest].set(t[s_tok])
    block_e = jnp.clip(jnp.searchsorted(pad_end, jnp.arange(n_blocks) * EXPERT_BLOCK, side='right'),
                       0, N_EXPERTS - 1)

    def expert_block(args):
        xb, e = args
        return (jax.nn.silu(xb @ w_gate[e]) * (xb @ w_up[e])) @ w_down[e]

    yd = lax.map(expert_block, (xd.reshape(n_blocks, EXPERT_BLOCK, d), block_e)).reshape(n_rows, d)
    y_assign = yd[dest] * s_w.astype(yd.dtype)[:, None]
    y = jax.ops.segment_sum(y_assign, s_tok, num_segments=n_tok)
    return y.reshape(b, s, d)


def setup_inputs(seed: int = 0) -> dict:
    key = jax.random.key(seed)
    ks = jax.random.split(key, 21)
    f32 = jnp.float32

    def normal(k, shape, scale):
        return jax.random.normal(k, shape, f32) * scale

    def gain(k, shape):
        return 1.0 + 0.02 * jax.random.normal(k, shape, f32)

    return {
        "x": normal(ks[0], (BATCH, SEQ, D_MODEL), 1.0),
        "g_norm_mix": gain(ks[1], (DEPTH, D_MODEL)),
        "w_in": normal(ks[2], (DEPTH, D_MODEL, IN_WIDTH), D_MODEL ** -0.5),
        "lambda_q1": normal(ks[3], (DEPTH, DIFF_QK_DIM), 0.1),
        "lambda_k1": normal(ks[4], (DEPTH, DIFF_QK_DIM), 0.1),
        "lambda_q2": normal(ks[5], (DEPTH, DIFF_QK_DIM), 0.1),
        "lambda_k2": normal(ks[6], (DEPTH, DIFF_QK_DIM), 0.1),
        "g_subln": gain(ks[7], (DEPTH, DIFF_V_DIM)),
        "w_up_a": normal(ks[8], (DEPTH, SB_WIDTH, D_MODEL), SB_WIDTH ** -0.5),
        "w_up_b": normal(ks[9], (DEPTH, DIFF_V_WIDTH, D_MODEL), DIFF_V_WIDTH ** -0.5),
        "w_out": normal(ks[10], (DEPTH, D_MODEL, D_MODEL), D_MODEL ** -0.5),
        "g_norm_ffn": gain(ks[11], (DEPTH, D_MODEL)),
        "w_router_group": normal(ks[12], (DEPTH, D_MODEL, N_GROUPS), D_MODEL ** -0.5),
        "b_router_group": normal(ks[13], (DEPTH, N_GROUPS), 0.01),
        "w_router_expert": normal(ks[14], (DEPTH, D_MODEL, N_EXPERTS), D_MODEL ** -0.5),
        "b_router_expert": normal(ks[15], (DEPTH, N_EXPERTS), 0.01),
        "w_expert_gate": normal(ks[16], (DEPTH, N_EXPERTS, D_MODEL, D_EXPERT), D_MODEL ** -0.5),
        "w_expert_up": normal(ks[17], (DEPTH, N_EXPERTS, D_MODEL, D_EXPERT), D_MODEL ** -0.5),
        "w_expert_down": normal(ks[18], (DEPTH, N_EXPERTS, D_EXPERT, D_MODEL), D_EXPERT ** -0.5),
        "g_norm_final": gain(ks[19], (D_MODEL,)),
    }


def reference(x, g_norm_mix, w_in, lambda_q1, lambda_k1, lambda_q2, lambda_k2, g_subln,
              w_up_a, w_up_b, w_out, g_norm_ffn, w_router_group, b_router_group,
              w_router_expert, b_router_expert, w_expert_gate, w_expert_up, w_expert_down,
              g_norm_final):
    b, s, _ = x.shape
    cos, sin = rope_tables(s, DIFF_QK_DIM)
    for l in range(DEPTH):
        h = rmsnorm(x, g_norm_mix[l])
        proj = jnp.einsum('bsd,de->bse', h, w_in[l])
        pieces = []
        off = 0
        for width in IN_SPLITS:
            pieces.append(proj[..., off:off + width])
            off += width
        sb_q, sb_k, sb_v, d_q, d_k, d_v, gate_a, gate_b = pieces

        o_a = stick_breaking_attention(split_heads(sb_q, SB_HEADS, HEAD_DIM),
                                       split_heads(sb_k, SB_HEADS, HEAD_DIM),
                                       split_heads(sb_v, SB_HEADS, HEAD_DIM))

        dq = d_q.reshape(b, s, DIFF_HEADS, 2, DIFF_QK_DIM).transpose(0, 2, 3, 1, 4)
        dk = d_k.reshape(b, s, DIFF_HEADS, 2, DIFF_QK_DIM).transpose(0, 2, 3, 1, 4)
        q1, q2 = apply_rope(dq[:, :, 0], cos, sin), apply_rope(dq[:, :, 1], cos, sin)
        k1, k2 = apply_rope(dk[:, :, 0], cos, sin), apply_rope(dk[:, :, 1], cos, sin)
        dv = split_heads(d_v, DIFF_HEADS, DIFF_V_DIM)
        lambda_init = 0.8 - 0.6 * math.exp(-0.3 * l)
        lam = (jnp.exp(jnp.sum(lambda_q1[l] * lambda_k1[l]))
               - jnp.exp(jnp.sum(lambda_q2[l] * lambda_k2[l])) + lambda_init)
        o_b = differential_attention(q1, q2, k1, k2, dv, lam)
        o_b = rmsnorm(o_b, g_subln[l]) * (1.0 - lambda_init)

        u_a = merge_heads(o_a) @ w_up_a[l]
        u_b = merge_heads(o_b) @ w_up_b[l]
        y = jax.nn.sigmoid(gate_a) * u_a + jax.nn.sigmoid(gate_b) * u_b
        x = x + y @ w_out[l]

        h2 = rmsnorm(x, g_norm_ffn[l])
        x = x + hierarchical_moe(h2, w_router_group[l], b_router_group[l],
                                 w_router_expert[l], b_router_expert[l],
                                 w_expert_gate[l], w_expert_up[l], w_expert_down[l])
    return rmsnorm(x, g_norm_final)
```

```python
import contextlib
import numpy as np
import ml_dtypes
import concourse.bass as bass
import concourse.mybir as mybir
from concourse.bass_utils import run_bass_kernel_spmd

F32 = mybir.dt.float32
BF16 = mybir.dt.bfloat16
I32 = mybir.dt.int32
U32 = mybir.dt.uint32
AF = mybir.ActivationFunctionType
ALU = mybir.AluOpType
AX = mybir.AxisListType

D_MODEL = 1024
SEQ = 4096
IN_WIDTH = 5120
N_EXPERTS = 32
D_EXPERT = 256
EPS = 1e-6
EPOCH = 12000


class KB:
    def __init__(self, nc, stack):
        self.nc = nc
        self.stack = stack
        self.sem_stack = stack
        self.bar_tile = stack.enter_context(nc.sbuf_tensor("bar_tile", [128, 8], F32))
        self.eng = {"pe": nc.tensor, "dve": nc.vector, "act": nc.scalar,
                    "pool": nc.gpsimd, "sp": nc.sync}
        self.cnt = {e: 0 for e in self.eng}
        self.sems = {}
        self.waited = {e: {} for e in self.eng}
        self.last_w = {}
        self.readers = {}
        self.dma_cnt = {}
        self.dma_latest = {}
        self.n_wait = 0

    def sem(self, key):
        s = self.sems.get(key)
        if s is None:
            name = "s_" + "_".join(str(k) for k in key)
            s = self.sem_stack.enter_context(self.nc.semaphore(name))
            self.sems[key] = s
        return s

    def sbuf(self, name, shape, dt):
        return self.stack.enter_context(self.nc.sbuf_tensor(name, list(shape), dt))

    def psum(self, name, shape, dt):
        return self.stack.enter_context(self.nc.psum_tensor(name, list(shape), dt))

    def _deps(self, reads, writes, e=None):
        deps = {}

        def add(st):
            sk, v = st
            if sk[0] == "dma":
                v = max(v, self.dma_latest.get(sk, 0))
            if deps.get(sk, 0) < v:
                deps[sk] = v
        for k in reads:
            lw = self.last_w.get(k)
            if lw is not None:
                add(lw)
            if isinstance(k, tuple) and k and k[0] == "ps":
                for sk, v in self.readers.get(k, {}).items():
                    if e is None or sk[0] != e:
                        add((sk, v))
        for k in writes:
            lw = self.last_w.get(k)
            if lw is not None:
                add(lw)
            for sk, v in self.readers.get(k, {}).items():
                add((sk, v))
        return deps

    def _emit_waits(self, e, deps):
        for sk, v in deps.items():
            if sk[0] == "pe" and e == "pe":
                continue
            if self.waited[e].get(sk, 0) >= v:
                continue
            self.eng[e].wait_ge(self.sem(sk), v)
            self.waited[e][sk] = v
            self.n_wait += 1

    def _stamp(self, stamp, reads, writes):
        sk, v = stamp
        for k in reads:
            self.readers.setdefault(k, {})[sk] = v
        for k in writes:
            self.last_w[k] = stamp
            self.readers[k] = {}

    def op(self, e, fn, reads=(), writes=()):
        self._emit_waits(e, self._deps(reads, writes, e))
        inst = fn(self.eng[e])
        n = self.cnt[e]
        self.cnt[e] = n + 1
        sk = (e, n // EPOCH)
        v = (n % EPOCH) + 1
        inst.then_inc(self.sem(sk), 1)
        self._stamp((sk, v), reads, writes)
        return inst

    def dma(self, q, fn, reads=(), writes=(), sem=None):
        self._emit_waits(q, self._deps(reads, writes))
        inst = fn(self.eng[q])
        n = self.dma_cnt.get(sem, 0)
        self.dma_cnt[sem] = n + 1
        per = EPOCH // 16
        sk = ("dma", sem, n // per)
        v = ((n % per) + 1) * 16
        inst.then_inc(self.sem(sk), 16)
        self.dma_latest[sk] = v
        self._stamp((sk, v), reads, writes)
        return inst

    def barrier(self):
        if self.bar_tile is None:
            self.bar_tile = self.sem_stack.enter_context(self.nc.sbuf_tensor("bar_tile", [128, 8], F32))
        deps = {}
        for e, n in self.cnt.items():
            if n:
                deps[(e, (n - 1) // EPOCH)] = ((n - 1) % EPOCH) + 1
        for sk, v in self.dma_latest.items():
            deps[sk] = v
        self._emit_waits("pool", deps)
        self.op("pool", lambda e: e.memset(self.bar_tile[:], 0.0))
        n = self.cnt["pool"]
        st = {("pool", (n - 1) // EPOCH): ((n - 1) % EPOCH) + 1}
        for e in self.eng:
            if e != "pool":
                self._emit_waits(e, dict(st))

    @contextlib.contextmanager
    def scope(self):
        old = self.stack
        with contextlib.ExitStack() as sub:
            self.stack = sub
            try:
                yield
            finally:
                self.stack = old
            self.barrier()

    def wait_all(self, e, keys):
        self._emit_waits(e, self._deps(keys, keys))


def host_consts(S):
    bf = ml_dtypes.bfloat16
    j = np.arange(128)[:, None]
    s = np.arange(128)[None, :]
    c = {}
    c["c_ident_bf"] = np.eye(128, dtype=np.float32).astype(bf)
    c["c_ident_f"] = np.eye(128, dtype=np.float32)
    c["c_tri_m8"] = np.where(j >= s, -8.0, 0.0).astype(bf)
    c["c_ones_m8"] = np.full((128, 128), -8.0, np.float32).astype(bf)
    c["c_mask_sb"] = np.where(j >= s, -10000.0, 0.0).astype(bf)
    c["c_mask_df"] = np.where(j > s, -10000.0, 0.0).astype(bf)
    c["c_m01_sb"] = np.where(j < s, 1.0, 0.0).astype(bf)
    c["c_m01_df"] = np.where(j <= s, 1.0, 0.0).astype(bf)
    src = np.where((np.arange(128) % 64) < 32, np.arange(128) + 32, np.arange(128) - 32)
    perm = np.zeros((128, 128), np.float32)
    perm[src, np.arange(128)] = 1.0
    c["c_perm"] = perm.astype(bf)
    c["c_zeros"] = np.zeros((128, 512), np.float32).astype(bf)
    c["c_tri_f"] = np.where(j < s, 1.0, 0.0).astype(np.float32)
    c["c_ones_f"] = np.ones((128, 128), np.float32)
    pos = np.arange(S, dtype=np.float32)
    inv_freq = (1.0 / (np.float32(10000.0) ** (np.arange(0, 64, 2, dtype=np.float32) / np.float32(64)))).astype(np.float32)
    fr = (pos[None, :] * inv_freq[:, None]).astype(np.float32)
    cos = np.cos(fr).astype(np.float32)
    sin = np.sin(fr).astype(np.float32)
    cos64 = np.concatenate([cos, cos], 0)
    sin64 = np.concatenate([-sin, sin], 0)
    c["c_cos"] = np.ascontiguousarray(np.concatenate([cos64, cos64], 0))
    c["c_sin"] = np.ascontiguousarray(np.concatenate([sin64, sin64], 0))
    return c


CONST_SPECS = {
    "c_ident_bf": ([128, 128], BF16), "c_ident_f": ([128, 128], F32),
    "c_tri_m8": ([128, 128], BF16), "c_ones_m8": ([128, 128], BF16),
    "c_mask_sb": ([128, 128], BF16), "c_mask_df": ([128, 128], BF16),
    "c_m01_sb": ([128, 128], BF16), "c_m01_df": ([128, 128], BF16), "c_perm": ([128, 128], BF16),
    "c_zeros": ([128, 512], BF16), "c_tri_f": ([128, 128], F32), "c_ones_f": ([128, 128], F32),
}


def bcast_rows(ap2d, n):
    return bass.AP(ap2d.tensor, ap2d.offset, [[0, 128], [1, n]])


def build(S, stages="AB", dbg=()):
    NT = S // 128
    NQ = S // 512
    nc = bass.Bass("TRN2", target_bir_lowering=False)

    def din(name, shape, dt=F32):
        return nc.dram_tensor(name, list(shape), dt, kind="ExternalInput").ap()

    def dout(name, shape, dt=F32):
        return nc.dram_tensor(name, list(shape), dt, kind="ExternalOutput").ap()

    x_d = din("x", [S, 1024])
    w_in_d = din("w_in", [1024, IN_WIDTH])
    g_mix_d = din("g_norm_mix", [1, 1024])
    lam_d = {k: din(k, [1, 64]) for k in ("lambda_q1", "lambda_k1", "lambda_q2", "lambda_k2")}
    g_sub_d = din("g_subln", [1, 128])
    cd = {k: din(k, sh, dt) for k, (sh, dt) in CONST_SPECS.items()}
    cos_d = din("c_cos", [128, S])
    sin_d = din("c_sin", [128, S])
    NB = 2 * NT + 32
    NROWS = NB * 128
    w_up_a_d = din("w_up_a", [512, 1024])
    w_up_b_d = din("w_up_b", [512, 1024])
    w_out_d = din("w_out", [1024, 1024])
    g_ffn_d = din("g_norm_ffn", [1, 1024])
    w_rg_d = din("w_router_group", [1024, 4])
    w_re_d = din("w_router_expert", [1024, 32])
    b_rg_d = din("b_router_group", [1, 4])
    b_re_d = din("b_router_expert", [1, 32])
    wg_d = din("wg_l", [4096, 2048])
    wu_d = din("wu_l", [4096, 2048])
    wd_d = din("wd_l", [4096, 2048])
    g_fin_d = din("g_norm_final", [1, 1024])
    out_d = dout("out", [S, 1024])
    yT_d = nc.dram_tensor("yT_scr", [8, 128, S], BF16).ap()
    x1_d = nc.dram_tensor("x1_scr", [S, 1024], F32).ap()
    xd_d = nc.dram_tensor("xd_scr", [NROWS, 1024], BF16).ap()
    yd_d = nc.dram_tensor("yd_scr", [NROWS, 1024], F32).ap()
    wall_d = nc.dram_tensor("wall_bf", [4096, 6144], BF16).ap()
    outs = {}
    XD_KEYS = [("xd_d", i) for i in range(4)]
    YD_KEYS = [("yd_d", i) for i in range(2)]
    WALL_KEYS = [("wall", i) for i in range(4)]

    with contextlib.ExitStack() as st:
        kb = KB(nc, st)
        psall = kb.psum("psall", [128, 8, 512], F32)
        ps = [psall[:, i, :] for i in range(8)]
        psk = lambda b: ("ps", b)

        csb = {}
        for k, (sh, dt) in CONST_SPECS.items():
            t = kb.sbuf("sb_" + k, sh, dt)
            kb.dma("sp", lambda e, t=t, k=k: e.dma_start(out=t[:], in_=cd[k]), writes=[k], sem=k)
            csb[k] = t
        ident = csb["c_ident_bf"]
        zeros = csb["c_zeros"]
        junk = kb.sbuf("junk", [128, 1024], BF16)
        st_main = contextlib.ExitStack()
        kb.stack = st_main
        hT = kb.sbuf("hT", [128, 8, S], BF16)
        OaT = kb.sbuf("OaT", [128, 4, S], BF16)
        ObT = kb.sbuf("ObT", [128, 4, S], BF16)

        def stage_A():
            gmix = kb.sbuf("gmix", [128, 1024], F32)
            kb.dma("sp", lambda e: e.dma_start(out=gmix[:], in_=bcast_rows(g_mix_d, 1024)), writes=["gmix"], sem="gmix")
            NA = 4
            xt = [kb.sbuf(f"xt{i}", [128, 1024], F32) for i in range(NA)]
            hb = [kb.sbuf(f"hb{i}", [128, 1024], BF16) for i in range(NA)]
            stA = [kb.sbuf(f"stA{i}", [128, 4], F32) for i in range(NA)]

            def a_tile(tt, i):
                kb.dma("sp", lambda e: e.dma_start(out=xt[i][:], in_=x_d[tt * 128:(tt + 1) * 128, :]),
                       writes=[("xt", i)], sem=f"xt{i}")
                yield
                kb.op("act", lambda e: e.activation(out=junk[:], in_=xt[i][:], func=AF.Square, accum_out=stA[i][:, 0:1]),
                      reads=[("xt", i)], writes=["junk", ("stA", i)])
                yield
                kb.op("act", lambda e: e.activation(out=stA[i][:, 1:2], in_=stA[i][:, 0:1], func=AF.Ln, bias=EPS, scale=1.0 / 1024),
                      reads=[("stA", i)], writes=[("stA", i)])
                yield
                kb.op("act", lambda e: e.activation(out=stA[i][:, 2:3], in_=stA[i][:, 1:2], func=AF.Exp, scale=-0.5),
                      reads=[("stA", i)], writes=[("stA", i)])
                yield
                kb.op("dve", lambda e: e.scalar_tensor_tensor(out=hb[i][:], in0=xt[i][:], scalar=stA[i][:, 2:3], in1=gmix[:],
                                                              op0=ALU.mult, op1=ALU.mult),
                      reads=[("xt", i), ("stA", i), "gmix"], writes=[("hb", i)])
                yield
                b = i
                psb = ps[b][:].bitcast(BF16)
                for kc in range(8):
                    kb.op("pe", lambda e: e.transpose(out=psb[:, kc * 128:(kc + 1) * 128], in_=hb[i][:, kc * 128:(kc + 1) * 128],
                                                      identity=ident[:]),
                          reads=[("hb", i), "c_ident_bf"], writes=[psk(b)])
                yield
                kb.op("dve", lambda e: e.tensor_copy(out=hT[:, :, tt * 128:(tt + 1) * 128], in_=psb.rearrange("p (k t) -> p k t", k=8)),
                      reads=[psk(b)], writes=[("hT", tt)])
                yield

            run_streams([(lambda sl, tt=tt: a_tile(tt, sl)) for tt in range(NT)], NA, bg_every=10 ** 9)

        hT_keys = lambda tq: [("hT", t) for t in range(4 * tq, 4 * tq + 4)]
        pcount = [0]
        PB = [4]

        def proj_fm(wt, wkey, tq, M=128):
            b = pcount[0] % PB[0]
            pcount[0] += 1
            for kc in range(8):
                kb.op("pe", lambda e: e.matmul(ps[b][0:M, 0:512], lhsT=wt[:, kc, 0:M], rhs=hT[:, kc, tq * 512:(tq + 1) * 512],
                                               start=(kc == 0), stop=(kc == 7)),
                      reads=[wkey] + hT_keys(tq), writes=[psk(b)])
            return b

        def load_w(wt, wkey, col0, ncols=128, q="pool"):
            kb.dma(q, lambda e: e.dma_start(out=wt[:, :, 0:ncols],
                                            in_=w_in_d[:, col0:col0 + ncols].rearrange("(kc p) c -> p kc c", p=128)),
                   writes=[wkey], sem=str(wkey))

        ccount = [0]

        def evac(out_ap, in_ap, reads, writes):
            if ccount[0] % 2 == 0:
                kb.op("act", lambda e: e.activation(out=out_ap, in_=in_ap, func=AF.Copy), reads=reads, writes=writes)
            else:
                kb.op("dve", lambda e: e.tensor_copy(out=out_ap, in_=in_ap), reads=reads, writes=writes)
            ccount[0] += 1

        qT = kb.sbuf("qT", [128, S], BF16)
        kT = kb.sbuf("kT", [128, S], BF16)
        V = kb.sbuf("V", [128, NT, 132], BF16)
        wq = [kb.sbuf(f"wq{i}", [128, 8, 128], BF16) for i in range(1)]
        wk = [kb.sbuf(f"wk{i}", [128, 8, 128], BF16) for i in range(1)]
        wv = [kb.sbuf(f"wv{i}", [128, 8, 128], BF16) for i in range(1)]

        def proj_v(wt, wkey):
            for t4 in range(NT // 4):
                b = pcount[0] % PB[0]
                pcount[0] += 1
                for j in range(4):
                    tt = t4 * 4 + j
                    for kc in range(8):
                        kb.op("pe", lambda e: e.matmul(ps[b][:, j * 128:(j + 1) * 128], lhsT=hT[:, kc, tt * 128:(tt + 1) * 128],
                                                       rhs=wt[:, kc, :], start=(kc == 0), stop=(kc == 7)),
                              reads=[wkey, ("hT", tt)], writes=[psk(b)])
                evac(V[:, t4 * 4:(t4 + 1) * 4, 0:128], ps[b][:, 0:512].rearrange("p (j d) -> p j d", j=4),
                     reads=[psk(b)], writes=[("V", t4)])

        bg_ops = []

        def run_streams(makers, ns, bg_every=12):
            pending = list(makers)
            active = {}
            rounds = 0
            while pending or active:
                rounds += 1
                if bg_ops and rounds % bg_every == 0:
                    bg_ops.pop(0)()
                for sl in range(ns):
                    if sl not in active and pending:
                        active[sl] = pending.pop(0)(sl)
                    g = active.get(sl)
                    if g is not None:
                        try:
                            next(g)
                        except StopIteration:
                            del active[sl]

        def stage_B1():
            NS = 4
            PB[0] = 8
            m01 = csb["c_m01_sb"]
            e32 = [kb.sbuf(f"e32_{i}", [128, 512], BF16) for i in range(NS)]
            Pb = [kb.sbuf(f"Pb{i}", [128, 512], BF16) for i in range(NS)]
            Ab = [kb.sbuf(f"Ab{i}", [128, 512], BF16) for i in range(NS)]
            R32 = [kb.sbuf(f"R32_{i}", [128, 512], F32) for i in range(NS)]
            Rb = [kb.sbuf(f"Rb{i}", [128, 512], BF16) for i in range(NS)]
            tri = csb["c_tri_m8"]
            onesm = csb["c_ones_m8"]
            msb = csb["c_mask_sb"]

            def sb_stream(hp, par, qt, sl):
                pb = 64 * par
                bs = sl
                bo = 4 + sl
                kb.op("pe", lambda e: e.matmul(ps[bo][pb:pb + 64, 0:512], lhsT=zeros[:, 0:64], rhs=zeros[:, 0:512],
                                               start=True, stop=False),
                      reads=["c_zeros"], writes=[psk(bo)])
                kb.op("pool", lambda e: e.memset(R32[sl][:], 0.0), writes=[("R32", sl)])
                kb.op("pool", lambda e: e.memset(Rb[sl][:], 0.0), writes=[("Rb", sl)])
                first = True
                for kbk in range(4 * qt + 3, -1, -1):
                    i = kbk - 4 * qt
                    diag = i >= 0
                    c0 = max(0, i) * 128
                    kb.op("pe", lambda e: e.matmul(ps[bs][:, c0:512], lhsT=kT[pb:pb + 64, kbk * 128:(kbk + 1) * 128],
                                                   rhs=qT[pb:pb + 64, qt * 512 + c0:(qt + 1) * 512], start=True, stop=True),
                          reads=[("kT", kbk // 4), ("qT", qt)], writes=[psk(bs)])
                    yield
                    kb.op("act", lambda e: e.activation(out=e32[sl][:, c0:512], in_=ps[bs][:, c0:512], func=AF.Exp, scale=0.125),
                          reads=[psk(bs)], writes=[("e32", sl)])
                    yield
                    kb.op("act", lambda e: e.activation(out=Pb[sl][:, c0:512], in_=e32[sl][:, c0:512], func=AF.Ln, bias=1.0),
                          reads=[("e32", sl)], writes=[("Pb", sl)])
                    if diag:
                        kb.op("dve", lambda e: e.tensor_tensor(out=Pb[sl][:, c0:c0 + 128], in0=Pb[sl][:, c0:c0 + 128], in1=m01[:], op=ALU.mult),
                              reads=[("Pb", sl), "c_m01_sb"], writes=[("Pb", sl)])
                    yield
                    kb.op("pe", lambda e: e.matmul(ps[bs][:, c0:512], lhsT=tri[:], rhs=Pb[sl][:, c0:512], start=False, stop=True,
                                                   skip_group_check=True),
                          reads=[("Pb", sl), "c_tri_m8"], writes=[psk(bs)])
                    if not first:
                        kb.op("pe", lambda e: e.matmul(ps[bs][:, c0:512], lhsT=onesm[:], rhs=Rb[sl][:, c0:512], start=False, stop=True,
                                                       skip_group_check=True),
                              reads=[("Rb", sl), "c_ones_m8"], writes=[psk(bs)])
                    yield
                    kb.op("act", lambda e: e.activation(out=Ab[sl][:, c0:512], in_=ps[bs][:, c0:512], func=AF.Exp, scale=0.125),
                          reads=[psk(bs)], writes=[("Ab", sl)])
                    if diag:
                        kb.op("dve", lambda e: e.tensor_tensor(out=Ab[sl][:, c0:c0 + 128], in0=Ab[sl][:, c0:c0 + 128], in1=m01[:], op=ALU.mult),
                              reads=[("Ab", sl), "c_m01_sb"], writes=[("Ab", sl)])
                    if kbk > 0:
                        kb.op("pool", lambda e: e.tensor_tensor(out=R32[sl][:, c0:512], in0=R32[sl][:, c0:512], in1=Pb[sl][:, c0:512], op=ALU.add),
                              reads=[("R32", sl), ("Pb", sl)], writes=[("R32", sl)])
                        kb.op("dve", lambda e: e.tensor_copy(out=Rb[sl][:, c0:512], in_=R32[sl][:, c0:512]), reads=[("R32", sl)], writes=[("Rb", sl)])
                    yield
                    kb.op("pe", lambda e: e.matmul(ps[bo][pb:pb + 64, c0:512], lhsT=V[:, kbk, pb:pb + 64], rhs=Ab[sl][:, c0:512],
                                                   start=False, stop=(kbk == 0)),
                          reads=[("V", kbk // 4), ("Ab", sl)], writes=[psk(bo)])
                    first = False
                kb.op("dve", lambda e: e.tensor_copy(out=OaT[pb:pb + 64, hp, qt * 512:(qt + 1) * 512], in_=ps[bo][pb:pb + 64, 0:512]),
                      reads=[psk(bo)], writes=[("OaT", 2 * hp + par, qt)])
                yield

            def lw_b1(hp):
                load_w(wq[0], ("wq", 0), hp * 128)
                load_w(wk[0], ("wk", 0), 512 + hp * 128)
                load_w(wv[0], ("wv", 0), 1024 + hp * 128)
            lw_b1(0)
            for hp in range(4):
                w = 0
                for tq in range(NQ):
                    b = proj_fm(wq[w], ("wq", w), tq)
                    evac(qT[:, tq * 512:(tq + 1) * 512], ps[b][:, 0:512], reads=[psk(b)], writes=[("qT", tq)])
                    b = proj_fm(wk[w], ("wk", w), tq)
                    evac(kT[:, tq * 512:(tq + 1) * 512], ps[b][:, 0:512], reads=[psk(b)], writes=[("kT", tq)])
                proj_v(wv[w], ("wv", w))
                if hp + 1 < 4:
                    lw_b1(hp + 1)
                makers = []
                for qt in range(NQ - 1, -1, -1):
                    for par in range(2):
                        makers.append(lambda sl, par=par, qt=qt: sb_stream(hp, par, qt, sl))
                run_streams(makers, NS)
            PB[0] = 4

        def stage_B2():
            qraws = [kb.sbuf(f"qraw{i}", [128, 512], BF16) for i in range(2)]
            PB[0] = 8
            rcnt = [0]
            perm = csb["c_perm"]
            lamt = kb.sbuf("lamt", [128, 4, 64], F32)
            lams = kb.sbuf("lams", [128, 8], F32)
            for n, k in enumerate(("lambda_q1", "lambda_k1", "lambda_q2", "lambda_k2")):
                kb.dma("sp", lambda e: e.dma_start(out=lamt[:, n, :], in_=bcast_rows(lam_d[k], 64)), writes=["lamt"], sem="lamt")
            kb.op("dve", lambda e: e.tensor_tensor(out=lamt[:, 0, :], in0=lamt[:, 0, :], in1=lamt[:, 1, :], op=ALU.mult),
                  reads=["lamt"], writes=["lamt"])
            kb.op("dve", lambda e: e.tensor_tensor(out=lamt[:, 2, :], in0=lamt[:, 2, :], in1=lamt[:, 3, :], op=ALU.mult),
                  reads=["lamt"], writes=["lamt"])
            kb.op("dve", lambda e: e.reduce_sum(out=lams[:, 0:1], in_=lamt[:, 0, :], axis=AX.X), reads=["lamt"], writes=["lams"])
            kb.op("dve", lambda e: e.reduce_sum(out=lams[:, 1:2], in_=lamt[:, 2, :], axis=AX.X), reads=["lamt"], writes=["lams"])
            kb.op("act", lambda e: e.activation(out=lams[:, 2:4], in_=lams[:, 0:2], func=AF.Exp), reads=["lams"], writes=["lams"])
            kb.op("dve", lambda e: e.tensor_tensor(out=lams[:, 4:5], in0=lams[:, 3:4], in1=lams[:, 2:3], op=ALU.subtract),
                  reads=["lams"], writes=["lams"])
            kb.op("dve", lambda e: e.tensor_scalar(out=lams[:, 5:6], in0=lams[:, 4:5], scalar1=-0.2, scalar2=None, op0=ALU.add),
                  reads=["lams"], writes=["lams"])
            neglam = lams[:, 5:6]
            gsub = kb.sbuf("gsub", [128, 128], F32)
            kb.dma("sp", lambda e: e.dma_start(out=gsub[:], in_=bcast_rows(g_sub_d, 128)), writes=["gsub"], sem="gsub")
            kb.op("dve", lambda e: e.tensor_scalar(out=gsub[:], in0=gsub[:], scalar1=0.8, scalar2=None, op0=ALU.mult),
                  reads=["gsub"], writes=["gsub"])
            kb.op("pool", lambda e: e.memset(V[:, :, 128:129], 1.0), writes=["Vones"])
            cs = [kb.sbuf(f"cos{i}", [128, 512], F32) for i in range(2)]
            sn = [kb.sbuf(f"sin{i}", [128, 512], F32) for i in range(2)]
            r1 = [kb.sbuf(f"rt1_{i}", [128, 512], F32) for i in range(2)]
            r2 = [kb.sbuf(f"rt2_{i}", [128, 512], F32) for i in range(2)]
            o32 = [kb.sbuf(f"o32_{i}", [128, 128], F32) for i in range(2)]
            t32 = [kb.sbuf(f"t32_{i}", [128, 128], F32) for i in range(2)]
            onb = [kb.sbuf(f"onb_{i}", [128, 128], BF16) for i in range(2)]
            rr = [kb.sbuf(f"rr_{i}", [128, 8], F32) for i in range(2)]
            mdf = csb["c_mask_df"]
            m01d = csb["c_m01_df"]
            m01db = bass.AP(m01d, 0, [[128, 128], [0, 2], [1, 128]])
            Pd = [kb.sbuf(f"Pd{i}", [128, 2, 512], BF16) for i in range(2)]
            rc = 0
            step = 0
            fc = 0
            def lw_b2(dh):
                w = 0
                cq = 1536 + dh * 128
                ck = 2048 + dh * 128
                cv = 2560 + dh * 128
                load_w(wq[w], ("wq", w), cq)
                load_w(wk[w], ("wk", w), ck)
                load_w(wv[w], ("wv", w), cv)
            accS = [kb.sbuf(f"accS{i}", [128, 3 * 396], F32) for i in range(2)]
            afc = [0]
            fin_pending = []
            fcc = [0]

            def drain(g):
                for _ in g:
                    pass

            def step_bg():
                if fin_pending:
                    try:
                        next(fin_pending[0])
                    except StopIteration:
                        fin_pending.pop(0)

            def finalize(dh, qt, af):
                psb7 = ps[7][:].bitcast(BF16)
                A_ = accS[af]
                for j in range(4):
                    f = fcc[0] % 2
                    fcc[0] += 1
                    a1 = j * 2
                    a2 = j * 2 + 1
                    c1 = (a1 // 3) * 396 + (a1 % 3) * 132
                    c2 = (a2 // 3) * 396 + (a2 % 3) * 132
                    k1 = ("accS", af, a1 // 3)
                    k2 = ("accS", af, a2 // 3)
                    kb.op("dve", lambda e: e.reciprocal(out=rr[f][:, 0:1], in_=A_[:, c1 + 128:c1 + 129]), reads=[k1], writes=[("rr", f)])
                    kb.op("dve", lambda e: e.reciprocal(out=rr[f][:, 1:2], in_=A_[:, c2 + 128:c2 + 129]), reads=[k2], writes=[("rr", f)])
                    kb.op("dve", lambda e: e.tensor_tensor(out=rr[f][:, 2:3], in0=rr[f][:, 1:2], in1=neglam, op=ALU.mult),
                          reads=[("rr", f), "lams"], writes=[("rr", f)])
                    yield
                    kb.op("dve", lambda e: e.tensor_scalar(out=t32[f][:], in0=A_[:, c2:c2 + 128], scalar1=rr[f][:, 2:3], scalar2=None, op0=ALU.mult),
                          reads=[k2, ("rr", f)], writes=[("t32", f)])
                    kb.op("dve", lambda e: e.scalar_tensor_tensor(out=o32[f][:], in0=A_[:, c1:c1 + 128], scalar=rr[f][:, 0:1], in1=t32[f][:],
                                                                  op0=ALU.mult, op1=ALU.add),
                          reads=[k1, ("rr", f), ("t32", f)], writes=[("o32", f)])
                    yield
                    kb.op("act", lambda e: e.activation(out=junk[:, 0:128], in_=o32[f][:], func=AF.Square, accum_out=rr[f][:, 3:4]),
                          reads=[("o32", f)], writes=["junk", ("rr", f)])
                    kb.op("act", lambda e: e.activation(out=rr[f][:, 4:5], in_=rr[f][:, 3:4], func=AF.Ln, bias=EPS, scale=1.0 / 128),
                          reads=[("rr", f)], writes=[("rr", f)])
                    kb.op("act", lambda e: e.activation(out=rr[f][:, 5:6], in_=rr[f][:, 4:5], func=AF.Exp, scale=-0.5),
                          reads=[("rr", f)], writes=[("rr", f)])
                    yield
                    kb.op("dve", lambda e: e.scalar_tensor_tensor(out=onb[f][:], in0=o32[f][:], scalar=rr[f][:, 5:6], in1=gsub[:],
                                                                  op0=ALU.mult, op1=ALU.mult),
                          reads=[("o32", f), ("rr", f), "gsub"], writes=[("onb", f)])
                    yield
                    kb.op("pe", lambda e: e.transpose(out=psb7[:, j * 128:(j + 1) * 128], in_=onb[f][:], identity=ident[:]),
                          reads=[("onb", f), "c_ident_bf"], writes=[psk(7)])
                    yield
                kb.op("act", lambda e: e.activation(out=ObT[:, dh, qt * 512:(qt + 1) * 512], in_=psb7[:, 0:512], func=AF.Copy),
                      reads=[psk(7)], writes=[("ObT", dh, qt)])

            lw_b2(0)
            for dh in range(4):
                w = 0
                pend_rope = []

                def rope_tail(tq, ci, ri, ba, dstT, dk_):
                    qraw = qraws[ri]
                    bb = pcount[0] % PB[0]
                    pcount[0] += 1
                    kb.op("pe", lambda e: e.matmul(ps[bb][:, 0:512], lhsT=perm[:], rhs=qraw[:], start=True, stop=True),
                          reads=[("qraw", ri), "c_perm"], writes=[psk(bb)])
                    kb.op("dve", lambda e: e.tensor_tensor(out=r1[ri][:], in0=ps[ba][:, 0:512], in1=cs[ci][:], op=ALU.mult),
                          reads=[psk(ba), ("cos", ci)], writes=[("r1", ri)])
                    kb.op("dve", lambda e: e.tensor_tensor(out=r2[ri][:], in0=ps[bb][:, 0:512], in1=sn[ci][:], op=ALU.mult),
                          reads=[psk(bb), ("sin", ci)], writes=[("r2", ri)])
                    kb.op("pool", lambda e: e.tensor_tensor(out=dstT[:, tq * 512:(tq + 1) * 512], in0=r1[ri][:], in1=r2[ri][:], op=ALU.add),
                          reads=[("r1", ri), ("r2", ri)], writes=[(dk_, tq)])

                for tq in range(NQ):
                    ci = tq % 2
                    kb.dma("sp", lambda e: e.dma_start(out=cs[ci][:], in_=cos_d[:, tq * 512:(tq + 1) * 512]), writes=[("cos", ci)], sem=f"cos{ci}")
                    kb.dma("sp", lambda e: e.dma_start(out=sn[ci][:], in_=sin_d[:, tq * 512:(tq + 1) * 512]), writes=[("sin", ci)], sem=f"sin{ci}")
                    for (wa, wak, dstT, dk_) in ((wq[w], ("wq", w), qT, "qT"), (wk[w], ("wk", w), kT, "kT")):
                        ri = rcnt[0] % 2
                        rcnt[0] += 1
                        ba = proj_fm(wa, wak, tq)
                        kb.op("act", lambda e: e.activation(out=qraws[ri][:], in_=ps[ba][:, 0:512], func=AF.Copy), reads=[psk(ba)], writes=[("qraw", ri)])
                        pend_rope.append((tq, ci, ri, ba, dstT, dk_))
                        if len(pend_rope) > 1:
                            rope_tail(*pend_rope.pop(0))
                while pend_rope:
                    rope_tail(*pend_rope.pop(0))
                proj_v(wv[w], ("wv", w))
                if dh + 1 < 4:
                    lw_b2(dh + 1)
                for qt in range(NQ):
                    for bz in (4, 5, 6):
                        kb.op("pe", lambda e: e.matmul(ps[bz][:, 0:512], lhsT=zeros[:, 0:128], rhs=zeros[:, 0:512], start=True, stop=False),
                              reads=["c_zeros"], writes=[psk(bz)])

                    def acc(j, br):
                        a = j * 2 + br
                        return 4 + a // 3, (a % 3) * 132
                    nsteps = 4 * qt + 4

                    def scores(kbk):
                        i = kbk - 4 * qt
                        diag = i >= 0
                        c0 = max(0, i) * 128
                        s = kbk % 2
                        for br in range(2):
                            bs = 2 * s + br
                            pb = 64 * br
                            kb.op("pe", lambda e: e.matmul(ps[bs][:, c0:512], lhsT=kT[pb:pb + 64, kbk * 128:(kbk + 1) * 128],
                                                           rhs=qT[pb:pb + 64, qt * 512 + c0:(qt + 1) * 512], start=True, stop=True),
                                  reads=[("kT", kbk // 4), ("qT", qt)], writes=[psk(bs)])
                        kb.op("act", lambda e: e.activation(out=Pd[s][:, :, c0:512], in_=psall[:, 2 * s:2 * s + 2, c0:512], func=AF.Exp, scale=0.125),
                              reads=[psk(2 * s), psk(2 * s + 1)], writes=[("P", s)])
                        if diag:
                            kb.op("pool", lambda e: e.tensor_tensor(out=Pd[s][:, :, c0:c0 + 128], in0=Pd[s][:, :, c0:c0 + 128], in1=m01db, op=ALU.mult),
                                  reads=[("P", s), "c_m01_df"], writes=[("P", s)])

                    def av(kbk):
                        i = kbk - 4 * qt
                        s = kbk % 2
                        for j in range(max(0, i), 4):
                            for br in range(2):
                                ba, off = acc(j, br)
                                kb.op("pe", lambda e: e.matmul(ps[ba][:, off:off + 129], lhsT=Pd[s][:, br, j * 128:(j + 1) * 128], rhs=V[:, kbk, 0:129],
                                                               start=False, stop=False),
                                      reads=[("P", s), ("V", kbk // 4), "Vones"], writes=[psk(ba)])

                    scores(0)
                    for kbk in range(nsteps):
                        if kbk + 1 < nsteps:
                            scores(kbk + 1)
                        av(kbk)
                        step_bg()
                    for bz in (4, 5, 6):
                        kb.op("pe", lambda e: e.matmul(ps[bz][:, 0:2], lhsT=zeros[:, 0:128], rhs=zeros[:, 0:2], start=False, stop=True),
                              reads=["c_zeros"], writes=[psk(bz)])
                    af = afc[0] % 2
                    afc[0] += 1
                    for bi in range(3):
                        evac(accS[af][:, bi * 396:(bi + 1) * 396], ps[4 + bi][:, 0:396], reads=[psk(4 + bi)], writes=[("accS", af, bi)])
                    while fin_pending:
                        drain(fin_pending[0])
                        fin_pending.pop(0)
                    fin_pending.append(finalize(dh, qt, af))
                while fin_pending:
                    drain(fin_pending[0])
                    fin_pending.pop(0)

        def stage_C():
            wua = [kb.sbuf(f"wua{i}", [128, 4, 128], BF16) for i in range(2)]
            wub = [kb.sbuf(f"wub{i}", [128, 4, 128], BF16) for i in range(2)]
            sga = [kb.sbuf(f"sga{i}", [128, 512], F32) for i in range(2)]
            sgb = [kb.sbuf(f"sgb{i}", [128, 512], F32) for i in range(2)]
            ya = [kb.sbuf(f"ya{i}", [128, 512], F32) for i in range(2)]
            yb = [kb.sbuf(f"yb{i}", [128, 512], F32) for i in range(2)]
            yo = [kb.sbuf(f"yo{i}", [128, 512], BF16) for i in range(2)]
            it = 0
            ub = 0
            wga = [kb.sbuf(f"wga{i}", [128, 8, 128], BF16) for i in range(2)]
            wgb = [kb.sbuf(f"wgb{i}", [128, 8, 128], BF16) for i in range(2)]

            def lw_c(ec):
                w = ec % 2
                load_w(wga[w], ("wga", w), 3072 + ec * 128)
                load_w(wgb[w], ("wgb", w), 4096 + ec * 128)
                kb.dma("pool", lambda e: e.dma_start(out=wua[w][:], in_=w_up_a_d[:, ec * 128:(ec + 1) * 128].rearrange("(c p) n -> p c n", p=128)),
                       writes=[("wua", w)], sem=f"wua{w}")
                kb.dma("pool", lambda e: e.dma_start(out=wub[w][:], in_=w_up_b_d[:, ec * 128:(ec + 1) * 128].rearrange("(c p) n -> p c n", p=128)),
                       writes=[("wub", w)], sem=f"wub{w}")
            lw_c(0)
            for ec in range(8):
                w = ec % 2
                if ec + 1 < 8:
                    lw_c(ec + 1)
                for tq in range(NQ):
                    i = it % 2
                    it += 1
                    bga = proj_fm(wga[w], ("wga", w), tq)
                    bgb = proj_fm(wgb[w], ("wgb", w), tq)
                    bua = 4 + (ub % 4)
                    bub = 4 + ((ub + 1) % 4)
                    ub += 2
                    for (bb_, wt_, wk_, OT, ok_, nh) in ((bua, wua[w], ("wua", w), OaT, "OaT", 8), (bub, wub[w], ("wub", w), ObT, "ObT", 4)):
                        for c in range(4):
                            rk = [(ok_, 2 * c, tq), (ok_, 2 * c + 1, tq)] if nh == 8 else [(ok_, c, tq)]
                            kb.op("pe", lambda e: e.matmul(ps[bb_][:, 0:512], lhsT=wt_[:, c, :], rhs=OT[:, c, tq * 512:(tq + 1) * 512],
                                                           start=(c == 0), stop=(c == 3)),
                                  reads=[wk_] + rk, writes=[psk(bb_)])
                    kb.op("act", lambda e: e.activation(out=sga[i][:], in_=ps[bga][:, 0:512], func=AF.Sigmoid), reads=[psk(bga)], writes=[("sga", i)])
                    kb.op("act", lambda e: e.activation(out=sgb[i][:], in_=ps[bgb][:, 0:512], func=AF.Sigmoid), reads=[psk(bgb)], writes=[("sgb", i)])
                    kb.op("dve", lambda e: e.tensor_tensor(out=ya[i][:], in0=ps[bua][:, 0:512], in1=sga[i][:], op=ALU.mult),
                          reads=[psk(bua), ("sga", i)], writes=[("ya", i)])
                    kb.op("dve", lambda e: e.tensor_tensor(out=yb[i][:], in0=ps[bub][:, 0:512], in1=sgb[i][:], op=ALU.mult),
                          reads=[psk(bub), ("sgb", i)], writes=[("yb", i)])
                    kb.op("pool", lambda e: e.tensor_tensor(out=yo[i][:], in0=ya[i][:], in1=yb[i][:], op=ALU.add),
                          reads=[("ya", i), ("yb", i)], writes=[("yo", i)])
                    kb.dma("sp", lambda e: e.dma_start(out=yT_d[ec, :, tq * 512:(tq + 1) * 512], in_=yo[i][:]),
                           reads=[("yo", i)], writes=[("yT_d", ec, tq)], sem=f"yo{i}")

        def stage_D(h2all, M32, OH, W12):
            wout = kb.sbuf("wout", [128, 8, 1024], BF16)
            for hf in range(2):
                kb.dma("pool", lambda e: e.dma_start(out=wout[:, :, hf * 512:(hf + 1) * 512],
                                                     in_=w_out_d[:, hf * 512:(hf + 1) * 512].rearrange("(kc p) n -> p kc n", p=128)),
                       writes=["wout"], sem="wout")
            g2 = kb.sbuf("g2", [128, 1024], F32)
            kb.dma("sp", lambda e: e.dma_start(out=g2[:], in_=bcast_rows(g_ffn_d, 1024)), writes=["g2"], sem="g2")
            wr = kb.sbuf("wr", [128, 8, 36], F32)
            kb.dma("sp", lambda e: e.dma_start(out=wr[:, :, 0:4], in_=w_rg_d.rearrange("(kc p) c -> p kc c", p=128)), writes=["wr"], sem="wr")
            kb.dma("sp", lambda e: e.dma_start(out=wr[:, :, 4:36], in_=w_re_d.rearrange("(kc p) c -> p kc c", p=128)), writes=["wr"], sem="wr")
            rbias = kb.sbuf("rbias", [128, 36], F32)
            kb.dma("sp", lambda e: e.dma_start(out=rbias[:, 0:4], in_=bcast_rows(b_rg_d, 4)), writes=["rbias"], sem="rbias")
            kb.dma("sp", lambda e: e.dma_start(out=rbias[:, 4:36], in_=bcast_rows(b_re_d, 32)), writes=["rbias"], sem="rbias")
            identf = csb["c_ident_f"]
            yt = [kb.sbuf(f"yt{i}", [128, 8, 512], BF16) for i in range(2)]
            xt = [kb.sbuf(f"xtD{i}", [128, 1024], F32) for i in range(3)]
            x1t = [kb.sbuf(f"x1t{i}", [128, 1024], F32) for i in range(3)]
            h2f = [kb.sbuf(f"h2f{i}", [128, 1024], F32) for i in range(3)]
            h2T = [kb.sbuf(f"h2T{i}", [128, 8, 128], F32) for i in range(3)]
            rt = [kb.sbuf(f"rt{i}", [128, 16], F32) for i in range(3)]
            lg = [kb.sbuf(f"lg{i}", [128, 36], F32) for i in range(3)]
            em = [kb.sbuf(f"em{i}", [128, 32], F32) for i in range(3)]
            em2 = [kb.sbuf(f"em2{i}", [128, 32], F32) for i in range(3)]
            gm = [kb.sbuf(f"gm{i}", [128, 8], F32) for i in range(3)]
            def d_tile(tq, j, sl):
                yi = tq % 2
                tt = 4 * tq + j
                i = sl
                kb.dma("sp", lambda e: e.dma_start(out=xt[i][:], in_=x_d[tt * 128:(tt + 1) * 128, :]), writes=[("xtD", i)], sem=f"xtD{i}")
                for hf in range(2):
                    b = 2 * sl + hf
                    for ec in range(8):
                        kb.op("pe", lambda e: e.matmul(ps[b][:, 0:512], lhsT=yt[yi][:, ec, j * 128:(j + 1) * 128],
                                                       rhs=wout[:, ec, hf * 512:(hf + 1) * 512], start=(ec == 0), stop=(ec == 7)),
                              reads=[("yt", yi), "wout"], writes=[psk(b)])
                    kb.op("dve", lambda e: e.tensor_tensor(out=x1t[i][:, hf * 512:(hf + 1) * 512], in0=ps[b][:, 0:512],
                                                           in1=xt[i][:, hf * 512:(hf + 1) * 512], op=ALU.add),
                          reads=[psk(b), ("xtD", i)], writes=[("x1t", i, hf)])
                kb.dma("pool", lambda e: e.dma_start(out=x1_d[tt * 128:(tt + 1) * 128, :], in_=x1t[i][:]),
                       reads=[("x1t", i, 0), ("x1t", i, 1)], writes=[("x1_d", tt)], sem=f"x1t{i}")
                yield
                R = ("rt", i)
                kb.op("act", lambda e: e.activation(out=junk[:], in_=x1t[i][:], func=AF.Square, accum_out=rt[i][:, 0:1]),
                      reads=[("x1t", i, 0), ("x1t", i, 1)], writes=["junk", R])
                kb.op("act", lambda e: e.activation(out=rt[i][:, 1:2], in_=rt[i][:, 0:1], func=AF.Ln, bias=EPS, scale=1.0 / 1024), reads=[R], writes=[R])
                kb.op("act", lambda e: e.activation(out=rt[i][:, 2:3], in_=rt[i][:, 1:2], func=AF.Exp, scale=-0.5), reads=[R], writes=[R])
                kb.op("dve", lambda e: e.scalar_tensor_tensor(out=h2f[i][:], in0=x1t[i][:], scalar=rt[i][:, 2:3], in1=g2[:], op0=ALU.mult, op1=ALU.mult),
                      reads=[("x1t", i, 0), ("x1t", i, 1), R, "g2"], writes=[("h2f", i)])
                kb.op("pool", lambda e: e.tensor_copy(out=h2all[:, tt, :], in_=h2f[i][:]), reads=[("h2f", i)], writes=[("h2all", tt)])
                yield
                for hf in range(2):
                    b = 6 + hf
                    for k4 in range(4):
                        kc = hf * 4 + k4
                        kb.op("pe", lambda e: e.transpose(out=ps[b][:, k4 * 128:(k4 + 1) * 128], in_=h2f[i][:, kc * 128:(kc + 1) * 128], identity=identf[:]),
                              reads=[("h2f", i), "c_ident_f"], writes=[psk(b)])
                    evac(h2T[i][:, hf * 4:(hf + 1) * 4, :], ps[b][:, 0:512].rearrange("p (k t) -> p k t", k=4), reads=[psk(b)], writes=[("h2T", i, hf)])
                yield
                b = 2 * sl
                for kc in range(8):
                    kb.op("pe", lambda e: e.matmul(ps[b][:, 0:36], lhsT=h2T[i][:, kc, :], rhs=wr[:, kc, :], start=(kc == 0), stop=(kc == 7)),
                          reads=[("h2T", i, kc // 4), "wr"], writes=[psk(b)])
                L = ("lg", i)
                kb.op("dve", lambda e: e.tensor_tensor(out=lg[i][:], in0=ps[b][:, 0:36], in1=rbias[:], op=ALU.add), reads=[psk(b), "rbias"], writes=[L])
                yield
                kb.op("dve", lambda e: e.reduce_max(out=rt[i][:, 3:4], in_=lg[i][:, 0:4], axis=AX.X), reads=[L], writes=[R])
                kb.op("dve", lambda e: e.tensor_scalar(out=gm[i][:, 0:4], in0=lg[i][:, 0:4], scalar1=rt[i][:, 3:4], scalar2=None, op0=ALU.is_equal),
                      reads=[L, R], writes=[("gm", i)])
                kb.op("dve", lambda e: e.tensor_scalar(out=rt[i][:, 4:5], in0=rt[i][:, 3:4], scalar1=-1.0, scalar2=None, op0=ALU.mult), reads=[R], writes=[R])
                kb.op("act", lambda e: e.activation(out=gm[i][:, 4:8], in_=lg[i][:, 0:4], func=AF.Exp, bias=rt[i][:, 4:5], accum_out=rt[i][:, 5:6]),
                      reads=[L, R], writes=[("gm2", i), R])
                kb.op("dve", lambda e: e.reciprocal(out=rt[i][:, 6:7], in_=rt[i][:, 5:6]), reads=[R], writes=[R])
                yield
                kb.op("dve", lambda e: e.tensor_scalar(out=gm[i][:, 0:4], in0=gm[i][:, 0:4], scalar1=1e30, scalar2=-1e30, op0=ALU.mult, op1=ALU.add),
                      reads=[("gm", i)], writes=[("gm", i)])
                pen = bass.AP(gm[i], 0, [[8, 128], [1, 4], [0, 8]])
                kb.op("dve", lambda e: e.tensor_tensor(out=em[i][:].rearrange("p (g e) -> p g e", g=4), in0=lg[i][:, 4:36].rearrange("p (g e) -> p g e", g=4),
                                                       in1=pen, op=ALU.add), reads=[L, ("gm", i)], writes=[("em", i)])
                kb.op("dve", lambda e: e.reduce_max(out=rt[i][:, 7:8], in_=em[i][:], axis=AX.X), reads=[("em", i)], writes=[R])
                kb.op("dve", lambda e: e.tensor_scalar(out=OH[:, tt, 0, :], in0=em[i][:], scalar1=rt[i][:, 7:8], scalar2=None, op0=ALU.is_equal),
                      reads=[("em", i), R], writes=[("OH", tt)])
                yield
                kb.op("dve", lambda e: e.scalar_tensor_tensor(out=em2[i][:], in0=OH[:, tt, 0, :], scalar=-1e30, in1=em[i][:], op0=ALU.mult, op1=ALU.add),
                      reads=[("OH", tt), ("em", i)], writes=[("em2", i)])
                kb.op("dve", lambda e: e.reduce_max(out=rt[i][:, 8:9], in_=em2[i][:], axis=AX.X), reads=[("em2", i)], writes=[R])
                kb.op("dve", lambda e: e.tensor_scalar(out=OH[:, tt, 1, :], in0=em2[i][:], scalar1=rt[i][:, 8:9], scalar2=None, op0=ALU.is_equal),
                      reads=[("em2", i), R], writes=[("OH", tt)])
                kb.op("dve", lambda e: e.tensor_tensor(out=rt[i][:, 9:10], in0=rt[i][:, 8:9], in1=rt[i][:, 7:8], op=ALU.subtract), reads=[R], writes=[R])
                kb.op("act", lambda e: e.activation(out=rt[i][:, 10:11], in_=rt[i][:, 9:10], func=AF.Exp), reads=[R], writes=[R])
                yield
                kb.op("dve", lambda e: e.tensor_scalar(out=rt[i][:, 11:12], in0=rt[i][:, 10:11], scalar1=1.0, scalar2=None, op0=ALU.add), reads=[R], writes=[R])
                kb.op("dve", lambda e: e.reciprocal(out=rt[i][:, 12:13], in_=rt[i][:, 11:12]), reads=[R], writes=[R])
                kb.op("dve", lambda e: e.tensor_tensor(out=W12[:, tt, 0:1], in0=rt[i][:, 6:7], in1=rt[i][:, 12:13], op=ALU.mult), reads=[R], writes=[("W12", tt)])
                kb.op("dve", lambda e: e.tensor_tensor(out=W12[:, tt, 1:2], in0=rt[i][:, 6:7], in1=W12[:, tt, 0:1], op=ALU.subtract),
                      reads=[R, ("W12", tt)], writes=[("W12", tt)])
                kb.op("dve", lambda e: e.tensor_tensor(out=M32[:, tt, :], in0=OH[:, tt, 0, :], in1=OH[:, tt, 1, :], op=ALU.add),
                      reads=[("OH", tt)], writes=[("M32", tt)])
                yield

            def d_tile_start(tq, j, sl):
                if j == 0:
                    yi = tq % 2
                    kb.dma("sp", lambda e: e.dma_start(out=yt[yi][:], in_=yT_d[:, :, tq * 512:(tq + 1) * 512].rearrange("c p t -> p c t")),
                           reads=[("yT_d", ec, tq) for ec in range(8)], writes=[("yt", yi)], sem=f"yt{yi}")
                yield from d_tile(tq, j, sl)

            makers = []
            for tq in range(NQ):
                for j in range(4):
                    makers.append(lambda sl, tq=tq, j=j: d_tile_start(tq, j, sl))
            run_streams(makers, 3, bg_every=10 ** 9)

        def stage_E(h2all, M32, OH, W12, desti, widx):
            trif = csb["c_tri_f"]
            onesf = csb["c_ones_f"]
            Mcum = kb.sbuf("Mcum", [128, NT + 1, 32], F32)
            kb.op("dve", lambda e: e.memset(Mcum[:, 0, :], 0.0), writes=[("Mcum", 0)])
            for tt in range(NT):
                kb.op("dve", lambda e: e.tensor_tensor(out=Mcum[:, tt + 1, :], in0=Mcum[:, tt, :], in1=M32[:, tt, :], op=ALU.add),
                      reads=[("Mcum", tt), ("M32", tt)], writes=[("Mcum", tt + 1)])
            cnt = kb.sbuf("cnt", [128, 32], F32)
            cnti = kb.sbuf("cnti", [128, 32], I32)
            cnti2 = kb.sbuf("cnti2", [128, 32], I32)
            padded = kb.sbuf("padded", [128, 32], F32)
            pend = kb.sbuf("pend", [128, 32], F32)
            pstart = kb.sbuf("pstart", [128, 32], F32)
            ones32 = kb.sbuf("ones32", [128, 32], F32)
            kb.op("pe", lambda e: e.matmul(ps[0][:, 0:32], lhsT=onesf[:], rhs=Mcum[:, NT, :], start=True, stop=True),
                  reads=[("Mcum", NT), "c_ones_f"], writes=[psk(0)])
            kb.op("dve", lambda e: e.tensor_copy(out=cnti[:], in_=ps[0][:, 0:32]), reads=[psk(0)], writes=["cnti"])
            kb.op("dve", lambda e: e.tensor_scalar(out=cnti2[:], in0=cnti[:], scalar1=127, scalar2=None, op0=ALU.add), reads=["cnti"], writes=["cnti2"])
            kb.op("dve", lambda e: e.tensor_scalar(out=cnti[:], in0=cnti2[:], scalar1=7, scalar2=7, op0=ALU.arith_shift_right, op1=ALU.logical_shift_left),
                  reads=["cnti2"], writes=["cnti"])
            kb.op("dve", lambda e: e.tensor_copy(out=padded[:], in_=cnti[:]), reads=["cnti"], writes=["padded"])
            kb.op("dve", lambda e: e.memset(ones32[:], 1.0), writes=["ones32"])
            kb.op("dve", lambda e: e.tensor_tensor_scan(out=pend[:], data0=ones32[:], data1=padded[:], initial=0.0, op0=ALU.mult, op1=ALU.add),
                  reads=["ones32", "padded"], writes=["pend"])
            kb.op("dve", lambda e: e.tensor_tensor(out=pstart[:], in0=pend[:], in1=padded[:], op=ALU.subtract), reads=["pend", "padded"], writes=["pstart"])
            destf = kb.sbuf("destf", [128, 2 * NT], F32)
            base = [kb.sbuf(f"base{i}", [128, 32], F32) for i in range(2)]
            prod = [kb.sbuf(f"prod{i}", [128, 32], F32) for i in range(2)]
            for tt in range(NT):
                i = tt % 2
                b = 1 + i
                kb.op("pe", lambda e: e.matmul(ps[b][:, 0:32], lhsT=onesf[:], rhs=Mcum[:, tt, :], start=True, stop=False),
                      reads=[("Mcum", tt), "c_ones_f"], writes=[psk(b)])
                kb.op("pe", lambda e: e.matmul(ps[b][:, 0:32], lhsT=trif[:], rhs=M32[:, tt, :], start=False, stop=True),
                      reads=[("M32", tt), "c_tri_f"], writes=[psk(b)])
                kb.op("dve", lambda e: e.tensor_tensor(out=base[i][:], in0=ps[b][:, 0:32], in1=pstart[:], op=ALU.add),
                      reads=[psk(b), "pstart"], writes=[("base", i)])
                for k in range(2):
                    kb.op("dve", lambda e: e.tensor_tensor(out=prod[k][:], in0=OH[:, tt, k, :], in1=base[i][:], op=ALU.mult),
                          reads=[("OH", tt), ("base", i)], writes=[("prod", k)])
                    kb.op("dve", lambda e: e.reduce_sum(out=destf[:, 2 * tt + k:2 * tt + k + 1], in_=prod[k][:], axis=AX.X),
                          reads=[("prod", k)], writes=["destf"])
            kb.op("dve", lambda e: e.tensor_copy(out=desti[:], in_=destf[:]), reads=["destf"], writes=["desti"])
            thr = kb.sbuf("thr", [128, NB], F32)
            cmp_ = kb.sbuf("cmp", [128, NB, 32], F32)
            be = kb.sbuf("be", [128, NB], F32)
            pidx = kb.sbuf("pidx", [128, 1], F32)
            kb.op("pool", lambda e: e.iota(thr[:], pattern=[[128, NB]], base=0, channel_multiplier=0, allow_small_or_imprecise_dtypes=True), writes=["thr"])
            kb.op("pool", lambda e: e.iota(pidx[:], pattern=[[1, 1]], base=0, channel_multiplier=1, allow_small_or_imprecise_dtypes=True), writes=["pidx"])
            pend_b = bass.AP(pend, 0, [[32, 128], [0, NB], [1, 32]])
            thr_b = bass.AP(thr, 0, [[NB, 128], [1, NB], [0, 32]])
            kb.op("dve", lambda e: e.tensor_tensor(out=cmp_[:], in0=pend_b, in1=thr_b, op=ALU.is_le), reads=["pend", "thr"], writes=["cmp"])
            kb.op("dve", lambda e: e.reduce_sum(out=be[:], in_=cmp_[:], axis=AX.X), reads=["cmp"], writes=["be"])
            kb.op("dve", lambda e: e.tensor_scalar(out=be[:], in0=be[:], scalar1=31.0, scalar2=128.0, op0=ALU.min, op1=ALU.mult), reads=["be"], writes=["be"])
            kb.op("dve", lambda e: e.tensor_scalar(out=be[:], in0=be[:], scalar1=pidx[:, 0:1], scalar2=None, op0=ALU.add), reads=["be", "pidx"], writes=["be"])
            skipm = kb.sbuf("skipm", [128, NB], F32)
            be2 = kb.sbuf("be2", [128, NB], F32)
            kb.op("dve", lambda e: e.tensor_tensor(out=skipm[:, 2:NB], in0=be[:, 2:NB], in1=be[:, 0:NB - 2], op=ALU.is_equal), reads=["be"], writes=["skipm"])
            kb.op("dve", lambda e: e.tensor_copy(out=be2[:, 0:2], in_=be[:, 0:2]), reads=["be"], writes=["be2"])
            kb.op("dve", lambda e: e.scalar_tensor_tensor(out=be2[:, 2:NB], in0=skipm[:, 2:NB], scalar=1048576.0, in1=be[:, 2:NB], op0=ALU.mult, op1=ALU.add),
                  reads=["skipm", "be"], writes=["be2"])
            kb.op("dve", lambda e: e.tensor_copy(out=widx[:], in_=be2[:]), reads=["be2"], writes=["widx"])
            for tt in range(NT):
                for k in range(2):
                    kb.dma("pool", lambda e: e.indirect_dma_start(out=xd_d, out_offset=bass.IndirectOffsetOnAxis(ap=desti[:, 2 * tt + k:2 * tt + k + 1], axis=0),
                                                                  in_=h2all[:, tt, :], in_offset=None),
                           reads=[("h2all", tt), "desti"], writes=[("xd_d", (2 * tt + k) % 4)], sem=f"scat{(2 * tt + k) % 4}")

        def stage_F(widx):
            NBUF = 3
            wcomb = [kb.sbuf(f"wcomb{i}", [128, 6144], BF16) for i in range(2)]
            wg = [t[:, 0:2048] for t in wcomb]
            wu = [t[:, 2048:4096] for t in wcomb]
            wd = [t[:, 4096:6144] for t in wcomb]
            xdt = [kb.sbuf(f"xdt{i}", [128, 1024], BF16) for i in range(NBUF)]
            xdT = [kb.sbuf(f"xdT{i}", [128, 8, 128], BF16) for i in range(2)]
            sg = [kb.sbuf(f"sg{i}", [128, 256], F32) for i in range(2)]
            actT = [kb.sbuf(f"actT{i}", [128, 256], BF16) for i in range(2)]
            ydt = [kb.sbuf(f"ydt{i}", [128, 1024], F32) for i in range(2)]

            oob_reg = st.enter_context(nc.gpsimd.register("oob_bound"))
            nc.gpsimd.reg_mov(oob_reg, 4095)

            def prefetch_w(b_):
                iw = b_ % 2
                kb.dma("pool", lambda e: e.indirect_dma_start(out=wcomb[iw][:], out_offset=None, in_=wall_d,
                                                              in_offset=bass.IndirectOffsetOnAxis(ap=widx[:, b_:b_ + 1], axis=0),
                                                              bounds_check=oob_reg, oob_is_err=False),
                       reads=["widx"] + WALL_KEYS, writes=[("wcomb", iw)], sem=f"wcomb{iw}")

            def prefetch(b_):
                i = b_ % NBUF
                kb.dma("sp", lambda e: e.dma_start(out=xdt[i][:], in_=xd_d[b_ * 128:(b_ + 1) * 128, :]), reads=XD_KEYS, writes=[("xdt", i)], sem=f"xdt{i}")

            def compute(b_):
                i = b_ % NBUF
                j = b_ % 2
                iw = b_ % 2
                bt = j
                psb = ps[bt][:].bitcast(BF16)
                for kc in range(8):
                    kb.op("pe", lambda e: e.transpose(out=psb[:, kc * 128:(kc + 1) * 128], in_=xdt[i][:, kc * 128:(kc + 1) * 128], identity=ident[:]),
                          reads=[("xdt", i), "c_ident_bf"], writes=[psk(bt)])
                evac(xdT[j][:], psb.rearrange("p (k t) -> p k t", k=8), reads=[psk(bt)], writes=[("xdT", j)])
                bg = 2 + j
                for slot, (wt, nm) in enumerate(((wg[iw], "wg"), (wg[iw], "wg"), (wu[iw], "wu"), (wu[iw], "wu"))):
                    n2 = slot % 2
                    for kc in range(8):
                        kb.op("pe", lambda e: e.matmul(ps[bg][:, slot * 128:(slot + 1) * 128], lhsT=wt[:, kc * 256 + n2 * 128:kc * 256 + (n2 + 1) * 128],
                                                       rhs=xdT[j][:, kc, :], start=(kc == 0), stop=(kc == 7)),
                              reads=[("wcomb", iw), ("xdT", j)], writes=[psk(bg)])
                kb.op("act", lambda e: e.activation(out=sg[j][:], in_=ps[bg][:, 0:256], func=AF.Silu), reads=[psk(bg)], writes=[("sg", j)])
                kb.op("dve", lambda e: e.tensor_tensor(out=actT[j][:], in0=ps[bg][:, 256:512], in1=sg[j][:], op=ALU.mult),
                      reads=[psk(bg), ("sg", j)], writes=[("actT", j)])
                for hf in range(2):
                    bd = 4 + (2 * b_ + hf) % 4
                    for n2 in range(2):
                        kb.op("pe", lambda e: e.matmul(ps[bd][:, 0:512], lhsT=actT[j][:, n2 * 128:(n2 + 1) * 128],
                                                       rhs=wd[iw][:, n2 * 1024 + hf * 512:n2 * 1024 + (hf + 1) * 512], start=(n2 == 0), stop=(n2 == 1)),
                              reads=[("actT", j), ("wcomb", iw)], writes=[psk(bd)])
                    evac(ydt[j][:, hf * 512:(hf + 1) * 512], ps[bd][:, 0:512], reads=[psk(bd)], writes=[("ydt", j, hf)])
                kb.dma("act", lambda e: e.dma_start(out=yd_d[b_ * 128:(b_ + 1) * 128, :], in_=ydt[j][:]),
                       reads=[("ydt", j, 0), ("ydt", j, 1)], writes=[("yd_d", b_ % 2)], sem=f"ydt{j}")

            prefetch_w(0)
            prefetch(0)
            if NB > 1:
                prefetch(1)
            for b_ in range(NB):
                if b_ + 1 < NB:
                    prefetch_w(b_ + 1)
                if b_ + 2 < NB:
                    prefetch(b_ + 2)
                compute(b_)

        def stage_G(desti, W12):
            NBUF = 3
            gf = kb.sbuf("gf", [128, 1024], F32)
            kb.dma("sp", lambda e: e.dma_start(out=gf[:], in_=bcast_rows(g_fin_d, 1024)), writes=["gf"], sem="gf")
            g0 = [kb.sbuf(f"g0_{i}", [128, 1024], F32) for i in range(NBUF)]
            g1 = [kb.sbuf(f"g1_{i}", [128, 1024], F32) for i in range(NBUF)]
            xx = [kb.sbuf(f"xx_{i}", [128, 1024], F32) for i in range(NBUF)]
            oo = [kb.sbuf(f"oo_{i}", [128, 1024], F32) for i in range(2)]
            rg = [kb.sbuf(f"rg_{i}", [128, 4], F32) for i in range(2)]

            def prefetch(tt):
                i = tt % NBUF
                for k, gt in ((0, g0[i]), (1, g1[i])):
                    kb.dma("pool", lambda e: e.indirect_dma_start(out=gt[:], out_offset=None, in_=yd_d,
                                                                  in_offset=bass.IndirectOffsetOnAxis(ap=desti[:, 2 * tt + k:2 * tt + k + 1], axis=0)),
                           reads=YD_KEYS + ["desti"], writes=[("gg", k, i)], sem=f"gg{k}{i}")
                kb.dma("sp", lambda e: e.dma_start(out=xx[i][:], in_=x1_d[tt * 128:(tt + 1) * 128, :]), reads=[("x1_d", tt)], writes=[("xx", i)], sem=f"xx{i}")

            def compute(tt):
                i = tt % NBUF
                j = tt % 2
                kb.op("dve", lambda e: e.scalar_tensor_tensor(out=xx[i][:], in0=g0[i][:], scalar=W12[:, tt, 0:1], in1=xx[i][:], op0=ALU.mult, op1=ALU.add),
                      reads=[("gg", 0, i), ("xx", i), ("W12", tt)], writes=[("xx", i)])
                kb.op("dve", lambda e: e.scalar_tensor_tensor(out=xx[i][:], in0=g1[i][:], scalar=W12[:, tt, 1:2], in1=xx[i][:], op0=ALU.mult, op1=ALU.add),
                      reads=[("gg", 1, i), ("xx", i), ("W12", tt)], writes=[("xx", i)])
                kb.op("act", lambda e: e.activation(out=junk[:], in_=xx[i][:], func=AF.Square, accum_out=rg[j][:, 0:1]), reads=[("xx", i)], writes=["junk", ("rg", j)])
                kb.op("act", lambda e: e.activation(out=rg[j][:, 1:2], in_=rg[j][:, 0:1], func=AF.Ln, bias=EPS, scale=1.0 / 1024), reads=[("rg", j)], writes=[("rg", j)])
                kb.op("act", lambda e: e.activation(out=rg[j][:, 2:3], in_=rg[j][:, 1:2], func=AF.Exp, scale=-0.5), reads=[("rg", j)], writes=[("rg", j)])
                kb.op("dve", lambda e: e.scalar_tensor_tensor(out=oo[j][:], in0=xx[i][:], scalar=rg[j][:, 2:3], in1=gf[:], op0=ALU.mult, op1=ALU.mult),
                      reads=[("xx", i), ("rg", j), "gf"], writes=[("oo", j)])
                kb.dma("act", lambda e: e.dma_start(out=out_d[tt * 128:(tt + 1) * 128, :], in_=oo[j][:]), reads=[("oo", j)], writes=[("out_d", tt)], sem=f"oo{j}")

            prefetch(0)
            if NT > 1:
                prefetch(1)
            for tt in range(NT):
                if tt + 2 < NT:
                    prefetch(tt + 2)
                compute(tt)

        with kb.scope():
            stage_A()
        zfill = kb.sbuf("zfill", [128, 2048], BF16)
        kb.op("pool", lambda e: e.memset(zfill[:], 0.0), writes=["zfill"])
        for c in range(NROWS // 256):
            bg_ops.append(lambda c=c: kb.dma(
                "sp", lambda e: e.dma_start(out=xd_d[c * 256:(c + 1) * 256, :].rearrange("(p j) d -> p (j d)", j=2), in_=zfill[:]),
                reads=["zfill"], writes=[("xd_d", c % 4)], sem="zf"))
        for k_, (nm, src) in enumerate((("wg", wg_d), ("wu", wu_d), ("wd", wd_d))):
            for c in range(16):
                bg_ops.append(lambda k_=k_, src=src, c=c: kb.dma(
                    "pool", lambda e: e.dma_start(out=wall_d[c * 256:(c + 1) * 256, k_ * 2048:(k_ + 1) * 2048], in_=src[c * 256:(c + 1) * 256, :]),
                    writes=[("wall", (k_ * 16 + c) % 4)], sem=f"pc{(k_ * 16 + c) % 4}"))
        with kb.scope():
            stage_B1()
        with kb.scope():
            stage_B2()
        if dbg:
            for nm, T, keys, nch in (("dbg_oa", OaT, [("OaT", h, q) for h in range(8) for q in range(NQ)], 4),
                                     ("dbg_ob", ObT, [("ObT", h, q) for h in range(4) for q in range(NQ)], 4)):
                if nm[4:] in dbg:
                    o = dout(nm, [128, nch, S])
                    tmp = kb.sbuf(nm + "_t", [128, nch, S], F32)
                    kb.op("dve", lambda e: e.tensor_copy(out=tmp[:], in_=T[:]), reads=keys, writes=[nm])
                    kb.dma("sp", lambda e: e.dma_start(out=o, in_=tmp[:]), reads=[nm], writes=[nm + "_d"], sem=nm)
        while bg_ops:
            bg_ops.pop(0)()
        PB[0] = 4
        with kb.scope():
            stage_C()
        st_main.close()
        with contextlib.ExitStack() as st2:
            kb.stack = st2
            h2all = kb.sbuf("h2all", [128, NT, 1024], BF16)
            M32 = kb.sbuf("M32", [128, NT, 32], F32)
            OH = kb.sbuf("OH", [128, NT, 2, 32], F32)
            W12 = kb.sbuf("W12", [128, NT, 2], F32)
            desti = kb.sbuf("desti", [128, 2 * NT], I32)
            widx = kb.sbuf("widx", [128, NB], I32)
            with kb.scope():
                stage_D(h2all, M32, OH, W12)
            with kb.scope():
                stage_E(h2all, M32, OH, W12, desti, widx)
            with kb.scope():
                stage_F(widx)
            with kb.scope():
                stage_G(desti, W12)
        print("build: instr", kb.cnt, "waits", kb.n_wait, "sems", len(kb.sems))
    return nc


def _core_inputs(inputs, b, S, shared):
    d = dict(shared)
    d["x"] = np.ascontiguousarray(inputs["x"][b, :S], dtype=np.float32)
    return d


def _shared_inputs(inputs, S):
    f = lambda a: np.ascontiguousarray(np.asarray(a, dtype=np.float32))
    sh = {
        "w_in": f(inputs["w_in"][0]),
        "g_norm_mix": f(inputs["g_norm_mix"]).reshape(1, 1024),
        "g_subln": f(inputs["g_subln"]).reshape(1, 128),
        "w_up_a": f(inputs["w_up_a"][0]), "w_up_b": f(inputs["w_up_b"][0]), "w_out": f(inputs["w_out"][0]),
        "g_norm_ffn": f(inputs["g_norm_ffn"]).reshape(1, 1024),
        "w_router_group": f(inputs["w_router_group"][0]), "w_router_expert": f(inputs["w_router_expert"][0]),
        "b_router_group": f(inputs["b_router_group"]).reshape(1, 4), "b_router_expert": f(inputs["b_router_expert"]).reshape(1, 32),
        "g_norm_final": f(inputs["g_norm_final"]).reshape(1, 1024),
    }
    for k in ("lambda_q1", "lambda_k1", "lambda_q2", "lambda_k2"):
        sh[k] = f(inputs[k]).reshape(1, 64)
    wg = np.asarray(inputs["w_expert_gate"][0], dtype=np.float32)
    wu = np.asarray(inputs["w_expert_up"][0], dtype=np.float32)
    wd = np.asarray(inputs["w_expert_down"][0], dtype=np.float32)
    sh["wg_l"] = np.ascontiguousarray(wg.reshape(32, 8, 128, 256).transpose(0, 2, 1, 3)).reshape(4096, 2048)
    sh["wu_l"] = np.ascontiguousarray(wu.reshape(32, 8, 128, 256).transpose(0, 2, 1, 3)).reshape(4096, 2048)
    sh["wd_l"] = np.ascontiguousarray(wd.reshape(32, 2, 128, 1024).transpose(0, 2, 1, 3)).reshape(4096, 2048)
    sh.update(host_consts(S))
    return sh


_NC_CACHE = {}


def kernel(**inputs):
    S = SEQ
    B = inputs["x"].shape[0]
    if S not in _NC_CACHE:
        _NC_CACHE[S] = build(S)
    nc = _NC_CACHE[S]
    shared = _shared_inputs(inputs, S)
    in_maps = [_core_inputs(inputs, b, S, shared) for b in range(B)]
    res = run_bass_kernel_spmd(nc, in_maps, core_ids=list(range(B)))
    return np.stack([np.asarray(r["out"], dtype=np.float32) for r in res.results], axis=0)
```

```python
import contextlib
import numpy as np
import ml_dtypes
import concourse.bass as bass
import concourse.mybir as mybir
from concourse.bass_utils import run_bass_kernel_spmd

F32 = mybir.dt.float32
BF16 = mybir.dt.bfloat16
I32 = mybir.dt.int32
U32 = mybir.dt.uint32
AF = mybir.ActivationFunctionType
ALU = mybir.AluOpType
AX = mybir.AxisListType

D_MODEL = 1024
SEQ = 4096
IN_WIDTH = 5120
N_EXPERTS = 32
D_EXPERT = 256
EPS = 1e-6
EPOCH = 12000


class KB:
    def __init__(self, nc, stack):
        self.nc = nc
        self.stack = stack
        self.sem_stack = stack
        self.bar_tile = stack.enter_context(nc.sbuf_tensor("bar_tile", [128, 8], F32))
        self.eng = {"pe": nc.tensor, "dve": nc.vector, "act": nc.scalar,
                    "pool": nc.gpsimd, "sp": nc.sync}
        self.cnt = {e: 0 for e in self.eng}
        self.sems = {}
        self.waited = {e: {} for e in self.eng}
        self.last_w = {}
        self.readers = {}
        self.dma_cnt = {}
        self.dma_latest = {}
        self.n_wait = 0

    def sem(self, key):
        s = self.sems.get(key)
        if s is None:
            name = "s_" + "_".join(str(k) for k in key)
            s = self.sem_stack.enter_context(self.nc.semaphore(name))
            self.sems[key] = s
        return s

    def sbuf(self, name, shape, dt):
        return self.stack.enter_context(self.nc.sbuf_tensor(name, list(shape), dt))

    def psum(self, name, shape, dt):
        return self.stack.enter_context(self.nc.psum_tensor(name, list(shape), dt))

    def _deps(self, reads, writes, e=None):
        deps = {}

        def add(st):
            sk, v = st
            if sk[0] == "dma":
                v = max(v, self.dma_latest.get(sk, 0))
            if deps.get(sk, 0) < v:
                deps[sk] = v
        for k in reads:
            lw = self.last_w.get(k)
            if lw is not None:
                add(lw)
            if isinstance(k, tuple) and k and k[0] == "ps":
                for sk, v in self.readers.get(k, {}).items():
                    if e is None or sk[0] != e:
                        add((sk, v))
        for k in writes:
            lw = self.last_w.get(k)
            if lw is not None:
                add(lw)
            for sk, v in self.readers.get(k, {}).items():
                add((sk, v))
        return deps

    def _emit_waits(self, e, deps):
        for sk, v in deps.items():
            if sk[0] == "pe" and e == "pe":
                continue
            if self.waited[e].get(sk, 0) >= v:
                continue
            self.eng[e].wait_ge(self.sem(sk), v)
            self.waited[e][sk] = v
            self.n_wait += 1

    def _stamp(self, stamp, reads, writes):
        sk, v = stamp
        for k in reads:
            self.readers.setdefault(k, {})[sk] = v
        for k in writes:
            self.last_w[k] = stamp
            self.readers[k] = {}

    def op(self, e, fn, reads=(), writes=()):
        self._emit_waits(e, self._deps(reads, writes, e))
        inst = fn(self.eng[e])
        n = self.cnt[e]
        self.cnt[e] = n + 1
        sk = (e, n // EPOCH)
        v = (n % EPOCH) + 1
        inst.then_inc(self.sem(sk), 1)
        self._stamp((sk, v), reads, writes)
        return inst

    def dma(self, q, fn, reads=(), writes=(), sem=None):
        self._emit_waits(q, self._deps(reads, writes))
        inst = fn(self.eng[q])
        n = self.dma_cnt.get(sem, 0)
        self.dma_cnt[sem] = n + 1
        per = EPOCH // 16
        sk = ("dma", sem, n // per)
        v = ((n % per) + 1) * 16
        inst.then_inc(self.sem(sk), 16)
        self.dma_latest[sk] = v
        self._stamp((sk, v), reads, writes)
        return inst

    def barrier(self):
        if self.bar_tile is None:
            self.bar_tile = self.sem_stack.enter_context(self.nc.sbuf_tensor("bar_tile", [128, 8], F32))
        deps = {}
        for e, n in self.cnt.items():
            if n:
                deps[(e, (n - 1) // EPOCH)] = ((n - 1) % EPOCH) + 1
        for sk, v in self.dma_latest.items():
            deps[sk] = v
        self._emit_waits("pool", deps)
        self.op("pool", lambda e: e.memset(self.bar_tile[:], 0.0))
        n = self.cnt["pool"]
        st = {("pool", (n - 1) // EPOCH): ((n - 1) % EPOCH) + 1}
        for e in self.eng:
            if e != "pool":
                self._emit_waits(e, dict(st))

    @contextlib.contextmanager
    def scope(self):
        old = self.stack
        with contextlib.ExitStack() as sub:
            self.stack = sub
            try:
                yield
            finally:
                self.stack = old
            self.barrier()

    def wait_all(self, e, keys):
        self._emit_waits(e, self._deps(keys, keys))


def host_consts(S):
    bf = ml_dtypes.bfloat16
    j = np.arange(128)[:, None]
    s = np.arange(128)[None, :]
    c = {}
    c["c_ident_bf"] = np.eye(128, dtype=np.float32).astype(bf)
    c["c_ident_f"] = np.eye(128, dtype=np.float32)
    c["c_tri_m8"] = np.where(j >= s, -8.0, 0.0).astype(bf)
    c["c_ones_m8"] = np.full((128, 128), -8.0, np.float32).astype(bf)
    c["c_mask_sb"] = np.where(j >= s, -10000.0, 0.0).astype(bf)
    c["c_mask_df"] = np.where(j > s, -10000.0, 0.0).astype(bf)
    c["c_m01_sb"] = np.where(j < s, 1.0, 0.0).astype(bf)
    c["c_m01_df"] = np.where(j <= s, 1.0, 0.0).astype(bf)
    src = np.where((np.arange(128) % 64) < 32, np.arange(128) + 32, np.arange(128) - 32)
    perm = np.zeros((128, 128), np.float32)
    perm[src, np.arange(128)] = 1.0
    c["c_perm"] = perm.astype(bf)
    c["c_zeros"] = np.zeros((128, 512), np.float32).astype(bf)
    c["c_tri_f"] = np.where(j < s, 1.0, 0.0).astype(np.float32)
    c["c_ones_f"] = np.ones((128, 128), np.float32)
    pos = np.arange(S, dtype=np.float32)
    inv_freq = (1.0 / (np.float32(10000.0) ** (np.arange(0, 64, 2, dtype=np.float32) / np.float32(64)))).astype(np.float32)
    fr = (pos[None, :] * inv_freq[:, None]).astype(np.float32)
    cos = np.cos(fr).astype(np.float32)
    sin = np.sin(fr).astype(np.float32)
    cos64 = np.concatenate([cos, cos], 0)
    sin64 = np.concatenate([-sin, sin], 0)
    c["c_cos"] = np.ascontiguousarray(np.concatenate([cos64, cos64], 0))
    c["c_sin"] = np.ascontiguousarray(np.concatenate([sin64, sin64], 0))
    return c


CONST_SPECS = {
    "c_ident_bf": ([128, 128], BF16), "c_ident_f": ([128, 128], F32),
    "c_tri_m8": ([128, 128], BF16), "c_ones_m8": ([128, 128], BF16),
    "c_mask_sb": ([128, 128], BF16), "c_mask_df": ([128, 128], BF16),
    "c_m01_sb": ([128, 128], BF16), "c_m01_df": ([128, 128], BF16), "c_perm": ([128, 128], BF16),
    "c_zeros": ([128, 512], BF16), "c_tri_f": ([128, 128], F32), "c_ones_f": ([128, 128], F32),
}


def bcast_rows(ap2d, n):
    return bass.AP(ap2d.tensor, ap2d.offset, [[0, 128], [1, n]])


def build(S, stages="AB", dbg=()):
    NT = S // 128
    NQ = S // 512
    nc = bass.Bass("TRN2", target_bir_lowering=False)

    def din(name, shape, dt=F32):
        return nc.dram_tensor(name, list(shape), dt, kind="ExternalInput").ap()

    def dout(name, shape, dt=F32):
        return nc.dram_tensor(name, list(shape), dt, kind="ExternalOutput").ap()

    x_d = din("x", [S, 1024])
    w_in_d = din("w_in", [1024, IN_WIDTH])
    g_mix_d = din("g_norm_mix", [1, 1024])
    lam_d = {k: din(k, [1, 64]) for k in ("lambda_q1", "lambda_k1", "lambda_q2", "lambda_k2")}
    g_sub_d = din("g_subln", [1, 128])
    cd = {k: din(k, sh, dt) for k, (sh, dt) in CONST_SPECS.items()}
    cos_d = din("c_cos", [128, S])
    sin_d = din("c_sin", [128, S])
    NB = 2 * NT + 32
    NROWS = NB * 128
    w_up_a_d = din("w_up_a", [512, 1024])
    w_up_b_d = din("w_up_b", [512, 1024])
    w_out_d = din("w_out", [1024, 1024])
    g_ffn_d = din("g_norm_ffn", [1, 1024])
    w_rg_d = din("w_router_group", [1024, 4])
    w_re_d = din("w_router_expert", [1024, 32])
    b_rg_d = din("b_router_group", [1, 4])
    b_re_d = din("b_router_expert", [1, 32])
    wg_d = din("wg_l", [4096, 2048])
    wu_d = din("wu_l", [4096, 2048])
    wd_d = din("wd_l", [4096, 2048])
    g_fin_d = din("g_norm_final", [1, 1024])
    out_d = dout("out", [S, 1024])
    yT_d = nc.dram_tensor("yT_scr", [8, 128, S], BF16).ap()
    x1_d = nc.dram_tensor("x1_scr", [S, 1024], F32).ap()
    xd_d = nc.dram_tensor("xd_scr", [NROWS, 1024], BF16).ap()
    yd_d = nc.dram_tensor("yd_scr", [NROWS, 1024], F32).ap()
    wall_d = nc.dram_tensor("wall_bf", [4096, 6144], BF16).ap()
    outs = {}
    XD_KEYS = [("xd_d", i) for i in range(4)]
    YD_KEYS = [("yd_d", i) for i in range(2)]
    WALL_KEYS = [("wall", i) for i in range(4)]

    with contextlib.ExitStack() as st:
        kb = KB(nc, st)
        psall = kb.psum("psall", [128, 8, 512], F32)
        ps = [psall[:, i, :] for i in range(8)]
        psk = lambda b: ("ps", b)

        csb = {}
        for k, (sh, dt) in CONST_SPECS.items():
            t = kb.sbuf("sb_" + k, sh, dt)
            kb.dma("sp", lambda e, t=t, k=k: e.dma_start(out=t[:], in_=cd[k]), writes=[k], sem=k)
            csb[k] = t
        ident = csb["c_ident_bf"]
        zeros = csb["c_zeros"]
        junk = kb.sbuf("junk", [128, 1024], BF16)
        st_main = contextlib.ExitStack()
        kb.stack = st_main
        hT = kb.sbuf("hT", [128, 8, S], BF16)
        OaT = kb.sbuf("OaT", [128, 4, S], BF16)
        ObT = kb.sbuf("ObT", [128, 4, S], BF16)

        def stage_A():
            gmix = kb.sbuf("gmix", [128, 1024], F32)
            kb.dma("sp", lambda e: e.dma_start(out=gmix[:], in_=bcast_rows(g_mix_d, 1024)), writes=["gmix"], sem="gmix")
            NA = 4
            xt = [kb.sbuf(f"xt{i}", [128, 1024], F32) for i in range(NA)]
            hb = [kb.sbuf(f"hb{i}", [128, 1024], BF16) for i in range(NA)]
            stA = [kb.sbuf(f"stA{i}", [128, 4], F32) for i in range(NA)]

            def a_tile(tt, i):
                kb.dma("sp", lambda e: e.dma_start(out=xt[i][:], in_=x_d[tt * 128:(tt + 1) * 128, :]),
                       writes=[("xt", i)], sem=f"xt{i}")
                yield
                kb.op("act", lambda e: e.activation(out=junk[:], in_=xt[i][:], func=AF.Square, accum_out=stA[i][:, 0:1]),
                      reads=[("xt", i)], writes=["junk", ("stA", i)])
                yield
                kb.op("act", lambda e: e.activation(out=stA[i][:, 1:2], in_=stA[i][:, 0:1], func=AF.Ln, bias=EPS, scale=1.0 / 1024),
                      reads=[("stA", i)], writes=[("stA", i)])
                yield
                kb.op("act", lambda e: e.activation(out=stA[i][:, 2:3], in_=stA[i][:, 1:2], func=AF.Exp, scale=-0.5),
                      reads=[("stA", i)], writes=[("stA", i)])
                yield
                kb.op("dve", lambda e: e.scalar_tensor_tensor(out=hb[i][:], in0=xt[i][:], scalar=stA[i][:, 2:3], in1=gmix[:],
                                                              op0=ALU.mult, op1=ALU.mult),
                      reads=[("xt", i), ("stA", i), "gmix"], writes=[("hb", i)])
                yield
                b = i
                psb = ps[b][:].bitcast(BF16)
                for kc in range(8):
                    kb.op("pe", lambda e: e.transpose(out=psb[:, kc * 128:(kc + 1) * 128], in_=hb[i][:, kc * 128:(kc + 1) * 128],
                                                      identity=ident[:]),
                          reads=[("hb", i), "c_ident_bf"], writes=[psk(b)])
                yield
                kb.op("dve", lambda e: e.tensor_copy(out=hT[:, :, tt * 128:(tt + 1) * 128], in_=psb.rearrange("p (k t) -> p k t", k=8)),
                      reads=[psk(b)], writes=[("hT", tt)])
                yield

            run_streams([(lambda sl, tt=tt: a_tile(tt, sl)) for tt in range(NT)], NA, bg_every=10 ** 9)

        hT_keys = lambda tq: [("hT", t) for t in range(4 * tq, 4 * tq + 4)]
        pcount = [0]
        PB = [4]

        def proj_fm(wt, wkey, tq, M=128):
            b = pcount[0] % PB[0]
            pcount[0] += 1
            for kc in range(8):
                kb.op("pe", lambda e: e.matmul(ps[b][0:M, 0:512], lhsT=wt[:, kc, 0:M], rhs=hT[:, kc, tq * 512:(tq + 1) * 512],
                                               start=(kc == 0), stop=(kc == 7)),
                      reads=[wkey] + hT_keys(tq), writes=[psk(b)])
            return b

        def load_w(wt, wkey, col0, ncols=128, q="pool"):
            kb.dma(q, lambda e: e.dma_start(out=wt[:, :, 0:ncols],
                                            in_=w_in_d[:, col0:col0 + ncols].rearrange("(kc p) c -> p kc c", p=128)),
                   writes=[wkey], sem=str(wkey))

        ccount = [0]

        def evac(out_ap, in_ap, reads, writes):
            if ccount[0] % 2 == 0:
                kb.op("act", lambda e: e.activation(out=out_ap, in_=in_ap, func=AF.Copy), reads=reads, writes=writes)
            else:
                kb.op("dve", lambda e: e.tensor_copy(out=out_ap, in_=in_ap), reads=reads, writes=writes)
            ccount[0] += 1

        qT = kb.sbuf("qT", [128, S], BF16)
        kT = kb.sbuf("kT", [128, S], BF16)
        V = kb.sbuf("V", [128, NT, 132], BF16)
        wq = [kb.sbuf(f"wq{i}", [128, 8, 128], BF16) for i in range(1)]
        wk = [kb.sbuf(f"wk{i}", [128, 8, 128], BF16) for i in range(1)]
        wv = [kb.sbuf(f"wv{i}", [128, 8, 128], BF16) for i in range(1)]

        def proj_v(wt, wkey):
            for t4 in range(NT // 4):
                b = pcount[0] % PB[0]
                pcount[0] += 1
                for j in range(4):
                    tt = t4 * 4 + j
                    for kc in range(8):
                        kb.op("pe", lambda e: e.matmul(ps[b][:, j * 128:(j + 1) * 128], lhsT=hT[:, kc, tt * 128:(tt + 1) * 128],
                                                       rhs=wt[:, kc, :], start=(kc == 0), stop=(kc == 7)),
                              reads=[wkey, ("hT", tt)], writes=[psk(b)])
                evac(V[:, t4 * 4:(t4 + 1) * 4, 0:128], ps[b][:, 0:512].rearrange("p (j d) -> p j d", j=4),
                     reads=[psk(b)], writes=[("V", t4)])

        bg_ops = []

        def run_streams(makers, ns, bg_every=12):
            pending = list(makers)
            active = {}
            rounds = 0
            while pending or active:
                rounds += 1
                if bg_ops and rounds % bg_every == 0:
                    bg_ops.pop(0)()
                for sl in range(ns):
                    if sl not in active and pending:
                        active[sl] = pending.pop(0)(sl)
                    g = active.get(sl)
                    if g is not None:
                        try:
                            next(g)
                        except StopIteration:
                            del active[sl]

        def stage_B1():
            NS = 4
            PB[0] = 8
            m01 = csb["c_m01_sb"]
            e32 = [kb.sbuf(f"e32_{i}", [128, 512], BF16) for i in range(NS)]
            Pb = [kb.sbuf(f"Pb{i}", [128, 512], BF16) for i in range(NS)]
            Ab = [kb.sbuf(f"Ab{i}", [128, 512], BF16) for i in range(NS)]
            R32 = [kb.sbuf(f"R32_{i}", [128, 512], F32) for i in range(NS)]
            Rb = [kb.sbuf(f"Rb{i}", [128, 512], BF16) for i in range(NS)]
            tri = csb["c_tri_m8"]
            onesm = csb["c_ones_m8"]
            msb = csb["c_mask_sb"]

            def sb_stream(hp, par, qt, sl):
                pb = 64 * par
                bs = sl
                bo = 4 + sl
                kb.op("pe", lambda e: e.matmul(ps[bo][pb:pb + 64, 0:512], lhsT=zeros[:, 0:64], rhs=zeros[:, 0:512],
                                               start=True, stop=False),
                      reads=["c_zeros"], writes=[psk(bo)])
                kb.op("pool", lambda e: e.memset(R32[sl][:], 0.0), writes=[("R32", sl)])
                kb.op("pool", lambda e: e.memset(Rb[sl][:], 0.0), writes=[("Rb", sl)])
                first = True
                for kbk in range(4 * qt + 3, -1, -1):
                    i = kbk - 4 * qt
                    diag = i >= 0
                    c0 = max(0, i) * 128
                    kb.op("pe", lambda e: e.matmul(ps[bs][:, c0:512], lhsT=kT[pb:pb + 64, kbk * 128:(kbk + 1) * 128],
                                                   rhs=qT[pb:pb + 64, qt * 512 + c0:(qt + 1) * 512], start=True, stop=True),
                          reads=[("kT", kbk // 4), ("qT", qt)], writes=[psk(bs)])
                    yield
                    kb.op("act", lambda e: e.activation(out=e32[sl][:, c0:512], in_=ps[bs][:, c0:512], func=AF.Exp, scale=0.125),
                          reads=[psk(bs)], writes=[("e32", sl)])
                    yield
                    kb.op("act", lambda e: e.activation(out=Pb[sl][:, c0:512], in_=e32[sl][:, c0:512], func=AF.Ln, bias=1.0),
                          reads=[("e32", sl)], writes=[("Pb", sl)])
                    if diag:
                        kb.op("dve", lambda e: e.tensor_tensor(out=Pb[sl][:, c0:c0 + 128], in0=Pb[sl][:, c0:c0 + 128], in1=m01[:], op=ALU.mult),
                              reads=[("Pb", sl), "c_m01_sb"], writes=[("Pb", sl)])
                    yield
                    kb.op("pe", lambda e: e.matmul(ps[bs][:, c0:512], lhsT=tri[:], rhs=Pb[sl][:, c0:512], start=False, stop=True,
                                                   skip_group_check=True),
                          reads=[("Pb", sl), "c_tri_m8"], writes=[psk(bs)])
                    if not first:
                        kb.op("pe", lambda e: e.matmul(ps[bs][:, c0:512], lhsT=onesm[:], rhs=Rb[sl][:, c0:512], start=False, stop=True,
                                                       skip_group_check=True),
                              reads=[("Rb", sl), "c_ones_m8"], writes=[psk(bs)])
                    yield
                    kb.op("act", lambda e: e.activation(out=Ab[sl][:, c0:512], in_=ps[bs][:, c0:512], func=AF.Exp, scale=0.125),
                          reads=[psk(bs)], writes=[("Ab", sl)])
                    if diag:
                        kb.op("dve", lambda e: e.tensor_tensor(out=Ab[sl][:, c0:c0 + 128], in0=Ab[sl][:, c0:c0 + 128], in1=m01[:], op=ALU.mult),
                              reads=[("Ab", sl), "c_m01_sb"], writes=[("Ab", sl)])
                    if kbk > 0:
                        kb.op("pool", lambda e: e.tensor_tensor(out=R32[sl][:, c0:512], in0=R32[sl][:, c0:512], in1=Pb[sl][:, c0:512], op=ALU.add),
                              reads=[("R32", sl), ("Pb", sl)], writes=[("R32", sl)])
                        kb.op("dve", lambda e: e.tensor_copy(out=Rb[sl][:, c0:512], in_=R32[sl][:, c0:512]), reads=[("R32", sl)], writes=[("Rb", sl)])
                    yield
                    kb.op("pe", lambda e: e.matmul(ps[bo][pb:pb + 64, c0:512], lhsT=V[:, kbk, pb:pb + 64], rhs=Ab[sl][:, c0:512],
                                                   start=False, stop=(kbk == 0)),
                          reads=[("V", kbk // 4), ("Ab", sl)], writes=[psk(bo)])
                    first = False
                kb.op("dve", lambda e: e.tensor_copy(out=OaT[pb:pb + 64, hp, qt * 512:(qt + 1) * 512], in_=ps[bo][pb:pb + 64, 0:512]),
                      reads=[psk(bo)], writes=[("OaT", 2 * hp + par, qt)])
                yield

            def lw_b1(hp):
                load_w(wq[0], ("wq", 0), hp * 128)
                load_w(wk[0], ("wk", 0), 512 + hp * 128)
                load_w(wv[0], ("wv", 0), 1024 + hp * 128)
            lw_b1(0)
            for hp in range(4):
                w = 0
                for tq in range(NQ):
                    b = proj_fm(wq[w], ("wq", w), tq)
                    evac(qT[:, tq * 512:(tq + 1) * 512], ps[b][:, 0:512], reads=[psk(b)], writes=[("qT", tq)])
                    b = proj_fm(wk[w], ("wk", w), tq)
                    evac(kT[:, tq * 512:(tq + 1) * 512], ps[b][:, 0:512], reads=[psk(b)], writes=[("kT", tq)])
                proj_v(wv[w], ("wv", w))
                if hp + 1 < 4:
                    lw_b1(hp + 1)
                makers = []
                for qt in range(NQ - 1, -1, -1):
                    for par in range(2):
                        makers.append(lambda sl, par=par, qt=qt: sb_stream(hp, par, qt, sl))
                run_streams(makers, NS)
            PB[0] = 4

        def stage_B2():
            qraws = [kb.sbuf(f"qraw{i}", [128, 512], BF16) for i in range(2)]
            PB[0] = 8
            rcnt = [0]
            perm = csb["c_perm"]
            lamt = kb.sbuf("lamt", [128, 4, 64], F32)
            lams = kb.sbuf("lams", [128, 8], F32)
            for n, k in enumerate(("lambda_q1", "lambda_k1", "lambda_q2", "lambda_k2")):
                kb.dma("sp", lambda e: e.dma_start(out=lamt[:, n, :], in_=bcast_rows(lam_d[k], 64)), writes=["lamt"], sem="lamt")
            kb.op("dve", lambda e: e.tensor_tensor(out=lamt[:, 0, :], in0=lamt[:, 0, :], in1=lamt[:, 1, :], op=ALU.mult),
                  reads=["lamt"], writes=["lamt"])
            kb.op("dve", lambda e: e.tensor_tensor(out=lamt[:, 2, :], in0=lamt[:, 2, :], in1=lamt[:, 3, :], op=ALU.mult),
                  reads=["lamt"], writes=["lamt"])
            kb.op("dve", lambda e: e.reduce_sum(out=lams[:, 0:1], in_=lamt[:, 0, :], axis=AX.X), reads=["lamt"], writes=["lams"])
            kb.op("dve", lambda e: e.reduce_sum(out=lams[:, 1:2], in_=lamt[:, 2, :], axis=AX.X), reads=["lamt"], writes=["lams"])
            kb.op("act", lambda e: e.activation(out=lams[:, 2:4], in_=lams[:, 0:2], func=AF.Exp), reads=["lams"], writes=["lams"])
            kb.op("dve", lambda e: e.tensor_tensor(out=lams[:, 4:5], in0=lams[:, 3:4], in1=lams[:, 2:3], op=ALU.subtract),
                  reads=["lams"], writes=["lams"])
            kb.op("dve", lambda e: e.tensor_scalar(out=lams[:, 5:6], in0=lams[:, 4:5], scalar1=-0.2, scalar2=None, op0=ALU.add),
                  reads=["lams"], writes=["lams"])
            neglam = lams[:, 5:6]
            gsub = kb.sbuf("gsub", [128, 128], F32)
            kb.dma("sp", lambda e: e.dma_start(out=gsub[:], in_=bcast_rows(g_sub_d, 128)), writes=["gsub"], sem="gsub")
            kb.op("dve", lambda e: e.tensor_scalar(out=gsub[:], in0=gsub[:], scalar1=0.8, scalar2=None, op0=ALU.mult),
                  reads=["gsub"], writes=["gsub"])
            kb.op("pool", lambda e: e.memset(V[:, :, 128:129], 1.0), writes=["Vones"])
            cs = [kb.sbuf(f"cos{i}", [128, 512], F32) for i in range(2)]
            sn = [kb.sbuf(f"sin{i}", [128, 512], F32) for i in range(2)]
            r1 = [kb.sbuf(f"rt1_{i}", [128, 512], F32) for i in range(2)]
            r2 = [kb.sbuf(f"rt2_{i}", [128, 512], F32) for i in range(2)]
            o32 = [kb.sbuf(f"o32_{i}", [128, 128], F32) for i in range(2)]
            t32 = [kb.sbuf(f"t32_{i}", [128, 128], F32) for i in range(2)]
            onb = [kb.sbuf(f"onb_{i}", [128, 128], BF16) for i in range(2)]
            rr = [kb.sbuf(f"rr_{i}", [128, 8], F32) for i in range(2)]
            mdf = csb["c_mask_df"]
            m01d = csb["c_m01_df"]
            m01db = bass.AP(m01d, 0, [[128, 128], [0, 2], [1, 128]])
            Pd = [kb.sbuf(f"Pd{i}", [128, 2, 512], BF16) for i in range(2)]
            rc = 0
            step = 0
            fc = 0
            def lw_b2(dh):
                w = 0
                cq = 1536 + dh * 128
                ck = 2048 + dh * 128
                cv = 2560 + dh * 128
                load_w(wq[w], ("wq", w), cq)
                load_w(wk[w], ("wk", w), ck)
                load_w(wv[w], ("wv", w), cv)
            accS = [kb.sbuf(f"accS{i}", [128, 3 * 396], F32) for i in range(2)]
            afc = [0]
            fin_pending = []
            fcc = [0]

            def drain(g):
                for _ in g:
                    pass

            def step_bg():
                if fin_pending:
                    try:
                        next(fin_pending[0])
                    except StopIteration:
                        fin_pending.pop(0)

            def finalize(dh, qt, af):
                psb7 = ps[7][:].bitcast(BF16)
                A_ = accS[af]
                for j in range(4):
                    f = fcc[0] % 2
                    fcc[0] += 1
                    a1 = j * 2
                    a2 = j * 2 + 1
                    c1 = (a1 // 3) * 396 + (a1 % 3) * 132
                    c2 = (a2 // 3) * 396 + (a2 % 3) * 132
                    k1 = ("accS", af, a1 // 3)
                    k2 = ("accS", af, a2 // 3)
                    kb.op("dve", lambda e: e.reciprocal(out=rr[f][:, 0:1], in_=A_[:, c1 + 128:c1 + 129]), reads=[k1], writes=[("rr", f)])
                    kb.op("dve", lambda e: e.reciprocal(out=rr[f][:, 1:2], in_=A_[:, c2 + 128:c2 + 129]), reads=[k2], writes=[("rr", f)])
                    kb.op("dve", lambda e: e.tensor_tensor(out=rr[f][:, 2:3], in0=rr[f][:, 1:2], in1=neglam, op=ALU.mult),
                          reads=[("rr", f), "lams"], writes=[("rr", f)])
                    yield
                    kb.op("dve", lambda e: e.tensor_scalar(out=t32[f][:], in0=A_[:, c2:c2 + 128], scalar1=rr[f][:, 2:3], scalar2=None, op0=ALU.mult),
                          reads=[k2, ("rr", f)], writes=[("t32", f)])
                    kb.op("dve", lambda e: e.scalar_tensor_tensor(out=o32[f][:], in0=A_[:, c1:c1 + 128], scalar=rr[f][:, 0:1], in1=t32[f][:],
                                                                  op0=ALU.mult, op1=ALU.add),
                          reads=[k1, ("rr", f), ("t32", f)], writes=[("o32", f)])
                    yield
                    kb.op("act", lambda e: e.activation(out=junk[:, 0:128], in_=o32[f][:], func=AF.Square, accum_out=rr[f][:, 3:4]),
                          reads=[("o32", f)], writes=["junk", ("rr", f)])
                    kb.op("act", lambda e: e.activation(out=rr[f][:, 4:5], in_=rr[f][:, 3:4], func=AF.Ln, bias=EPS, scale=1.0 / 128),
                          reads=[("rr", f)], writes=[("rr", f)])
                    kb.op("act", lambda e: e.activation(out=rr[f][:, 5:6], in_=rr[f][:, 4:5], func=AF.Exp, scale=-0.5),
                          reads=[("rr", f)], writes=[("rr", f)])
                    yield
                    kb.op("dve", lambda e: e.scalar_tensor_tensor(out=onb[f][:], in0=o32[f][:], scalar=rr[f][:, 5:6], in1=gsub[:],
                                                                  op0=ALU.mult, op1=ALU.mult),
                          reads=[("o32", f), ("rr", f), "gsub"], writes=[("onb", f)])
                    yield
                    kb.op("pe", lambda e: e.transpose(out=psb7[:, j * 128:(j + 1) * 128], in_=onb[f][:], identity=ident[:]),
                          reads=[("onb", f), "c_ident_bf"], writes=[psk(7)])
                    yield
                kb.op("act", lambda e: e.activation(out=ObT[:, dh, qt * 512:(qt + 1) * 512], in_=psb7[:, 0:512], func=AF.Copy),
                      reads=[psk(7)], writes=[("ObT", dh, qt)])

            lw_b2(0)
            for dh in range(4):
                w = 0
                pend_rope = []

                def rope_tail(tq, ci, ri, ba, dstT, dk_):
                    qraw = qraws[ri]
                    bb = pcount[0] % PB[0]
                    pcount[0] += 1
                    kb.op("pe", lambda e: e.matmul(ps[bb][:, 0:512], lhsT=perm[:], rhs=qraw[:], start=True, stop=True),
                          reads=[("qraw", ri), "c_perm"], writes=[psk(bb)])
                    kb.op("dve", lambda e: e.tensor_tensor(out=r1[ri][:], in0=ps[ba][:, 0:512], in1=cs[ci][:], op=ALU.mult),
                          reads=[psk(ba), ("cos", ci)], writes=[("r1", ri)])
                    kb.op("dve", lambda e: e.tensor_tensor(out=r2[ri][:], in0=ps[bb][:, 0:512], in1=sn[ci][:], op=ALU.mult),
                          reads=[psk(bb), ("sin", ci)], writes=[("r2", ri)])
                    kb.op("pool", lambda e: e.tensor_tensor(out=dstT[:, tq * 512:(tq + 1) * 512], in0=r1[ri][:], in1=r2[ri][:], op=ALU.add),
                          reads=[("r1", ri), ("r2", ri)], writes=[(dk_, tq)])

                for tq in range(NQ):
                    ci = tq % 2
                    kb.dma("sp", lambda e: e.dma_start(out=cs[ci][:], in_=cos_d[:, tq * 512:(tq + 1) * 512]), writes=[("cos", ci)], sem=f"cos{ci}")
                    kb.dma("sp", lambda e: e.dma_start(out=sn[ci][:], in_=sin_d[:, tq * 512:(tq + 1) * 512]), writes=[("sin", ci)], sem=f"sin{ci}")
                    for (wa, wak, dstT, dk_) in ((wq[w], ("wq", w), qT, "qT"), (wk[w], ("wk", w), kT, "kT")):
                        ri = rcnt[0] % 2
                        rcnt[0] += 1
                        ba = proj_fm(wa, wak, tq)
                        kb.op("act", lambda e: e.activation(out=qraws[ri][:], in_=ps[ba][:, 0:512], func=AF.Copy), reads=[psk(ba)], writes=[("qraw", ri)])
                        pend_rope.append((tq, ci, ri, ba, dstT, dk_))
                        if len(pend_rope) > 1:
                            rope_tail(*pend_rope.pop(0))
                while pend_rope:
                    rope_tail(*pend_rope.pop(0))
                proj_v(wv[w], ("wv", w))
                if dh + 1 < 4:
                    lw_b2(dh + 1)
                for qt in range(NQ):
                    for bz in (4, 5, 6):
                        kb.op("pe", lambda e: e.matmul(ps[bz][:, 0:512], lhsT=zeros[:, 0:128], rhs=zeros[:, 0:512], start=True, stop=False),
                              reads=["c_zeros"], writes=[psk(bz)])

                    def acc(j, br):
                        a = j * 2 + br
                        return 4 + a // 3, (a % 3) * 132
                    nsteps = 4 * qt + 4

                    def scores(kbk):
                        i = kbk - 4 * qt
                        diag = i >= 0
                        c0 = max(0, i) * 128
                        s = kbk % 2
                        for br in range(2):
                            bs = 2 * s + br
                            pb = 64 * br
                            kb.op("pe", lambda e: e.matmul(ps[bs][:, c0:512], lhsT=kT[pb:pb + 64, kbk * 128:(kbk + 1) * 128],
                                                           rhs=qT[pb:pb + 64, qt * 512 + c0:(qt + 1) * 512], start=True, stop=True),
                                  reads=[("kT", kbk // 4), ("qT", qt)], writes=[psk(bs)])
                        kb.op("act", lambda e: e.activation(out=Pd[s][:, :, c0:512], in_=psall[:, 2 * s:2 * s + 2, c0:512], func=AF.Exp, scale=0.125),
                              reads=[psk(2 * s), psk(2 * s + 1)], writes=[("P", s)])
                        if diag:
                            kb.op("pool", lambda e: e.tensor_tensor(out=Pd[s][:, :, c0:c0 + 128], in0=Pd[s][:, :, c0:c0 + 128], in1=m01db, op=ALU.mult),
                                  reads=[("P", s), "c_m01_df"], writes=[("P", s)])

                    def av(kbk):
                        i = kbk - 4 * qt
                        s = kbk % 2
                        for j in range(max(0, i), 4):
                            for br in range(2):
                                ba, off = acc(j, br)
                                kb.op("pe", lambda e: e.matmul(ps[ba][:, off:off + 129], lhsT=Pd[s][:, br, j * 128:(j + 1) * 128], rhs=V[:, kbk, 0:129],
                                                               start=False, stop=False),
                                      reads=[("P", s), ("V", kbk // 4), "Vones"], writes=[psk(ba)])

                    scores(0)
                    for kbk in range(nsteps):
                        if kbk + 1 < nsteps:
                            scores(kbk + 1)
                        av(kbk)
                        step_bg()
                    for bz in (4, 5, 6):
                        kb.op("pe", lambda e: e.matmul(ps[bz][:, 0:2], lhsT=zeros[:, 0:128], rhs=zeros[:, 0:2], start=False, stop=True),
                              reads=["c_zeros"], writes=[psk(bz)])
                    af = afc[0] % 2
                    afc[0] += 1
                    for bi in range(3):
                        evac(accS[af][:, bi * 396:(bi + 1) * 396], ps[4 + bi][:, 0:396], reads=[psk(4 + bi)], writes=[("accS", af, bi)])
                    while fin_pending:
                        drain(fin_pending[0])
                        fin_pending.pop(0)
                    fin_pending.append(finalize(dh, qt, af))
                while fin_pending:
                    drain(fin_pending[0])
                    fin_pending.pop(0)

        def stage_C():
            wua = [kb.sbuf(f"wua{i}", [128, 4, 128], BF16) for i in range(2)]
            wub = [kb.sbuf(f"wub{i}", [128, 4, 128], BF16) for i in range(2)]
            sga = [kb.sbuf(f"sga{i}", [128, 512], F32) for i in range(2)]
            sgb = [kb.sbuf(f"sgb{i}", [128, 512], F32) for i in range(2)]
            ya = [kb.sbuf(f"ya{i}", [128, 512], F32) for i in range(2)]
            yb = [kb.sbuf(f"yb{i}", [128, 512], F32) for i in range(2)]
            yo = [kb.sbuf(f"yo{i}", [128, 512], BF16) for i in range(2)]
            it = 0
            ub = 0
            wga = [kb.sbuf(f"wga{i}", [128, 8, 128], BF16) for i in range(2)]
            wgb = [kb.sbuf(f"wgb{i}", [128, 8, 128], BF16) for i in range(2)]

            def lw_c(ec):
                w = ec % 2
                load_w(wga[w], ("wga", w), 3072 + ec * 128)
                load_w(wgb[w], ("wgb", w), 4096 + ec * 128)
                kb.dma("pool", lambda e: e.dma_start(out=wua[w][:], in_=w_up_a_d[:, ec * 128:(ec + 1) * 128].rearrange("(c p) n -> p c n", p=128)),
                       writes=[("wua", w)], sem=f"wua{w}")
                kb.dma("pool", lambda e: e.dma_start(out=wub[w][:], in_=w_up_b_d[:, ec * 128:(ec + 1) * 128].rearrange("(c p) n -> p c n", p=128)),
                       writes=[("wub", w)], sem=f"wub{w}")
            lw_c(0)
            for ec in range(8):
                w = ec % 2
                if ec + 1 < 8:
                    lw_c(ec + 1)
                for tq in range(NQ):
                    i = it % 2
                    it += 1
                    bga = proj_fm(wga[w], ("wga", w), tq)
                    bgb = proj_fm(wgb[w], ("wgb", w), tq)
                    bua = 4 + (ub % 4)
                    bub = 4 + ((ub + 1) % 4)
                    ub += 2
                    for (bb_, wt_, wk_, OT, ok_, nh) in ((bua, wua[w], ("wua", w), OaT, "OaT", 8), (bub, wub[w], ("wub", w), ObT, "ObT", 4)):
                        for c in range(4):
                            rk = [(ok_, 2 * c, tq), (ok_, 2 * c + 1, tq)] if nh == 8 else [(ok_, c, tq)]
                            kb.op("pe", lambda e: e.matmul(ps[bb_][:, 0:512], lhsT=wt_[:, c, :], rhs=OT[:, c, tq * 512:(tq + 1) * 512],
                                                           start=(c == 0), stop=(c == 3)),
                                  reads=[wk_] + rk, writes=[psk(bb_)])
                    kb.op("act", lambda e: e.activation(out=sga[i][:], in_=ps[bga][:, 0:512], func=AF.Sigmoid), reads=[psk(bga)], writes=[("sga", i)])
                    kb.op("act", lambda e: e.activation(out=sgb[i][:], in_=ps[bgb][:, 0:512], func=AF.Sigmoid), reads=[psk(bgb)], writes=[("sgb", i)])
                    kb.op("dve", lambda e: e.tensor_tensor(out=ya[i][:], in0=ps[bua][:, 0:512], in1=sga[i][:], op=ALU.mult),
                          reads=[psk(bua), ("sga", i)], writes=[("ya", i)])
                    kb.op("dve", lambda e: e.tensor_tensor(out=yb[i][:], in0=ps[bub][:, 0:512], in1=sgb[i][:], op=ALU.mult),
                          reads=[psk(bub), ("sgb", i)], writes=[("yb", i)])
                    kb.op("pool", lambda e: e.tensor_tensor(out=yo[i][:], in0=ya[i][:], in1=yb[i][:], op=ALU.add),
                          reads=[("ya", i), ("yb", i)], writes=[("yo", i)])
                    kb.dma("sp", lambda e: e.dma_start(out=yT_d[ec, :, tq * 512:(tq + 1) * 512], in_=yo[i][:]),
                           reads=[("yo", i)], writes=[("yT_d", ec, tq)], sem=f"yo{i}")

        def stage_D(h2all, M32, OH, W12):
            wout = kb.sbuf("wout", [128, 8, 1024], BF16)
            for hf in range(2):
                kb.dma("pool", lambda e: e.dma_start(out=wout[:, :, hf * 512:(hf + 1) * 512],
                                                     in_=w_out_d[:, hf * 512:(hf + 1) * 512].rearrange("(kc p) n -> p kc n", p=128)),
                       writes=["wout"], sem="wout")
            g2 = kb.sbuf("g2", [128, 1024], F32)
            kb.dma("sp", lambda e: e.dma_start(out=g2[:], in_=bcast_rows(g_ffn_d, 1024)), writes=["g2"], sem="g2")
            wr = kb.sbuf("wr", [128, 8, 36], F32)
            kb.dma("sp", lambda e: e.dma_start(out=wr[:, :, 0:4], in_=w_rg_d.rearrange("(kc p) c -> p kc c", p=128)), writes=["wr"], sem="wr")
            kb.dma("sp", lambda e: e.dma_start(out=wr[:, :, 4:36], in_=w_re_d.rearrange("(kc p) c -> p kc c", p=128)), writes=["wr"], sem="wr")
            rbias = kb.sbuf("rbias", [128, 36], F32)
            kb.dma("sp", lambda e: e.dma_start(out=rbias[:, 0:4], in_=bcast_rows(b_rg_d, 4)), writes=["rbias"], sem="rbias")
            kb.dma("sp", lambda e: e.dma_start(out=rbias[:, 4:36], in_=bcast_rows(b_re_d, 32)), writes=["rbias"], sem="rbias")
            identf = csb["c_ident_f"]
            yt = [kb.sbuf(f"yt{i}", [128, 8, 512], BF16) for i in range(2)]
            xt = [kb.sbuf(f"xtD{i}", [128, 1024], F32) for i in range(4)]
            x1t = [kb.sbuf(f"x1t{i}", [128, 1024], F32) for i in range(4)]
            h2f = [kb.sbuf(f"h2f{i}", [128, 1024], F32) for i in range(4)]
            h2T = [kb.sbuf(f"h2T{i}", [128, 8, 128], F32) for i in range(4)]
            rt = [kb.sbuf(f"rt{i}", [128, 16], F32) for i in range(4)]
            lg = [kb.sbuf(f"lg{i}", [128, 36], F32) for i in range(4)]
            em = [kb.sbuf(f"em{i}", [128, 32], F32) for i in range(4)]
            em2 = [kb.sbuf(f"em2{i}", [128, 32], F32) for i in range(4)]
            gm = [kb.sbuf(f"gm{i}", [128, 8], F32) for i in range(4)]
            def d_tile(tq, j, sl):
                yi = tq % 2
                tt = 4 * tq + j
                i = sl
                kb.dma("sp", lambda e: e.dma_start(out=xt[i][:], in_=x_d[tt * 128:(tt + 1) * 128, :]), writes=[("xtD", i)], sem=f"xtD{i}")
                for hf in range(2):
                    b = 2 * sl + hf
                    for ec in range(8):
                        kb.op("pe", lambda e: e.matmul(ps[b][:, 0:512], lhsT=yt[yi][:, ec, j * 128:(j + 1) * 128],
                                                       rhs=wout[:, ec, hf * 512:(hf + 1) * 512], start=(ec == 0), stop=(ec == 7)),
                              reads=[("yt", yi), "wout"], writes=[psk(b)])
                    kb.op("dve", lambda e: e.tensor_tensor(out=x1t[i][:, hf * 512:(hf + 1) * 512], in0=ps[b][:, 0:512],
                                                           in1=xt[i][:, hf * 512:(hf + 1) * 512], op=ALU.add),
                          reads=[psk(b), ("xtD", i)], writes=[("x1t", i, hf)])
                kb.dma("pool", lambda e: e.dma_start(out=x1_d[tt * 128:(tt + 1) * 128, :], in_=x1t[i][:]),
                       reads=[("x1t", i, 0), ("x1t", i, 1)], writes=[("x1_d", tt)], sem=f"x1t{i}")
                yield
                R = ("rt", i)
                kb.op("act", lambda e: e.activation(out=junk[:], in_=x1t[i][:], func=AF.Square, accum_out=rt[i][:, 0:1]),
                      reads=[("x1t", i, 0), ("x1t", i, 1)], writes=["junk", R])
                kb.op("act", lambda e: e.activation(out=rt[i][:, 1:2], in_=rt[i][:, 0:1], func=AF.Ln, bias=EPS, scale=1.0 / 1024), reads=[R], writes=[R])
                kb.op("act", lambda e: e.activation(out=rt[i][:, 2:3], in_=rt[i][:, 1:2], func=AF.Exp, scale=-0.5), reads=[R], writes=[R])
                kb.op("dve", lambda e: e.scalar_tensor_tensor(out=h2f[i][:], in0=x1t[i][:], scalar=rt[i][:, 2:3], in1=g2[:], op0=ALU.mult, op1=ALU.mult),
                      reads=[("x1t", i, 0), ("x1t", i, 1), R, "g2"], writes=[("h2f", i)])
                kb.op("pool", lambda e: e.tensor_copy(out=h2all[:, tt, :], in_=h2f[i][:]), reads=[("h2f", i)], writes=[("h2all", tt)])
                yield
                for hf in range(2):
                    b = 2 * sl + hf
                    for k4 in range(4):
                        kc = hf * 4 + k4
                        kb.op("pe", lambda e: e.transpose(out=ps[b][:, k4 * 128:(k4 + 1) * 128], in_=h2f[i][:, kc * 128:(kc + 1) * 128], identity=identf[:]),
                              reads=[("h2f", i), "c_ident_f"], writes=[psk(b)])
                    evac(h2T[i][:, hf * 4:(hf + 1) * 4, :], ps[b][:, 0:512].rearrange("p (k t) -> p k t", k=4), reads=[psk(b)], writes=[("h2T", i, hf)])
                yield
                b = 2 * sl
                for kc in range(8):
                    kb.op("pe", lambda e: e.matmul(ps[b][:, 0:36], lhsT=h2T[i][:, kc, :], rhs=wr[:, kc, :], start=(kc == 0), stop=(kc == 7)),
                          reads=[("h2T", i, kc // 4), "wr"], writes=[psk(b)])
                L = ("lg", i)
                kb.op("dve", lambda e: e.tensor_tensor(out=lg[i][:], in0=ps[b][:, 0:36], in1=rbias[:], op=ALU.add), reads=[psk(b), "rbias"], writes=[L])
                yield
                kb.op("dve", lambda e: e.reduce_max(out=rt[i][:, 3:4], in_=lg[i][:, 0:4], axis=AX.X), reads=[L], writes=[R])
                kb.op("dve", lambda e: e.tensor_scalar(out=gm[i][:, 0:4], in0=lg[i][:, 0:4], scalar1=rt[i][:, 3:4], scalar2=None, op0=ALU.is_equal),
                      reads=[L, R], writes=[("gm", i)])
                kb.op("dve", lambda e: e.tensor_scalar(out=rt[i][:, 4:5], in0=rt[i][:, 3:4], scalar1=-1.0, scalar2=None, op0=ALU.mult), reads=[R], writes=[R])
                kb.op("act", lambda e: e.activation(out=gm[i][:, 4:8], in_=lg[i][:, 0:4], func=AF.Exp, bias=rt[i][:, 4:5], accum_out=rt[i][:, 5:6]),
                      reads=[L, R], writes=[("gm2", i), R])
                kb.op("dve", lambda e: e.reciprocal(out=rt[i][:, 6:7], in_=rt[i][:, 5:6]), reads=[R], writes=[R])
                yield
                kb.op("dve", lambda e: e.tensor_scalar(out=gm[i][:, 0:4], in0=gm[i][:, 0:4], scalar1=1e30, scalar2=-1e30, op0=ALU.mult, op1=ALU.add),
                      reads=[("gm", i)], writes=[("gm", i)])
                pen = bass.AP(gm[i], 0, [[8, 128], [1, 4], [0, 8]])
                kb.op("dve", lambda e: e.tensor_tensor(out=em[i][:].rearrange("p (g e) -> p g e", g=4), in0=lg[i][:, 4:36].rearrange("p (g e) -> p g e", g=4),
                                                       in1=pen, op=ALU.add), reads=[L, ("gm", i)], writes=[("em", i)])
                kb.op("dve", lambda e: e.reduce_max(out=rt[i][:, 7:8], in_=em[i][:], axis=AX.X), reads=[("em", i)], writes=[R])
                kb.op("dve", lambda e: e.tensor_scalar(out=OH[:, tt, 0, :], in0=em[i][:], scalar1=rt[i][:, 7:8], scalar2=None, op0=ALU.is_equal),
                      reads=[("em", i), R], writes=[("OH", tt)])
                yield
                kb.op("dve", lambda e: e.scalar_tensor_tensor(out=em2[i][:], in0=OH[:, tt, 0, :], scalar=-1e30, in1=em[i][:], op0=ALU.mult, op1=ALU.add),
                      reads=[("OH", tt), ("em", i)], writes=[("em2", i)])
                kb.op("dve", lambda e: e.reduce_max(out=rt[i][:, 8:9], in_=em2[i][:], axis=AX.X), reads=[("em2", i)], writes=[R])
                kb.op("dve", lambda e: e.tensor_scalar(out=OH[:, tt, 1, :], in0=em2[i][:], scalar1=rt[i][:, 8:9], scalar2=None, op0=ALU.is_equal),
                      reads=[("em2", i), R], writes=[("OH", tt)])
                kb.op("dve", lambda e: e.tensor_tensor(out=rt[i][:, 9:10], in0=rt[i][:, 8:9], in1=rt[i][:, 7:8], op=ALU.subtract), reads=[R], writes=[R])
                kb.op("act", lambda e: e.activation(out=rt[i][:, 10:11], in_=rt[i][:, 9:10], func=AF.Exp), reads=[R], writes=[R])
                yield
                kb.op("dve", lambda e: e.tensor_scalar(out=rt[i][:, 11:12], in0=rt[i][:, 10:11], scalar1=1.0, scalar2=None, op0=ALU.add), reads=[R], writes=[R])
                kb.op("dve", lambda e: e.reciprocal(out=rt[i][:, 12:13], in_=rt[i][:, 11:12]), reads=[R], writes=[R])
                kb.op("dve", lambda e: e.tensor_tensor(out=W12[:, tt, 0:1], in0=rt[i][:, 6:7], in1=rt[i][:, 12:13], op=ALU.mult), reads=[R], writes=[("W12", tt)])
                kb.op("dve", lambda e: e.tensor_tensor(out=W12[:, tt, 1:2], in0=rt[i][:, 6:7], in1=W12[:, tt, 0:1], op=ALU.subtract),
                      reads=[R, ("W12", tt)], writes=[("W12", tt)])
                kb.op("dve", lambda e: e.tensor_tensor(out=M32[:, tt, :], in0=OH[:, tt, 0, :], in1=OH[:, tt, 1, :], op=ALU.add),
                      reads=[("OH", tt)], writes=[("M32", tt)])
                yield

            def d_tile_start(tq, j, sl):
                if j == 0:
                    yi = tq % 2
                    kb.dma("sp", lambda e: e.dma_start(out=yt[yi][:], in_=yT_d[:, :, tq * 512:(tq + 1) * 512].rearrange("c p t -> p c t")),
                           reads=[("yT_d", ec, tq) for ec in range(8)], writes=[("yt", yi)], sem=f"yt{yi}")
                yield from d_tile(tq, j, sl)

            makers = []
            for tq in range(NQ):
                for j in range(4):
                    makers.append(lambda sl, tq=tq, j=j: d_tile_start(tq, j, sl))
            run_streams(makers, 4, bg_every=10 ** 9)

        def stage_E(h2all, M32, OH, W12, desti, widx):
            trif = csb["c_tri_f"]
            onesf = csb["c_ones_f"]
            Mcum = kb.sbuf("Mcum", [128, NT + 1, 32], F32)
            kb.op("dve", lambda e: e.memset(Mcum[:, 0, :], 0.0), writes=[("Mcum", 0)])
            for tt in range(NT):
                kb.op("dve", lambda e: e.tensor_tensor(out=Mcum[:, tt + 1, :], in0=Mcum[:, tt, :], in1=M32[:, tt, :], op=ALU.add),
                      reads=[("Mcum", tt), ("M32", tt)], writes=[("Mcum", tt + 1)])
            cnt = kb.sbuf("cnt", [128, 32], F32)
            cnti = kb.sbuf("cnti", [128, 32], I32)
            cnti2 = kb.sbuf("cnti2", [128, 32], I32)
            padded = kb.sbuf("padded", [128, 32], F32)
            pend = kb.sbuf("pend", [128, 32], F32)
            pstart = kb.sbuf("pstart", [128, 32], F32)
            ones32 = kb.sbuf("ones32", [128, 32], F32)
            kb.op("pe", lambda e: e.matmul(ps[0][:, 0:32], lhsT=onesf[:], rhs=Mcum[:, NT, :], start=True, stop=True),
                  reads=[("Mcum", NT), "c_ones_f"], writes=[psk(0)])
            kb.op("dve", lambda e: e.tensor_copy(out=cnti[:], in_=ps[0][:, 0:32]), reads=[psk(0)], writes=["cnti"])
            kb.op("dve", lambda e: e.tensor_scalar(out=cnti2[:], in0=cnti[:], scalar1=127, scalar2=None, op0=ALU.add), reads=["cnti"], writes=["cnti2"])
            kb.op("dve", lambda e: e.tensor_scalar(out=cnti[:], in0=cnti2[:], scalar1=7, scalar2=7, op0=ALU.arith_shift_right, op1=ALU.logical_shift_left),
                  reads=["cnti2"], writes=["cnti"])
            kb.op("dve", lambda e: e.tensor_copy(out=padded[:], in_=cnti[:]), reads=["cnti"], writes=["padded"])
            kb.op("dve", lambda e: e.memset(ones32[:], 1.0), writes=["ones32"])
            kb.op("dve", lambda e: e.tensor_tensor_scan(out=pend[:], data0=ones32[:], data1=padded[:], initial=0.0, op0=ALU.mult, op1=ALU.add),
                  reads=["ones32", "padded"], writes=["pend"])
            kb.op("dve", lambda e: e.tensor_tensor(out=pstart[:], in0=pend[:], in1=padded[:], op=ALU.subtract), reads=["pend", "padded"], writes=["pstart"])
            destf = kb.sbuf("destf", [128, 2 * NT], F32)
            base = [kb.sbuf(f"base{i}", [128, 32], F32) for i in range(2)]
            prod = [kb.sbuf(f"prod{i}", [128, 32], F32) for i in range(2)]
            for tt in range(NT):
                i = tt % 2
                b = 1 + i
                kb.op("pe", lambda e: e.matmul(ps[b][:, 0:32], lhsT=onesf[:], rhs=Mcum[:, tt, :], start=True, stop=False),
                      reads=[("Mcum", tt), "c_ones_f"], writes=[psk(b)])
                kb.op("pe", lambda e: e.matmul(ps[b][:, 0:32], lhsT=trif[:], rhs=M32[:, tt, :], start=False, stop=True),
                      reads=[("M32", tt), "c_tri_f"], writes=[psk(b)])
                kb.op("dve", lambda e: e.tensor_tensor(out=base[i][:], in0=ps[b][:, 0:32], in1=pstart[:], op=ALU.add),
                      reads=[psk(b), "pstart"], writes=[("base", i)])
                for k in range(2):
                    kb.op("dve", lambda e: e.tensor_tensor(out=prod[k][:], in0=OH[:, tt, k, :], in1=base[i][:], op=ALU.mult),
                          reads=[("OH", tt), ("base", i)], writes=[("prod", k)])
                    kb.op("dve", lambda e: e.reduce_sum(out=destf[:, 2 * tt + k:2 * tt + k + 1], in_=prod[k][:], axis=AX.X),
                          reads=[("prod", k)], writes=["destf"])
            kb.op("dve", lambda e: e.tensor_copy(out=desti[:], in_=destf[:]), reads=["destf"], writes=["desti"])
            thr = kb.sbuf("thr", [128, NB], F32)
            cmp_ = kb.sbuf("cmp", [128, NB, 32], F32)
            be = kb.sbuf("be", [128, NB], F32)
            pidx = kb.sbuf("pidx", [128, 1], F32)
            kb.op("pool", lambda e: e.iota(thr[:], pattern=[[128, NB]], base=0, channel_multiplier=0, allow_small_or_imprecise_dtypes=True), writes=["thr"])
            kb.op("pool", lambda e: e.iota(pidx[:], pattern=[[1, 1]], base=0, channel_multiplier=1, allow_small_or_imprecise_dtypes=True), writes=["pidx"])
            pend_b = bass.AP(pend, 0, [[32, 128], [0, NB], [1, 32]])
            thr_b = bass.AP(thr, 0, [[NB, 128], [1, NB], [0, 32]])
            kb.op("dve", lambda e: e.tensor_tensor(out=cmp_[:], in0=pend_b, in1=thr_b, op=ALU.is_le), reads=["pend", "thr"], writes=["cmp"])
            kb.op("dve", lambda e: e.reduce_sum(out=be[:], in_=cmp_[:], axis=AX.X), reads=["cmp"], writes=["be"])
            kb.op("dve", lambda e: e.tensor_scalar(out=be[:], in0=be[:], scalar1=31.0, scalar2=128.0, op0=ALU.min, op1=ALU.mult), reads=["be"], writes=["be"])
            kb.op("dve", lambda e: e.tensor_scalar(out=be[:], in0=be[:], scalar1=pidx[:, 0:1], scalar2=None, op0=ALU.add), reads=["be", "pidx"], writes=["be"])
            kb.op("dve", lambda e: e.tensor_copy(out=widx[:], in_=be[:]), reads=["be"], writes=["widx"])
            for tt in range(NT):
                for k in range(2):
                    kb.dma("pool", lambda e: e.indirect_dma_start(out=xd_d, out_offset=bass.IndirectOffsetOnAxis(ap=desti[:, 2 * tt + k:2 * tt + k + 1], axis=0),
                                                                  in_=h2all[:, tt, :], in_offset=None),
                           reads=[("h2all", tt), "desti"], writes=[("xd_d", (2 * tt + k) % 4)], sem=f"scat{(2 * tt + k) % 4}")

        def stage_F(widx):
            NBUF = 3
            wcomb = [kb.sbuf(f"wcomb{i}", [128, 6144], BF16) for i in range(NBUF)]
            wg = [t[:, 0:2048] for t in wcomb]
            wu = [t[:, 2048:4096] for t in wcomb]
            wd = [t[:, 4096:6144] for t in wcomb]
            xdt = [kb.sbuf(f"xdt{i}", [128, 1024], BF16) for i in range(NBUF)]
            xdT = [kb.sbuf(f"xdT{i}", [128, 8, 128], BF16) for i in range(2)]
            sg = [kb.sbuf(f"sg{i}", [128, 256], F32) for i in range(2)]
            actT = [kb.sbuf(f"actT{i}", [128, 256], BF16) for i in range(2)]
            ydt = [kb.sbuf(f"ydt{i}", [128, 1024], F32) for i in range(2)]

            def prefetch(b_):
                i = b_ % NBUF
                kb.dma("pool", lambda e: e.indirect_dma_start(out=wcomb[i][:], out_offset=None, in_=wall_d,
                                                              in_offset=bass.IndirectOffsetOnAxis(ap=widx[:, b_:b_ + 1], axis=0)),
                       reads=["widx"] + WALL_KEYS, writes=[("wcomb", i)], sem=f"wcomb{i}")
                kb.dma("sp", lambda e: e.dma_start(out=xdt[i][:], in_=xd_d[b_ * 128:(b_ + 1) * 128, :]), reads=XD_KEYS, writes=[("xdt", i)], sem=f"xdt{i}")

            def compute(b_):
                i = b_ % NBUF
                j = b_ % 2
                bt = j
                psb = ps[bt][:].bitcast(BF16)
                for kc in range(8):
                    kb.op("pe", lambda e: e.transpose(out=psb[:, kc * 128:(kc + 1) * 128], in_=xdt[i][:, kc * 128:(kc + 1) * 128], identity=ident[:]),
                          reads=[("xdt", i), "c_ident_bf"], writes=[psk(bt)])
                evac(xdT[j][:], psb.rearrange("p (k t) -> p k t", k=8), reads=[psk(bt)], writes=[("xdT", j)])
                bg = 2 + j
                for slot, (wt, nm) in enumerate(((wg[i], "wg"), (wg[i], "wg"), (wu[i], "wu"), (wu[i], "wu"))):
                    n2 = slot % 2
                    for kc in range(8):
                        kb.op("pe", lambda e: e.matmul(ps[bg][:, slot * 128:(slot + 1) * 128], lhsT=wt[:, kc * 256 + n2 * 128:kc * 256 + (n2 + 1) * 128],
                                                       rhs=xdT[j][:, kc, :], start=(kc == 0), stop=(kc == 7)),
                              reads=[("wcomb", i), ("xdT", j)], writes=[psk(bg)])
                kb.op("act", lambda e: e.activation(out=sg[j][:], in_=ps[bg][:, 0:256], func=AF.Silu), reads=[psk(bg)], writes=[("sg", j)])
                kb.op("dve", lambda e: e.tensor_tensor(out=actT[j][:], in0=ps[bg][:, 256:512], in1=sg[j][:], op=ALU.mult),
                      reads=[psk(bg), ("sg", j)], writes=[("actT", j)])
                for hf in range(2):
                    bd = 4 + (2 * b_ + hf) % 4
                    for n2 in range(2):
                        kb.op("pe", lambda e: e.matmul(ps[bd][:, 0:512], lhsT=actT[j][:, n2 * 128:(n2 + 1) * 128],
                                                       rhs=wd[i][:, n2 * 1024 + hf * 512:n2 * 1024 + (hf + 1) * 512], start=(n2 == 0), stop=(n2 == 1)),
                              reads=[("actT", j), ("wcomb", i)], writes=[psk(bd)])
                    evac(ydt[j][:, hf * 512:(hf + 1) * 512], ps[bd][:, 0:512], reads=[psk(bd)], writes=[("ydt", j, hf)])
                kb.dma("act", lambda e: e.dma_start(out=yd_d[b_ * 128:(b_ + 1) * 128, :], in_=ydt[j][:]),
                       reads=[("ydt", j, 0), ("ydt", j, 1)], writes=[("yd_d", b_ % 2)], sem=f"ydt{j}")

            prefetch(0)
            if NB > 1:
                prefetch(1)
            for b_ in range(NB):
                if b_ + 2 < NB:
                    prefetch(b_ + 2)
                compute(b_)

        def stage_G(desti, W12):
            NBUF = 3
            gf = kb.sbuf("gf", [128, 1024], F32)
            kb.dma("sp", lambda e: e.dma_start(out=gf[:], in_=bcast_rows(g_fin_d, 1024)), writes=["gf"], sem="gf")
            g0 = [kb.sbuf(f"g0_{i}", [128, 1024], F32) for i in range(NBUF)]
            g1 = [kb.sbuf(f"g1_{i}", [128, 1024], F32) for i in range(NBUF)]
            xx = [kb.sbuf(f"xx_{i}", [128, 1024], F32) for i in range(NBUF)]
            oo = [kb.sbuf(f"oo_{i}", [128, 1024], F32) for i in range(2)]
            rg = [kb.sbuf(f"rg_{i}", [128, 4], F32) for i in range(2)]

            def prefetch(tt):
                i = tt % NBUF
                for k, gt in ((0, g0[i]), (1, g1[i])):
                    kb.dma("pool", lambda e: e.indirect_dma_start(out=gt[:], out_offset=None, in_=yd_d,
                                                                  in_offset=bass.IndirectOffsetOnAxis(ap=desti[:, 2 * tt + k:2 * tt + k + 1], axis=0)),
                           reads=YD_KEYS + ["desti"], writes=[("gg", k, i)], sem=f"gg{k}{i}")
                kb.dma("sp", lambda e: e.dma_start(out=xx[i][:], in_=x1_d[tt * 128:(tt + 1) * 128, :]), reads=[("x1_d", tt)], writes=[("xx", i)], sem=f"xx{i}")

            def compute(tt):
                i = tt % NBUF
                j = tt % 2
                kb.op("dve", lambda e: e.scalar_tensor_tensor(out=xx[i][:], in0=g0[i][:], scalar=W12[:, tt, 0:1], in1=xx[i][:], op0=ALU.mult, op1=ALU.add),
                      reads=[("gg", 0, i), ("xx", i), ("W12", tt)], writes=[("xx", i)])
                kb.op("dve", lambda e: e.scalar_tensor_tensor(out=xx[i][:], in0=g1[i][:], scalar=W12[:, tt, 1:2], in1=xx[i][:], op0=ALU.mult, op1=ALU.add),
                      reads=[("gg", 1, i), ("xx", i), ("W12", tt)], writes=[("xx", i)])
                kb.op("act", lambda e: e.activation(out=junk[:], in_=xx[i][:], func=AF.Square, accum_out=rg[j][:, 0:1]), reads=[("xx", i)], writes=["junk", ("rg", j)])
                kb.op("act", lambda e: e.activation(out=rg[j][:, 1:2], in_=rg[j][:, 0:1], func=AF.Ln, bias=EPS, scale=1.0 / 1024), reads=[("rg", j)], writes=[("rg", j)])
                kb.op("act", lambda e: e.activation(out=rg[j][:, 2:3], in_=rg[j][:, 1:2], func=AF.Exp, scale=-0.5), reads=[("rg", j)], writes=[("rg", j)])
                kb.op("dve", lambda e: e.scalar_tensor_tensor(out=oo[j][:], in0=xx[i][:], scalar=rg[j][:, 2:3], in1=gf[:], op0=ALU.mult, op1=ALU.mult),
                      reads=[("xx", i), ("rg", j), "gf"], writes=[("oo", j)])
                kb.dma("act", lambda e: e.dma_start(out=out_d[tt * 128:(tt + 1) * 128, :], in_=oo[j][:]), reads=[("oo", j)], writes=[("out_d", tt)], sem=f"oo{j}")

            prefetch(0)
            if NT > 1:
                prefetch(1)
            for tt in range(NT):
                if tt + 2 < NT:
                    prefetch(tt + 2)
                compute(tt)

        with kb.scope():
            stage_A()
        zfill = kb.sbuf("zfill", [128, 2048], BF16)
        kb.op("pool", lambda e: e.memset(zfill[:], 0.0), writes=["zfill"])
        for c in range(NROWS // 256):
            bg_ops.append(lambda c=c: kb.dma(
                "sp", lambda e: e.dma_start(out=xd_d[c * 256:(c + 1) * 256, :].rearrange("(p j) d -> p (j d)", j=2), in_=zfill[:]),
                reads=["zfill"], writes=[("xd_d", c % 4)], sem="zf"))
        for k_, (nm, src) in enumerate((("wg", wg_d), ("wu", wu_d), ("wd", wd_d))):
            for c in range(16):
                bg_ops.append(lambda k_=k_, src=src, c=c: kb.dma(
                    "pool", lambda e: e.dma_start(out=wall_d[c * 256:(c + 1) * 256, k_ * 2048:(k_ + 1) * 2048], in_=src[c * 256:(c + 1) * 256, :]),
                    writes=[("wall", (k_ * 16 + c) % 4)], sem=f"pc{(k_ * 16 + c) % 4}"))
        with kb.scope():
            stage_B1()
        with kb.scope():
            stage_B2()
        if dbg:
            for nm, T, keys, nch in (("dbg_oa", OaT, [("OaT", h, q) for h in range(8) for q in range(NQ)], 4),
                                     ("dbg_ob", ObT, [("ObT", h, q) for h in range(4) for q in range(NQ)], 4)):
                if nm[4:] in dbg:
                    o = dout(nm, [128, nch, S])
                    tmp = kb.sbuf(nm + "_t", [128, nch, S], F32)
                    kb.op("dve", lambda e: e.tensor_copy(out=tmp[:], in_=T[:]), reads=keys, writes=[nm])
                    kb.dma("sp", lambda e: e.dma_start(out=o, in_=tmp[:]), reads=[nm], writes=[nm + "_d"], sem=nm)
        while bg_ops:
            bg_ops.pop(0)()
        PB[0] = 4
        with kb.scope():
            stage_C()
        st_main.close()
        with contextlib.ExitStack() as st2:
            kb.stack = st2
            h2all = kb.sbuf("h2all", [128, NT, 1024], BF16)
            M32 = kb.sbuf("M32", [128, NT, 32], F32)
            OH = kb.sbuf("OH", [128, NT, 2, 32], F32)
            W12 = kb.sbuf("W12", [128, NT, 2], F32)
            desti = kb.sbuf("desti", [128, 2 * NT], I32)
            widx = kb.sbuf("widx", [128, NB], I32)
            with kb.scope():
                stage_D(h2all, M32, OH, W12)
            with kb.scope():
                stage_E(h2all, M32, OH, W12, desti, widx)
            with kb.scope():
                stage_F(widx)
            with kb.scope():
                stage_G(desti, W12)
        print("build: instr", kb.cnt, "waits", kb.n_wait, "sems", len(kb.sems))
    return nc


def _core_inputs(inputs, b, S, shared):
    d = dict(shared)
    d["x"] = np.ascontiguousarray(inputs["x"][b, :S], dtype=np.float32)
    return d


def _shared_inputs(inputs, S):
    f = lambda a: np.ascontiguousarray(np.asarray(a, dtype=np.float32))
    sh = {
        "w_in": f(inputs["w_in"][0]),
        "g_norm_mix": f(inputs["g_norm_mix"]).reshape(1, 1024),
        "g_subln": f(inputs["g_subln"]).reshape(1, 128),
        "w_up_a": f(inputs["w_up_a"][0]), "w_up_b": f(inputs["w_up_b"][0]), "w_out": f(inputs["w_out"][0]),
        "g_norm_ffn": f(inputs["g_norm_ffn"]).reshape(1, 1024),
        "w_router_group": f(inputs["w_router_group"][0]), "w_router_expert": f(inputs["w_router_expert"][0]),
        "b_router_group": f(inputs["b_router_group"]).reshape(1, 4), "b_router_expert": f(inputs["b_router_expert"]).reshape(1, 32),
        "g_norm_final": f(inputs["g_norm_final"]).reshape(1, 1024),
    }
    for k in ("lambda_q1", "lambda_k1", "lambda_q2", "lambda_k2"):
        sh[k] = f(inputs[k]).reshape(1, 64)
    wg = np.asarray(inputs["w_expert_gate"][0], dtype=np.float32)
    wu = np.asarray(inputs["w_expert_up"][0], dtype=np.float32)
    wd = np.asarray(inputs["w_expert_down"][0], dtype=np.float32)
    sh["wg_l"] = np.ascontiguousarray(wg.reshape(32, 8, 128, 256).transpose(0, 2, 1, 3)).reshape(4096, 2048)
    sh["wu_l"] = np.ascontiguousarray(wu.reshape(32, 8, 128, 256).transpose(0, 2, 1, 3)).reshape(4096, 2048)
    sh["wd_l"] = np.ascontiguousarray(wd.reshape(32, 2, 128, 1024).transpose(0, 2, 1, 3)).reshape(4096, 2048)
    sh.update(host_consts(S))
    return sh


_NC_CACHE = {}


def kernel(**inputs):
    S = SEQ
    B = inputs["x"].shape[0]
    if S not in _NC_CACHE:
        _NC_CACHE[S] = build(S)
    nc = _NC_CACHE[S]
    shared = _shared_inputs(inputs, S)
    in_maps = [_core_inputs(inputs, b, S, shared) for b in range(B)]
    res = run_bass_kernel_spmd(nc, in_maps, core_ids=list(range(B)))
    return np.stack([np.asarray(r["out"], dtype=np.float32) for r in res.results], axis=0)
```

```python
import contextlib
import numpy as np
import ml_dtypes
import concourse.bass as bass
import concourse.mybir as mybir
from concourse.bass_utils import run_bass_kernel_spmd

F32 = mybir.dt.float32
BF16 = mybir.dt.bfloat16
I32 = mybir.dt.int32
U32 = mybir.dt.uint32
AF = mybir.ActivationFunctionType
ALU = mybir.AluOpType
AX = mybir.AxisListType

D_MODEL = 1024
SEQ = 4096
IN_WIDTH = 5120
N_EXPERTS = 32
D_EXPERT = 256
EPS = 1e-6
EPOCH = 12000


class KB:
    def __init__(self, nc, stack):
        self.nc = nc
        self.stack = stack
        self.sem_stack = stack
        self.bar_tile = stack.enter_context(nc.sbuf_tensor("bar_tile", [128, 8], F32))
        self.eng = {"pe": nc.tensor, "dve": nc.vector, "act": nc.scalar,
                    "pool": nc.gpsimd, "sp": nc.sync}
        self.cnt = {e: 0 for e in self.eng}
        self.sems = {}
        self.waited = {e: {} for e in self.eng}
        self.last_w = {}
        self.readers = {}
        self.dma_cnt = {}
        self.dma_latest = {}
        self.n_wait = 0

    def sem(self, key):
        s = self.sems.get(key)
        if s is None:
            name = "s_" + "_".join(str(k) for k in key)
            s = self.sem_stack.enter_context(self.nc.semaphore(name))
            self.sems[key] = s
        return s

    def sbuf(self, name, shape, dt):
        return self.stack.enter_context(self.nc.sbuf_tensor(name, list(shape), dt))

    def psum(self, name, shape, dt):
        return self.stack.enter_context(self.nc.psum_tensor(name, list(shape), dt))

    def _deps(self, reads, writes, e=None):
        deps = {}

        def add(st):
            sk, v = st
            if sk[0] == "dma":
                v = max(v, self.dma_latest.get(sk, 0))
            if deps.get(sk, 0) < v:
                deps[sk] = v
        for k in reads:
            lw = self.last_w.get(k)
            if lw is not None:
                add(lw)
            if isinstance(k, tuple) and k and k[0] == "ps":
                for sk, v in self.readers.get(k, {}).items():
                    if e is None or sk[0] != e:
                        add((sk, v))
        for k in writes:
            lw = self.last_w.get(k)
            if lw is not None:
                add(lw)
            for sk, v in self.readers.get(k, {}).items():
                add((sk, v))
        return deps

    def _emit_waits(self, e, deps):
        for sk, v in deps.items():
            if sk[0] == "pe" and e == "pe":
                continue
            if self.waited[e].get(sk, 0) >= v:
                continue
            self.eng[e].wait_ge(self.sem(sk), v)
            self.waited[e][sk] = v
            self.n_wait += 1

    def _stamp(self, stamp, reads, writes):
        sk, v = stamp
        for k in reads:
            self.readers.setdefault(k, {})[sk] = v
        for k in writes:
            self.last_w[k] = stamp
            self.readers[k] = {}

    def op(self, e, fn, reads=(), writes=()):
        self._emit_waits(e, self._deps(reads, writes, e))
        inst = fn(self.eng[e])
        n = self.cnt[e]
        self.cnt[e] = n + 1
        sk = (e, n // EPOCH)
        v = (n % EPOCH) + 1
        inst.then_inc(self.sem(sk), 1)
        self._stamp((sk, v), reads, writes)
        return inst

    def dma(self, q, fn, reads=(), writes=(), sem=None):
        self._emit_waits(q, self._deps(reads, writes))
        inst = fn(self.eng[q])
        n = self.dma_cnt.get(sem, 0)
        self.dma_cnt[sem] = n + 1
        per = EPOCH // 16
        sk = ("dma", sem, n // per)
        v = ((n % per) + 1) * 16
        inst.then_inc(self.sem(sk), 16)
        self.dma_latest[sk] = v
        self._stamp((sk, v), reads, writes)
        return inst

    def barrier(self):
        if self.bar_tile is None:
            self.bar_tile = self.sem_stack.enter_context(self.nc.sbuf_tensor("bar_tile", [128, 8], F32))
        deps = {}
        for e, n in self.cnt.items():
            if n:
                deps[(e, (n - 1) // EPOCH)] = ((n - 1) % EPOCH) + 1
        for sk, v in self.dma_latest.items():
            deps[sk] = v
        self._emit_waits("pool", deps)
        self.op("pool", lambda e: e.memset(self.bar_tile[:], 0.0))
        n = self.cnt["pool"]
        st = {("pool", (n - 1) // EPOCH): ((n - 1) % EPOCH) + 1}
        for e in self.eng:
            if e != "pool":
                self._emit_waits(e, dict(st))

    @contextlib.contextmanager
    def scope(self):
        old = self.stack
        with contextlib.ExitStack() as sub:
            self.stack = sub
            try:
                yield
            finally:
                self.stack = old
            self.barrier()

    def wait_all(self, e, keys):
        self._emit_waits(e, self._deps(keys, keys))


def host_consts(S):
    bf = ml_dtypes.bfloat16
    j = np.arange(128)[:, None]
    s = np.arange(128)[None, :]
    c = {}
    c["c_ident_bf"] = np.eye(128, dtype=np.float32).astype(bf)
    c["c_ident_f"] = np.eye(128, dtype=np.float32)
    c["c_tri_m8"] = np.where(j >= s, -8.0, 0.0).astype(bf)
    c["c_ones_m8"] = np.full((128, 128), -8.0, np.float32).astype(bf)
    c["c_mask_sb"] = np.where(j >= s, -10000.0, 0.0).astype(bf)
    c["c_mask_df"] = np.where(j > s, -10000.0, 0.0).astype(bf)
    c["c_m01_sb"] = np.where(j < s, 1.0, 0.0).astype(bf)
    c["c_m01_df"] = np.where(j <= s, 1.0, 0.0).astype(bf)
    src = np.where((np.arange(128) % 64) < 32, np.arange(128) + 32, np.arange(128) - 32)
    perm = np.zeros((128, 128), np.float32)
    perm[src, np.arange(128)] = 1.0
    c["c_perm"] = perm.astype(bf)
    c["c_zeros"] = np.zeros((128, 512), np.float32).astype(bf)
    c["c_tri_f"] = np.where(j < s, 1.0, 0.0).astype(np.float32)
    c["c_ones_f"] = np.ones((128, 128), np.float32)
    pos = np.arange(S, dtype=np.float32)
    inv_freq = (1.0 / (np.float32(10000.0) ** (np.arange(0, 64, 2, dtype=np.float32) / np.float32(64)))).astype(np.float32)
    fr = (pos[None, :] * inv_freq[:, None]).astype(np.float32)
    cos = np.cos(fr).astype(np.float32)
    sin = np.sin(fr).astype(np.float32)
    cos64 = np.concatenate([cos, cos], 0)
    sin64 = np.concatenate([-sin, sin], 0)
    c["c_cos"] = np.ascontiguousarray(np.concatenate([cos64, cos64], 0))
    c["c_sin"] = np.ascontiguousarray(np.concatenate([sin64, sin64], 0))
    return c


CONST_SPECS = {
    "c_ident_bf": ([128, 128], BF16), "c_ident_f": ([128, 128], F32),
    "c_tri_m8": ([128, 128], BF16), "c_ones_m8": ([128, 128], BF16),
    "c_mask_sb": ([128, 128], BF16), "c_mask_df": ([128, 128], BF16),
    "c_m01_sb": ([128, 128], BF16), "c_m01_df": ([128, 128], BF16), "c_perm": ([128, 128], BF16),
    "c_zeros": ([128, 512], BF16), "c_tri_f": ([128, 128], F32), "c_ones_f": ([128, 128], F32),
}


def bcast_rows(ap2d, n):
    return bass.AP(ap2d.tensor, ap2d.offset, [[0, 128], [1, n]])


def build(S, stages="AB", dbg=()):
    NT = S // 128
    NQ = S // 512
    nc = bass.Bass("TRN2", target_bir_lowering=False)

    def din(name, shape, dt=F32):
        return nc.dram_tensor(name, list(shape), dt, kind="ExternalInput").ap()

    def dout(name, shape, dt=F32):
        return nc.dram_tensor(name, list(shape), dt, kind="ExternalOutput").ap()

    x_d = din("x", [S, 1024])
    w_in_d = din("w_in", [1024, IN_WIDTH])
    g_mix_d = din("g_norm_mix", [1, 1024])
    lam_d = {k: din(k, [1, 64]) for k in ("lambda_q1", "lambda_k1", "lambda_q2", "lambda_k2")}
    g_sub_d = din("g_subln", [1, 128])
    cd = {k: din(k, sh, dt) for k, (sh, dt) in CONST_SPECS.items()}
    cos_d = din("c_cos", [128, S])
    sin_d = din("c_sin", [128, S])
    NB = 2 * NT + 32
    NROWS = NB * 128
    w_up_a_d = din("w_up_a", [512, 1024])
    w_up_b_d = din("w_up_b", [512, 1024])
    w_out_d = din("w_out", [1024, 1024])
    g_ffn_d = din("g_norm_ffn", [1, 1024])
    w_rg_d = din("w_router_group", [1024, 4])
    w_re_d = din("w_router_expert", [1024, 32])
    b_rg_d = din("b_router_group", [1, 4])
    b_re_d = din("b_router_expert", [1, 32])
    wg_d = din("wg_l", [4096, 2048])
    wu_d = din("wu_l", [4096, 2048])
    wd_d = din("wd_l", [4096, 2048])
    g_fin_d = din("g_norm_final", [1, 1024])
    out_d = dout("out", [S, 1024])
    yT_d = nc.dram_tensor("yT_scr", [8, 128, S], BF16).ap()
    x1_d = nc.dram_tensor("x1_scr", [S, 1024], F32).ap()
    xd_d = nc.dram_tensor("xd_scr", [NROWS, 1024], BF16).ap()
    yd_d = nc.dram_tensor("yd_scr", [NROWS, 1024], F32).ap()
    wall_d = nc.dram_tensor("wall_bf", [4096, 6144], BF16).ap()
    outs = {}
    XD_KEYS = [("xd_d", i) for i in range(4)]
    YD_KEYS = [("yd_d", i) for i in range(2)]
    WALL_KEYS = [("wall", i) for i in range(4)]

    with contextlib.ExitStack() as st:
        kb = KB(nc, st)
        psall = kb.psum("psall", [128, 8, 512], F32)
        ps = [psall[:, i, :] for i in range(8)]
        psk = lambda b: ("ps", b)

        csb = {}
        for k, (sh, dt) in CONST_SPECS.items():
            t = kb.sbuf("sb_" + k, sh, dt)
            kb.dma("sp", lambda e, t=t, k=k: e.dma_start(out=t[:], in_=cd[k]), writes=[k], sem=k)
            csb[k] = t
        ident = csb["c_ident_bf"]
        zeros = csb["c_zeros"]
        junk = kb.sbuf("junk", [128, 1024], BF16)
        st_main = contextlib.ExitStack()
        kb.stack = st_main
        hT = kb.sbuf("hT", [128, 8, S], BF16)
        OaT = kb.sbuf("OaT", [128, 4, S], BF16)
        ObT = kb.sbuf("ObT", [128, 4, S], BF16)

        def stage_A():
            gmix = kb.sbuf("gmix", [128, 1024], F32)
            kb.dma("sp", lambda e: e.dma_start(out=gmix[:], in_=bcast_rows(g_mix_d, 1024)), writes=["gmix"], sem="gmix")
            NA = 4
            xt = [kb.sbuf(f"xt{i}", [128, 1024], F32) for i in range(NA)]
            hb = [kb.sbuf(f"hb{i}", [128, 1024], BF16) for i in range(NA)]
            stA = [kb.sbuf(f"stA{i}", [128, 4], F32) for i in range(NA)]

            def a_tile(tt, i):
                kb.dma("sp", lambda e: e.dma_start(out=xt[i][:], in_=x_d[tt * 128:(tt + 1) * 128, :]),
                       writes=[("xt", i)], sem=f"xt{i}")
                yield
                kb.op("act", lambda e: e.activation(out=junk[:], in_=xt[i][:], func=AF.Square, accum_out=stA[i][:, 0:1]),
                      reads=[("xt", i)], writes=["junk", ("stA", i)])
                yield
                kb.op("act", lambda e: e.activation(out=stA[i][:, 1:2], in_=stA[i][:, 0:1], func=AF.Ln, bias=EPS, scale=1.0 / 1024),
                      reads=[("stA", i)], writes=[("stA", i)])
                yield
                kb.op("act", lambda e: e.activation(out=stA[i][:, 2:3], in_=stA[i][:, 1:2], func=AF.Exp, scale=-0.5),
                      reads=[("stA", i)], writes=[("stA", i)])
                yield
                kb.op("dve", lambda e: e.scalar_tensor_tensor(out=hb[i][:], in0=xt[i][:], scalar=stA[i][:, 2:3], in1=gmix[:],
                                                              op0=ALU.mult, op1=ALU.mult),
                      reads=[("xt", i), ("stA", i), "gmix"], writes=[("hb", i)])
                yield
                b = i
                psb = ps[b][:].bitcast(BF16)
                for kc in range(8):
                    kb.op("pe", lambda e: e.transpose(out=psb[:, kc * 128:(kc + 1) * 128], in_=hb[i][:, kc * 128:(kc + 1) * 128],
                                                      identity=ident[:]),
                          reads=[("hb", i), "c_ident_bf"], writes=[psk(b)])
                yield
                kb.op("dve", lambda e: e.tensor_copy(out=hT[:, :, tt * 128:(tt + 1) * 128], in_=psb.rearrange("p (k t) -> p k t", k=8)),
                      reads=[psk(b)], writes=[("hT", tt)])
                yield

            run_streams([(lambda sl, tt=tt: a_tile(tt, sl)) for tt in range(NT)], NA, bg_every=10 ** 9)

        hT_keys = lambda tq: [("hT", t) for t in range(4 * tq, 4 * tq + 4)]
        pcount = [0]
        PB = [4]

        def proj_fm(wt, wkey, tq, M=128):
            b = pcount[0] % PB[0]
            pcount[0] += 1
            for kc in range(8):
                kb.op("pe", lambda e: e.matmul(ps[b][0:M, 0:512], lhsT=wt[:, kc, 0:M], rhs=hT[:, kc, tq * 512:(tq + 1) * 512],
                                               start=(kc == 0), stop=(kc == 7)),
                      reads=[wkey] + hT_keys(tq), writes=[psk(b)])
            return b

        def load_w(wt, wkey, col0, ncols=128, q="pool"):
            kb.dma(q, lambda e: e.dma_start(out=wt[:, :, 0:ncols],
                                            in_=w_in_d[:, col0:col0 + ncols].rearrange("(kc p) c -> p kc c", p=128)),
                   writes=[wkey], sem=str(wkey))

        ccount = [0]

        def evac(out_ap, in_ap, reads, writes):
            if ccount[0] % 2 == 0:
                kb.op("act", lambda e: e.activation(out=out_ap, in_=in_ap, func=AF.Copy), reads=reads, writes=writes)
            else:
                kb.op("dve", lambda e: e.tensor_copy(out=out_ap, in_=in_ap), reads=reads, writes=writes)
            ccount[0] += 1

        qT = kb.sbuf("qT", [128, S], BF16)
        kT = kb.sbuf("kT", [128, S], BF16)
        V = kb.sbuf("V", [128, NT, 132], BF16)
        wq = [kb.sbuf(f"wq{i}", [128, 8, 128], BF16) for i in range(1)]
        wk = [kb.sbuf(f"wk{i}", [128, 8, 128], BF16) for i in range(1)]
        wv = [kb.sbuf(f"wv{i}", [128, 8, 128], BF16) for i in range(1)]

        def proj_v(wt, wkey):
            for t4 in range(NT // 4):
                b = pcount[0] % PB[0]
                pcount[0] += 1
                for j in range(4):
                    tt = t4 * 4 + j
                    for kc in range(8):
                        kb.op("pe", lambda e: e.matmul(ps[b][:, j * 128:(j + 1) * 128], lhsT=hT[:, kc, tt * 128:(tt + 1) * 128],
                                                       rhs=wt[:, kc, :], start=(kc == 0), stop=(kc == 7)),
                              reads=[wkey, ("hT", tt)], writes=[psk(b)])
                evac(V[:, t4 * 4:(t4 + 1) * 4, 0:128], ps[b][:, 0:512].rearrange("p (j d) -> p j d", j=4),
                     reads=[psk(b)], writes=[("V", t4)])

        bg_ops = []

        def run_streams(makers, ns, bg_every=12):
            pending = list(makers)
            active = {}
            rounds = 0
            while pending or active:
                rounds += 1
                if bg_ops and rounds % bg_every == 0:
                    bg_ops.pop(0)()
                for sl in range(ns):
                    if sl not in active and pending:
                        active[sl] = pending.pop(0)(sl)
                    g = active.get(sl)
                    if g is not None:
                        try:
                            next(g)
                        except StopIteration:
                            del active[sl]

        def stage_B1():
            NS = 4
            PB[0] = 8
            m01 = csb["c_m01_sb"]
            e32 = [kb.sbuf(f"e32_{i}", [128, 512], BF16) for i in range(NS)]
            Pb = [kb.sbuf(f"Pb{i}", [128, 512], BF16) for i in range(NS)]
            Ab = [kb.sbuf(f"Ab{i}", [128, 512], BF16) for i in range(NS)]
            R32 = [kb.sbuf(f"R32_{i}", [128, 512], F32) for i in range(NS)]
            Rb = [kb.sbuf(f"Rb{i}", [128, 512], BF16) for i in range(NS)]
            tri = csb["c_tri_m8"]
            onesm = csb["c_ones_m8"]
            msb = csb["c_mask_sb"]

            def sb_stream(hp, par, qt, sl):
                pb = 64 * par
                bs = sl
                bo = 4 + sl
                kb.op("pe", lambda e: e.matmul(ps[bo][pb:pb + 64, 0:512], lhsT=zeros[:, 0:64], rhs=zeros[:, 0:512],
                                               start=True, stop=False),
                      reads=["c_zeros"], writes=[psk(bo)])
                kb.op("pool", lambda e: e.memset(R32[sl][:], 0.0), writes=[("R32", sl)])
                kb.op("pool", lambda e: e.memset(Rb[sl][:], 0.0), writes=[("Rb", sl)])
                first = True
                for kbk in range(4 * qt + 3, -1, -1):
                    i = kbk - 4 * qt
                    diag = i >= 0
                    c0 = max(0, i) * 128
                    kb.op("pe", lambda e: e.matmul(ps[bs][:, c0:512], lhsT=kT[pb:pb + 64, kbk * 128:(kbk + 1) * 128],
                                                   rhs=qT[pb:pb + 64, qt * 512 + c0:(qt + 1) * 512], start=True, stop=True),
                          reads=[("kT", kbk // 4), ("qT", qt)], writes=[psk(bs)])
                    yield
                    kb.op("act", lambda e: e.activation(out=e32[sl][:, c0:512], in_=ps[bs][:, c0:512], func=AF.Exp, scale=0.125),
                          reads=[psk(bs)], writes=[("e32", sl)])
                    yield
                    kb.op("act", lambda e: e.activation(out=Pb[sl][:, c0:512], in_=e32[sl][:, c0:512], func=AF.Ln, bias=1.0),
                          reads=[("e32", sl)], writes=[("Pb", sl)])
                    if diag:
                        kb.op("dve", lambda e: e.tensor_tensor(out=Pb[sl][:, c0:c0 + 128], in0=Pb[sl][:, c0:c0 + 128], in1=m01[:], op=ALU.mult),
                              reads=[("Pb", sl), "c_m01_sb"], writes=[("Pb", sl)])
                    yield
                    kb.op("pe", lambda e: e.matmul(ps[bs][:, c0:512], lhsT=tri[:], rhs=Pb[sl][:, c0:512], start=False, stop=True,
                                                   skip_group_check=True),
                          reads=[("Pb", sl), "c_tri_m8"], writes=[psk(bs)])
                    if not first:
                        kb.op("pe", lambda e: e.matmul(ps[bs][:, c0:512], lhsT=onesm[:], rhs=Rb[sl][:, c0:512], start=False, stop=True,
                                                       skip_group_check=True),
                              reads=[("Rb", sl), "c_ones_m8"], writes=[psk(bs)])
                    yield
                    kb.op("act", lambda e: e.activation(out=Ab[sl][:, c0:512], in_=ps[bs][:, c0:512], func=AF.Exp, scale=0.125),
                          reads=[psk(bs)], writes=[("Ab", sl)])
                    if diag:
                        kb.op("dve", lambda e: e.tensor_tensor(out=Ab[sl][:, c0:c0 + 128], in0=Ab[sl][:, c0:c0 + 128], in1=m01[:], op=ALU.mult),
                              reads=[("Ab", sl), "c_m01_sb"], writes=[("Ab", sl)])
                    if kbk > 0:
                        kb.op("pool", lambda e: e.tensor_tensor(out=R32[sl][:, c0:512], in0=R32[sl][:, c0:512], in1=Pb[sl][:, c0:512], op=ALU.add),
                              reads=[("R32", sl), ("Pb", sl)], writes=[("R32", sl)])
                        kb.op("dve", lambda e: e.tensor_copy(out=Rb[sl][:, c0:512], in_=R32[sl][:, c0:512]), reads=[("R32", sl)], writes=[("Rb", sl)])
                    yield
                    kb.op("pe", lambda e: e.matmul(ps[bo][pb:pb + 64, c0:512], lhsT=V[:, kbk, pb:pb + 64], rhs=Ab[sl][:, c0:512],
                                                   start=False, stop=(kbk == 0)),
                          reads=[("V", kbk // 4), ("Ab", sl)], writes=[psk(bo)])
                    first = False
                kb.op("dve", lambda e: e.tensor_copy(out=OaT[pb:pb + 64, hp, qt * 512:(qt + 1) * 512], in_=ps[bo][pb:pb + 64, 0:512]),
                      reads=[psk(bo)], writes=[("OaT", 2 * hp + par, qt)])
                yield

            def lw_b1(hp):
                load_w(wq[0], ("wq", 0), hp * 128)
                load_w(wk[0], ("wk", 0), 512 + hp * 128)
                load_w(wv[0], ("wv", 0), 1024 + hp * 128)
            lw_b1(0)
            for hp in range(4):
                w = 0
                for tq in range(NQ):
                    b = proj_fm(wq[w], ("wq", w), tq)
                    evac(qT[:, tq * 512:(tq + 1) * 512], ps[b][:, 0:512], reads=[psk(b)], writes=[("qT", tq)])
                    b = proj_fm(wk[w], ("wk", w), tq)
                    evac(kT[:, tq * 512:(tq + 1) * 512], ps[b][:, 0:512], reads=[psk(b)], writes=[("kT", tq)])
                proj_v(wv[w], ("wv", w))
                if hp + 1 < 4:
                    lw_b1(hp + 1)
                makers = []
                for qt in range(NQ - 1, -1, -1):
                    for par in range(2):
                        makers.append(lambda sl, par=par, qt=qt: sb_stream(hp, par, qt, sl))
                run_streams(makers, NS)
            PB[0] = 4

        def stage_B2():
            qraws = [kb.sbuf(f"qraw{i}", [128, 512], BF16) for i in range(2)]
            PB[0] = 8
            rcnt = [0]
            perm = csb["c_perm"]
            lamt = kb.sbuf("lamt", [128, 4, 64], F32)
            lams = kb.sbuf("lams", [128, 8], F32)
            for n, k in enumerate(("lambda_q1", "lambda_k1", "lambda_q2", "lambda_k2")):
                kb.dma("sp", lambda e: e.dma_start(out=lamt[:, n, :], in_=bcast_rows(lam_d[k], 64)), writes=["lamt"], sem="lamt")
            kb.op("dve", lambda e: e.tensor_tensor(out=lamt[:, 0, :], in0=lamt[:, 0, :], in1=lamt[:, 1, :], op=ALU.mult),
                  reads=["lamt"], writes=["lamt"])
            kb.op("dve", lambda e: e.tensor_tensor(out=lamt[:, 2, :], in0=lamt[:, 2, :], in1=lamt[:, 3, :], op=ALU.mult),
                  reads=["lamt"], writes=["lamt"])
            kb.op("dve", lambda e: e.reduce_sum(out=lams[:, 0:1], in_=lamt[:, 0, :], axis=AX.X), reads=["lamt"], writes=["lams"])
            kb.op("dve", lambda e: e.reduce_sum(out=lams[:, 1:2], in_=lamt[:, 2, :], axis=AX.X), reads=["lamt"], writes=["lams"])
            kb.op("act", lambda e: e.activation(out=lams[:, 2:4], in_=lams[:, 0:2], func=AF.Exp), reads=["lams"], writes=["lams"])
            kb.op("dve", lambda e: e.tensor_tensor(out=lams[:, 4:5], in0=lams[:, 3:4], in1=lams[:, 2:3], op=ALU.subtract),
                  reads=["lams"], writes=["lams"])
            kb.op("dve", lambda e: e.tensor_scalar(out=lams[:, 5:6], in0=lams[:, 4:5], scalar1=-0.2, scalar2=None, op0=ALU.add),
                  reads=["lams"], writes=["lams"])
            neglam = lams[:, 5:6]
            gsub = kb.sbuf("gsub", [128, 128], F32)
            kb.dma("sp", lambda e: e.dma_start(out=gsub[:], in_=bcast_rows(g_sub_d, 128)), writes=["gsub"], sem="gsub")
            kb.op("dve", lambda e: e.tensor_scalar(out=gsub[:], in0=gsub[:], scalar1=0.8, scalar2=None, op0=ALU.mult),
                  reads=["gsub"], writes=["gsub"])
            kb.op("pool", lambda e: e.memset(V[:, :, 128:129], 1.0), writes=["Vones"])
            cs = [kb.sbuf(f"cos{i}", [128, 512], F32) for i in range(2)]
            sn = [kb.sbuf(f"sin{i}", [128, 512], F32) for i in range(2)]
            r1 = [kb.sbuf(f"rt1_{i}", [128, 512], F32) for i in range(2)]
            r2 = [kb.sbuf(f"rt2_{i}", [128, 512], F32) for i in range(2)]
            o32 = [kb.sbuf(f"o32_{i}", [128, 128], F32) for i in range(2)]
            t32 = [kb.sbuf(f"t32_{i}", [128, 128], F32) for i in range(2)]
            onb = [kb.sbuf(f"onb_{i}", [128, 128], BF16) for i in range(2)]
            rr = [kb.sbuf(f"rr_{i}", [128, 8], F32) for i in range(2)]
            mdf = csb["c_mask_df"]
            m01d = csb["c_m01_df"]
            m01db = bass.AP(m01d, 0, [[128, 128], [0, 2], [1, 128]])
            Pd = [kb.sbuf(f"Pd{i}", [128, 2, 512], BF16) for i in range(2)]
            rc = 0
            step = 0
            fc = 0
            def lw_b2(dh):
                w = 0
                cq = 1536 + dh * 128
                ck = 2048 + dh * 128
                cv = 2560 + dh * 128
                load_w(wq[w], ("wq", w), cq)
                load_w(wk[w], ("wk", w), ck)
                load_w(wv[w], ("wv", w), cv)
            accS = [kb.sbuf(f"accS{i}", [128, 3 * 396], F32) for i in range(2)]
            afc = [0]
            fin_pending = []
            fcc = [0]

            def drain(g):
                for _ in g:
                    pass

            def step_bg():
                if fin_pending:
                    try:
                        next(fin_pending[0])
                    except StopIteration:
                        fin_pending.pop(0)

            def finalize(dh, qt, af):
                psb7 = ps[7][:].bitcast(BF16)
                A_ = accS[af]
                for j in range(4):
                    f = fcc[0] % 2
                    fcc[0] += 1
                    a1 = j * 2
                    a2 = j * 2 + 1
                    c1 = (a1 // 3) * 396 + (a1 % 3) * 132
                    c2 = (a2 // 3) * 396 + (a2 % 3) * 132
                    k1 = ("accS", af, a1 // 3)
                    k2 = ("accS", af, a2 // 3)
                    kb.op("dve", lambda e: e.reciprocal(out=rr[f][:, 0:1], in_=A_[:, c1 + 128:c1 + 129]), reads=[k1], writes=[("rr", f)])
                    kb.op("dve", lambda e: e.reciprocal(out=rr[f][:, 1:2], in_=A_[:, c2 + 128:c2 + 129]), reads=[k2], writes=[("rr", f)])
                    kb.op("dve", lambda e: e.tensor_tensor(out=rr[f][:, 2:3], in0=rr[f][:, 1:2], in1=neglam, op=ALU.mult),
                          reads=[("rr", f), "lams"], writes=[("rr", f)])
                    yield
                    kb.op("dve", lambda e: e.tensor_scalar(out=t32[f][:], in0=A_[:, c2:c2 + 128], scalar1=rr[f][:, 2:3], scalar2=None, op0=ALU.mult),
                          reads=[k2, ("rr", f)], writes=[("t32", f)])
                    kb.op("dve", lambda e: e.scalar_tensor_tensor(out=o32[f][:], in0=A_[:, c1:c1 + 128], scalar=rr[f][:, 0:1], in1=t32[f][:],
                                                                  op0=ALU.mult, op1=ALU.add),
                          reads=[k1, ("rr", f), ("t32", f)], writes=[("o32", f)])
                    yield
                    kb.op("dve", lambda e: e.scalar_tensor_tensor(out=t32[f][:], in0=o32[f][:], scalar=1.0, in1=o32[f][:], op0=ALU.mult, op1=ALU.mult,
                                                                  accum_out=rr[f][:, 3:4]),
                          reads=[("o32", f)], writes=[("t32", f), ("rr", f)])
                    kb.op("act", lambda e: e.activation(out=rr[f][:, 4:5], in_=rr[f][:, 3:4], func=AF.Ln, bias=EPS, scale=1.0 / 128),
                          reads=[("rr", f)], writes=[("rr", f)])
                    kb.op("act", lambda e: e.activation(out=rr[f][:, 5:6], in_=rr[f][:, 4:5], func=AF.Exp, scale=-0.5),
                          reads=[("rr", f)], writes=[("rr", f)])
                    yield
                    kb.op("dve", lambda e: e.scalar_tensor_tensor(out=onb[f][:], in0=o32[f][:], scalar=rr[f][:, 5:6], in1=gsub[:],
                                                                  op0=ALU.mult, op1=ALU.mult),
                          reads=[("o32", f), ("rr", f), "gsub"], writes=[("onb", f)])
                    yield
                    kb.op("pe", lambda e: e.transpose(out=psb7[:, j * 128:(j + 1) * 128], in_=onb[f][:], identity=ident[:]),
                          reads=[("onb", f), "c_ident_bf"], writes=[psk(7)])
                    yield
                kb.op("act", lambda e: e.activation(out=ObT[:, dh, qt * 512:(qt + 1) * 512], in_=psb7[:, 0:512], func=AF.Copy),
                      reads=[psk(7)], writes=[("ObT", dh, qt)])

            lw_b2(0)
            for dh in range(4):
                w = 0
                pend_rope = []

                def rope_tail(tq, ci, ri, ba, dstT, dk_):
                    qraw = qraws[ri]
                    bb = pcount[0] % PB[0]
                    pcount[0] += 1
                    kb.op("pe", lambda e: e.matmul(ps[bb][:, 0:512], lhsT=perm[:], rhs=qraw[:], start=True, stop=True),
                          reads=[("qraw", ri), "c_perm"], writes=[psk(bb)])
                    kb.op("dve", lambda e: e.tensor_tensor(out=r1[ri][:], in0=ps[ba][:, 0:512], in1=cs[ci][:], op=ALU.mult),
                          reads=[psk(ba), ("cos", ci)], writes=[("r1", ri)])
                    kb.op("dve", lambda e: e.tensor_tensor(out=r2[ri][:], in0=ps[bb][:, 0:512], in1=sn[ci][:], op=ALU.mult),
                          reads=[psk(bb), ("sin", ci)], writes=[("r2", ri)])
                    kb.op("pool", lambda e: e.tensor_tensor(out=dstT[:, tq * 512:(tq + 1) * 512], in0=r1[ri][:], in1=r2[ri][:], op=ALU.add),
                          reads=[("r1", ri), ("r2", ri)], writes=[(dk_, tq)])

                for tq in range(NQ):
                    ci = tq % 2
                    kb.dma("sp", lambda e: e.dma_start(out=cs[ci][:], in_=cos_d[:, tq * 512:(tq + 1) * 512]), writes=[("cos", ci)], sem=f"cos{ci}")
                    kb.dma("sp", lambda e: e.dma_start(out=sn[ci][:], in_=sin_d[:, tq * 512:(tq + 1) * 512]), writes=[("sin", ci)], sem=f"sin{ci}")
                    for (wa, wak, dstT, dk_) in ((wq[w], ("wq", w), qT, "qT"), (wk[w], ("wk", w), kT, "kT")):
                        ri = rcnt[0] % 2
                        rcnt[0] += 1
                        ba = proj_fm(wa, wak, tq)
                        kb.op("act", lambda e: e.activation(out=qraws[ri][:], in_=ps[ba][:, 0:512], func=AF.Copy), reads=[psk(ba)], writes=[("qraw", ri)])
                        pend_rope.append((tq, ci, ri, ba, dstT, dk_))
                        if len(pend_rope) > 1:
                            rope_tail(*pend_rope.pop(0))
                while pend_rope:
                    rope_tail(*pend_rope.pop(0))
                proj_v(wv[w], ("wv", w))
                if dh + 1 < 4:
                    lw_b2(dh + 1)
                for qt in range(NQ):
                    for bz in (4, 5, 6):
                        kb.op("pe", lambda e: e.matmul(ps[bz][:, 0:512], lhsT=zeros[:, 0:128], rhs=zeros[:, 0:512], start=True, stop=False),
                              reads=["c_zeros"], writes=[psk(bz)])

                    def acc(j, br):
                        a = j * 2 + br
                        return 4 + a // 3, (a % 3) * 132
                    nsteps = 4 * qt + 4

                    def scores(kbk):
                        i = kbk - 4 * qt
                        diag = i >= 0
                        c0 = max(0, i) * 128
                        s = kbk % 2
                        for br in range(2):
                            bs = 2 * s + br
                            pb = 64 * br
                            kb.op("pe", lambda e: e.matmul(ps[bs][:, c0:512], lhsT=kT[pb:pb + 64, kbk * 128:(kbk + 1) * 128],
                                                           rhs=qT[pb:pb + 64, qt * 512 + c0:(qt + 1) * 512], start=True, stop=True),
                                  reads=[("kT", kbk // 4), ("qT", qt)], writes=[psk(bs)])
                        kb.op("act", lambda e: e.activation(out=Pd[s][:, :, c0:512], in_=psall[:, 2 * s:2 * s + 2, c0:512], func=AF.Exp, scale=0.125),
                              reads=[psk(2 * s), psk(2 * s + 1)], writes=[("P", s)])
                        if diag:
                            kb.op("pool", lambda e: e.tensor_tensor(out=Pd[s][:, :, c0:c0 + 128], in0=Pd[s][:, :, c0:c0 + 128], in1=m01db, op=ALU.mult),
                                  reads=[("P", s), "c_m01_df"], writes=[("P", s)])

                    def av(kbk):
                        i = kbk - 4 * qt
                        s = kbk % 2
                        for j in range(max(0, i), 4):
                            for br in range(2):
                                ba, off = acc(j, br)
                                kb.op("pe", lambda e: e.matmul(ps[ba][:, off:off + 129], lhsT=Pd[s][:, br, j * 128:(j + 1) * 128], rhs=V[:, kbk, 0:129],
                                                               start=False, stop=False),
                                      reads=[("P", s), ("V", kbk // 4), "Vones"], writes=[psk(ba)])

                    scores(0)
                    for kbk in range(nsteps):
                        if kbk + 1 < nsteps:
                            scores(kbk + 1)
                        av(kbk)
                        step_bg()
                    for bz in (4, 5, 6):
                        kb.op("pe", lambda e: e.matmul(ps[bz][:, 0:2], lhsT=zeros[:, 0:128], rhs=zeros[:, 0:2], start=False, stop=True),
                              reads=["c_zeros"], writes=[psk(bz)])
                    af = afc[0] % 2
                    afc[0] += 1
                    for bi in range(3):
                        evac(accS[af][:, bi * 396:(bi + 1) * 396], ps[4 + bi][:, 0:396], reads=[psk(4 + bi)], writes=[("accS", af, bi)])
                    while fin_pending:
                        drain(fin_pending[0])
                        fin_pending.pop(0)
                    fin_pending.append(finalize(dh, qt, af))
                while fin_pending:
                    drain(fin_pending[0])
                    fin_pending.pop(0)

        def stage_C():
            wua = [kb.sbuf(f"wua{i}", [128, 4, 128], BF16) for i in range(2)]
            wub = [kb.sbuf(f"wub{i}", [128, 4, 128], BF16) for i in range(2)]
            sga = [kb.sbuf(f"sga{i}", [128, 512], F32) for i in range(2)]
            sgb = [kb.sbuf(f"sgb{i}", [128, 512], F32) for i in range(2)]
            ya = [kb.sbuf(f"ya{i}", [128, 512], F32) for i in range(2)]
            yb = [kb.sbuf(f"yb{i}", [128, 512], F32) for i in range(2)]
            yo = [kb.sbuf(f"yo{i}", [128, 512], BF16) for i in range(2)]
            it = 0
            ub = 0
            wga = [kb.sbuf(f"wga{i}", [128, 8, 128], BF16) for i in range(2)]
            wgb = [kb.sbuf(f"wgb{i}", [128, 8, 128], BF16) for i in range(2)]

            def lw_c(ec):
                w = ec % 2
                load_w(wga[w], ("wga", w), 3072 + ec * 128)
                load_w(wgb[w], ("wgb", w), 4096 + ec * 128)
                kb.dma("pool", lambda e: e.dma_start(out=wua[w][:], in_=w_up_a_d[:, ec * 128:(ec + 1) * 128].rearrange("(c p) n -> p c n", p=128)),
                       writes=[("wua", w)], sem=f"wua{w}")
                kb.dma("pool", lambda e: e.dma_start(out=wub[w][:], in_=w_up_b_d[:, ec * 128:(ec + 1) * 128].rearrange("(c p) n -> p c n", p=128)),
                       writes=[("wub", w)], sem=f"wub{w}")
            lw_c(0)
            for ec in range(8):
                w = ec % 2
                if ec + 1 < 8:
                    lw_c(ec + 1)
                for tq in range(NQ):
                    i = it % 2
                    it += 1
                    bga = proj_fm(wga[w], ("wga", w), tq)
                    bgb = proj_fm(wgb[w], ("wgb", w), tq)
                    bua = 4 + (ub % 4)
                    bub = 4 + ((ub + 1) % 4)
                    ub += 2
                    for (bb_, wt_, wk_, OT, ok_, nh) in ((bua, wua[w], ("wua", w), OaT, "OaT", 8), (bub, wub[w], ("wub", w), ObT, "ObT", 4)):
                        for c in range(4):
                            rk = [(ok_, 2 * c, tq), (ok_, 2 * c + 1, tq)] if nh == 8 else [(ok_, c, tq)]
                            kb.op("pe", lambda e: e.matmul(ps[bb_][:, 0:512], lhsT=wt_[:, c, :], rhs=OT[:, c, tq * 512:(tq + 1) * 512],
                                                           start=(c == 0), stop=(c == 3)),
                                  reads=[wk_] + rk, writes=[psk(bb_)])
                    kb.op("act", lambda e: e.activation(out=sga[i][:], in_=ps[bga][:, 0:512], func=AF.Sigmoid), reads=[psk(bga)], writes=[("sga", i)])
                    kb.op("act", lambda e: e.activation(out=sgb[i][:], in_=ps[bgb][:, 0:512], func=AF.Sigmoid), reads=[psk(bgb)], writes=[("sgb", i)])
                    kb.op("dve", lambda e: e.tensor_tensor(out=ya[i][:], in0=ps[bua][:, 0:512], in1=sga[i][:], op=ALU.mult),
                          reads=[psk(bua), ("sga", i)], writes=[("ya", i)])
                    kb.op("dve", lambda e: e.tensor_tensor(out=yb[i][:], in0=ps[bub][:, 0:512], in1=sgb[i][:], op=ALU.mult),
                          reads=[psk(bub), ("sgb", i)], writes=[("yb", i)])
                    kb.op("pool", lambda e: e.tensor_tensor(out=yo[i][:], in0=ya[i][:], in1=yb[i][:], op=ALU.add),
                          reads=[("ya", i), ("yb", i)], writes=[("yo", i)])
                    kb.dma("sp", lambda e: e.dma_start(out=yT_d[ec, :, tq * 512:(tq + 1) * 512], in_=yo[i][:]),
                           reads=[("yo", i)], writes=[("yT_d", ec, tq)], sem=f"yo{i}")

        def stage_D(h2all, M32, OH, W12):
            wout = kb.sbuf("wout", [128, 8, 1024], BF16)
            for hf in range(2):
                kb.dma("pool", lambda e: e.dma_start(out=wout[:, :, hf * 512:(hf + 1) * 512],
                                                     in_=w_out_d[:, hf * 512:(hf + 1) * 512].rearrange("(kc p) n -> p kc n", p=128)),
                       writes=["wout"], sem="wout")
            g2 = kb.sbuf("g2", [128, 1024], F32)
            kb.dma("sp", lambda e: e.dma_start(out=g2[:], in_=bcast_rows(g_ffn_d, 1024)), writes=["g2"], sem="g2")
            wr = kb.sbuf("wr", [128, 8, 36], F32)
            kb.dma("sp", lambda e: e.dma_start(out=wr[:, :, 0:4], in_=w_rg_d.rearrange("(kc p) c -> p kc c", p=128)), writes=["wr"], sem="wr")
            kb.dma("sp", lambda e: e.dma_start(out=wr[:, :, 4:36], in_=w_re_d.rearrange("(kc p) c -> p kc c", p=128)), writes=["wr"], sem="wr")
            rbias = kb.sbuf("rbias", [128, 36], F32)
            kb.dma("sp", lambda e: e.dma_start(out=rbias[:, 0:4], in_=bcast_rows(b_rg_d, 4)), writes=["rbias"], sem="rbias")
            kb.dma("sp", lambda e: e.dma_start(out=rbias[:, 4:36], in_=bcast_rows(b_re_d, 32)), writes=["rbias"], sem="rbias")
            identf = csb["c_ident_f"]
            yt = [kb.sbuf(f"yt{i}", [128, 8, 512], BF16) for i in range(2)]
            xt = [kb.sbuf(f"xtD{i}", [128, 1024], F32) for i in range(4)]
            x1t = [kb.sbuf(f"x1t{i}", [128, 1024], F32) for i in range(4)]
            h2f = [kb.sbuf(f"h2f{i}", [128, 1024], F32) for i in range(4)]
            h2T = [kb.sbuf(f"h2T{i}", [128, 8, 128], F32) for i in range(4)]
            rt = [kb.sbuf(f"rt{i}", [128, 16], F32) for i in range(4)]
            lg = [kb.sbuf(f"lg{i}", [128, 36], F32) for i in range(4)]
            em = [kb.sbuf(f"em{i}", [128, 32], F32) for i in range(4)]
            em2 = [kb.sbuf(f"em2{i}", [128, 32], F32) for i in range(4)]
            gm = [kb.sbuf(f"gm{i}", [128, 8], F32) for i in range(4)]
            def d_tile(tq, j, sl):
                yi = tq % 2
                tt = 4 * tq + j
                i = sl
                kb.dma("sp", lambda e: e.dma_start(out=xt[i][:], in_=x_d[tt * 128:(tt + 1) * 128, :]), writes=[("xtD", i)], sem=f"xtD{i}")
                for hf in range(2):
                    b = 2 * sl + hf
                    for ec in range(8):
                        kb.op("pe", lambda e: e.matmul(ps[b][:, 0:512], lhsT=yt[yi][:, ec, j * 128:(j + 1) * 128],
                                                       rhs=wout[:, ec, hf * 512:(hf + 1) * 512], start=(ec == 0), stop=(ec == 7)),
                              reads=[("yt", yi), "wout"], writes=[psk(b)])
                    kb.op("dve", lambda e: e.tensor_tensor(out=x1t[i][:, hf * 512:(hf + 1) * 512], in0=ps[b][:, 0:512],
                                                           in1=xt[i][:, hf * 512:(hf + 1) * 512], op=ALU.add),
                          reads=[psk(b), ("xtD", i)], writes=[("x1t", i, hf)])
                kb.dma("pool", lambda e: e.dma_start(out=x1_d[tt * 128:(tt + 1) * 128, :], in_=x1t[i][:]),
                       reads=[("x1t", i, 0), ("x1t", i, 1)], writes=[("x1_d", tt)], sem=f"x1t{i}")
                yield
                R = ("rt", i)
                kb.op("act", lambda e: e.activation(out=junk[:], in_=x1t[i][:], func=AF.Square, accum_out=rt[i][:, 0:1]),
                      reads=[("x1t", i, 0), ("x1t", i, 1)], writes=["junk", R])
                kb.op("act", lambda e: e.activation(out=rt[i][:, 1:2], in_=rt[i][:, 0:1], func=AF.Ln, bias=EPS, scale=1.0 / 1024), reads=[R], writes=[R])
                kb.op("act", lambda e: e.activation(out=rt[i][:, 2:3], in_=rt[i][:, 1:2], func=AF.Exp, scale=-0.5), reads=[R], writes=[R])
                kb.op("dve", lambda e: e.scalar_tensor_tensor(out=h2f[i][:], in0=x1t[i][:], scalar=rt[i][:, 2:3], in1=g2[:], op0=ALU.mult, op1=ALU.mult),
                      reads=[("x1t", i, 0), ("x1t", i, 1), R, "g2"], writes=[("h2f", i)])
                kb.op("pool", lambda e: e.tensor_copy(out=h2all[:, tt, :], in_=h2f[i][:]), reads=[("h2f", i)], writes=[("h2all", tt)])
                yield
                for hf in range(2):
                    b = 2 * sl + hf
                    for k4 in range(4):
                        kc = hf * 4 + k4
                        kb.op("pe", lambda e: e.transpose(out=ps[b][:, k4 * 128:(k4 + 1) * 128], in_=h2f[i][:, kc * 128:(kc + 1) * 128], identity=identf[:]),
                              reads=[("h2f", i), "c_ident_f"], writes=[psk(b)])
                    evac(h2T[i][:, hf * 4:(hf + 1) * 4, :], ps[b][:, 0:512].rearrange("p (k t) -> p k t", k=4), reads=[psk(b)], writes=[("h2T", i, hf)])
                yield
                b = 2 * sl
                for kc in range(8):
                    kb.op("pe", lambda e: e.matmul(ps[b][:, 0:36], lhsT=h2T[i][:, kc, :], rhs=wr[:, kc, :], start=(kc == 0), stop=(kc == 7)),
                          reads=[("h2T", i, kc // 4), "wr"], writes=[psk(b)])
                L = ("lg", i)
                kb.op("dve", lambda e: e.tensor_tensor(out=lg[i][:], in0=ps[b][:, 0:36], in1=rbias[:], op=ALU.add), reads=[psk(b), "rbias"], writes=[L])
                yield
                kb.op("dve", lambda e: e.reduce_max(out=rt[i][:, 3:4], in_=lg[i][:, 0:4], axis=AX.X), reads=[L], writes=[R])
                kb.op("dve", lambda e: e.tensor_scalar(out=gm[i][:, 0:4], in0=lg[i][:, 0:4], scalar1=rt[i][:, 3:4], scalar2=None, op0=ALU.is_equal),
                      reads=[L, R], writes=[("gm", i)])
                kb.op("dve", lambda e: e.tensor_scalar(out=rt[i][:, 4:5], in0=rt[i][:, 3:4], scalar1=-1.0, scalar2=None, op0=ALU.mult), reads=[R], writes=[R])
                kb.op("act", lambda e: e.activation(out=gm[i][:, 4:8], in_=lg[i][:, 0:4], func=AF.Exp, bias=rt[i][:, 4:5], accum_out=rt[i][:, 5:6]),
                      reads=[L, R], writes=[("gm2", i), R])
                kb.op("dve", lambda e: e.reciprocal(out=rt[i][:, 6:7], in_=rt[i][:, 5:6]), reads=[R], writes=[R])
                yield
                kb.op("dve", lambda e: e.tensor_scalar(out=gm[i][:, 0:4], in0=gm[i][:, 0:4], scalar1=1e30, scalar2=-1e30, op0=ALU.mult, op1=ALU.add),
                      reads=[("gm", i)], writes=[("gm", i)])
                pen = bass.AP(gm[i], 0, [[8, 128], [1, 4], [0, 8]])
                kb.op("dve", lambda e: e.tensor_tensor(out=em[i][:].rearrange("p (g e) -> p g e", g=4), in0=lg[i][:, 4:36].rearrange("p (g e) -> p g e", g=4),
                                                       in1=pen, op=ALU.add), reads=[L, ("gm", i)], writes=[("em", i)])
                kb.op("dve", lambda e: e.reduce_max(out=rt[i][:, 7:8], in_=em[i][:], axis=AX.X), reads=[("em", i)], writes=[R])
                kb.op("dve", lambda e: e.tensor_scalar(out=OH[:, tt, 0, :], in0=em[i][:], scalar1=rt[i][:, 7:8], scalar2=None, op0=ALU.is_equal),
                      reads=[("em", i), R], writes=[("OH", tt)])
                yield
                kb.op("dve", lambda e: e.scalar_tensor_tensor(out=em2[i][:], in0=OH[:, tt, 0, :], scalar=-1e30, in1=em[i][:], op0=ALU.mult, op1=ALU.add),
                      reads=[("OH", tt), ("em", i)], writes=[("em2", i)])
                kb.op("dve", lambda e: e.reduce_max(out=rt[i][:, 8:9], in_=em2[i][:], axis=AX.X), reads=[("em2", i)], writes=[R])
                kb.op("dve", lambda e: e.tensor_scalar(out=OH[:, tt, 1, :], in0=em2[i][:], scalar1=rt[i][:, 8:9], scalar2=None, op0=ALU.is_equal),
                      reads=[("em2", i), R], writes=[("OH", tt)])
                kb.op("dve", lambda e: e.tensor_tensor(out=rt[i][:, 9:10], in0=rt[i][:, 8:9], in1=rt[i][:, 7:8], op=ALU.subtract), reads=[R], writes=[R])
                kb.op("act", lambda e: e.activation(out=rt[i][:, 10:11], in_=rt[i][:, 9:10], func=AF.Exp), reads=[R], writes=[R])
                yield
                kb.op("dve", lambda e: e.tensor_scalar(out=rt[i][:, 11:12], in0=rt[i][:, 10:11], scalar1=1.0, scalar2=None, op0=ALU.add), reads=[R], writes=[R])
                kb.op("dve", lambda e: e.reciprocal(out=rt[i][:, 12:13], in_=rt[i][:, 11:12]), reads=[R], writes=[R])
                kb.op("dve", lambda e: e.tensor_tensor(out=W12[:, tt, 0:1], in0=rt[i][:, 6:7], in1=rt[i][:, 12:13], op=ALU.mult), reads=[R], writes=[("W12", tt)])
                kb.op("dve", lambda e: e.tensor_tensor(out=W12[:, tt, 1:2], in0=rt[i][:, 6:7], in1=W12[:, tt, 0:1], op=ALU.subtract),
                      reads=[R, ("W12", tt)], writes=[("W12", tt)])
                kb.op("dve", lambda e: e.tensor_tensor(out=M32[:, tt, :], in0=OH[:, tt, 0, :], in1=OH[:, tt, 1, :], op=ALU.add),
                      reads=[("OH", tt)], writes=[("M32", tt)])
                yield

            def d_tile_start(tq, j, sl):
                if j == 0:
                    yi = tq % 2
                    kb.dma("sp", lambda e: e.dma_start(out=yt[yi][:], in_=yT_d[:, :, tq * 512:(tq + 1) * 512].rearrange("c p t -> p c t")),
                           reads=[("yT_d", ec, tq) for ec in range(8)], writes=[("yt", yi)], sem=f"yt{yi}")
                yield from d_tile(tq, j, sl)

            makers = []
            for tq in range(NQ):
                for j in range(4):
                    makers.append(lambda sl, tq=tq, j=j: d_tile_start(tq, j, sl))
            run_streams(makers, 4, bg_every=10 ** 9)

        def stage_E(h2all, M32, OH, W12, desti, widx):
            trif = csb["c_tri_f"]
            onesf = csb["c_ones_f"]
            Mcum = kb.sbuf("Mcum", [128, NT + 1, 32], F32)
            kb.op("dve", lambda e: e.memset(Mcum[:, 0, :], 0.0), writes=[("Mcum", 0)])
            for tt in range(NT):
                kb.op("dve", lambda e: e.tensor_tensor(out=Mcum[:, tt + 1, :], in0=Mcum[:, tt, :], in1=M32[:, tt, :], op=ALU.add),
                      reads=[("Mcum", tt), ("M32", tt)], writes=[("Mcum", tt + 1)])
            cnt = kb.sbuf("cnt", [128, 32], F32)
            cnti = kb.sbuf("cnti", [128, 32], I32)
            cnti2 = kb.sbuf("cnti2", [128, 32], I32)
            padded = kb.sbuf("padded", [128, 32], F32)
            pend = kb.sbuf("pend", [128, 32], F32)
            pstart = kb.sbuf("pstart", [128, 32], F32)
            ones32 = kb.sbuf("ones32", [128, 32], F32)
            kb.op("pe", lambda e: e.matmul(ps[0][:, 0:32], lhsT=onesf[:], rhs=Mcum[:, NT, :], start=True, stop=True),
                  reads=[("Mcum", NT), "c_ones_f"], writes=[psk(0)])
            kb.op("dve", lambda e: e.tensor_copy(out=cnti[:], in_=ps[0][:, 0:32]), reads=[psk(0)], writes=["cnti"])
            kb.op("dve", lambda e: e.tensor_scalar(out=cnti2[:], in0=cnti[:], scalar1=127, scalar2=None, op0=ALU.add), reads=["cnti"], writes=["cnti2"])
            kb.op("dve", lambda e: e.tensor_scalar(out=cnti[:], in0=cnti2[:], scalar1=7, scalar2=7, op0=ALU.arith_shift_right, op1=ALU.logical_shift_left),
                  reads=["cnti2"], writes=["cnti"])
            kb.op("dve", lambda e: e.tensor_copy(out=padded[:], in_=cnti[:]), reads=["cnti"], writes=["padded"])
            kb.op("dve", lambda e: e.memset(ones32[:], 1.0), writes=["ones32"])
            kb.op("dve", lambda e: e.tensor_tensor_scan(out=pend[:], data0=ones32[:], data1=padded[:], initial=0.0, op0=ALU.mult, op1=ALU.add),
                  reads=["ones32", "padded"], writes=["pend"])
            kb.op("dve", lambda e: e.tensor_tensor(out=pstart[:], in0=pend[:], in1=padded[:], op=ALU.subtract), reads=["pend", "padded"], writes=["pstart"])
            destf = kb.sbuf("destf", [128, 2 * NT], F32)
            base = [kb.sbuf(f"base{i}", [128, 32], F32) for i in range(2)]
            prod = [kb.sbuf(f"prod{i}", [128, 32], F32) for i in range(2)]
            for tt in range(NT):
                i = tt % 2
                b = 1 + i
                kb.op("pe", lambda e: e.matmul(ps[b][:, 0:32], lhsT=onesf[:], rhs=Mcum[:, tt, :], start=True, stop=False),
                      reads=[("Mcum", tt), "c_ones_f"], writes=[psk(b)])
                kb.op("pe", lambda e: e.matmul(ps[b][:, 0:32], lhsT=trif[:], rhs=M32[:, tt, :], start=False, stop=True),
                      reads=[("M32", tt), "c_tri_f"], writes=[psk(b)])
                kb.op("dve", lambda e: e.tensor_tensor(out=base[i][:], in0=ps[b][:, 0:32], in1=pstart[:], op=ALU.add),
                      reads=[psk(b), "pstart"], writes=[("base", i)])
                for k in range(2):
                    kb.op("dve", lambda e: e.tensor_tensor(out=prod[k][:], in0=OH[:, tt, k, :], in1=base[i][:], op=ALU.mult),
                          reads=[("OH", tt), ("base", i)], writes=[("prod", k)])
                    kb.op("dve", lambda e: e.reduce_sum(out=destf[:, 2 * tt + k:2 * tt + k + 1], in_=prod[k][:], axis=AX.X),
                          reads=[("prod", k)], writes=["destf"])
            kb.op("dve", lambda e: e.tensor_copy(out=desti[:], in_=destf[:]), reads=["destf"], writes=["desti"])
            thr = kb.sbuf("thr", [128, NB], F32)
            cmp_ = kb.sbuf("cmp", [128, NB, 32], F32)
            be = kb.sbuf("be", [128, NB], F32)
            pidx = kb.sbuf("pidx", [128, 1], F32)
            kb.op("pool", lambda e: e.iota(thr[:], pattern=[[128, NB]], base=0, channel_multiplier=0, allow_small_or_imprecise_dtypes=True), writes=["thr"])
            kb.op("pool", lambda e: e.iota(pidx[:], pattern=[[1, 1]], base=0, channel_multiplier=1, allow_small_or_imprecise_dtypes=True), writes=["pidx"])
            pend_b = bass.AP(pend, 0, [[32, 128], [0, NB], [1, 32]])
            thr_b = bass.AP(thr, 0, [[NB, 128], [1, NB], [0, 32]])
            kb.op("dve", lambda e: e.tensor_tensor(out=cmp_[:], in0=pend_b, in1=thr_b, op=ALU.is_le), reads=["pend", "thr"], writes=["cmp"])
            kb.op("dve", lambda e: e.reduce_sum(out=be[:], in_=cmp_[:], axis=AX.X), reads=["cmp"], writes=["be"])
            kb.op("dve", lambda e: e.tensor_scalar(out=be[:], in0=be[:], scalar1=31.0, scalar2=128.0, op0=ALU.min, op1=ALU.mult), reads=["be"], writes=["be"])
            kb.op("dve", lambda e: e.tensor_scalar(out=be[:], in0=be[:], scalar1=pidx[:, 0:1], scalar2=None, op0=ALU.add), reads=["be", "pidx"], writes=["be"])
            kb.op("dve", lambda e: e.tensor_copy(out=widx[:], in_=be[:]), reads=["be"], writes=["widx"])
            for tt in range(NT):
                for k in range(2):
                    kb.dma("pool", lambda e: e.indirect_dma_start(out=xd_d, out_offset=bass.IndirectOffsetOnAxis(ap=desti[:, 2 * tt + k:2 * tt + k + 1], axis=0),
                                                                  in_=h2all[:, tt, :], in_offset=None),
                           reads=[("h2all", tt), "desti"], writes=[("xd_d", (2 * tt + k) % 4)], sem=f"scat{(2 * tt + k) % 4}")

        def stage_F(widx):
            NBUF = 3
            wcomb = [kb.sbuf(f"wcomb{i}", [128, 6144], BF16) for i in range(NBUF)]
            wg = [t[:, 0:2048] for t in wcomb]
            wu = [t[:, 2048:4096] for t in wcomb]
            wd = [t[:, 4096:6144] for t in wcomb]
            xdt = [kb.sbuf(f"xdt{i}", [128, 1024], BF16) for i in range(NBUF)]
            xdT = [kb.sbuf(f"xdT{i}", [128, 8, 128], BF16) for i in range(2)]
            sg = [kb.sbuf(f"sg{i}", [128, 256], F32) for i in range(2)]
            actT = [kb.sbuf(f"actT{i}", [128, 256], BF16) for i in range(2)]
            ydt = [kb.sbuf(f"ydt{i}", [128, 1024], F32) for i in range(2)]

            def prefetch(b_):
                i = b_ % NBUF
                kb.dma("pool", lambda e: e.indirect_dma_start(out=wcomb[i][:], out_offset=None, in_=wall_d,
                                                              in_offset=bass.IndirectOffsetOnAxis(ap=widx[:, b_:b_ + 1], axis=0)),
                       reads=["widx"] + WALL_KEYS, writes=[("wcomb", i)], sem=f"wcomb{i}")
                kb.dma("sp", lambda e: e.dma_start(out=xdt[i][:], in_=xd_d[b_ * 128:(b_ + 1) * 128, :]), reads=XD_KEYS, writes=[("xdt", i)], sem=f"xdt{i}")

            def compute(b_):
                i = b_ % NBUF
                j = b_ % 2
                bt = j
                psb = ps[bt][:].bitcast(BF16)
                for kc in range(8):
                    kb.op("pe", lambda e: e.transpose(out=psb[:, kc * 128:(kc + 1) * 128], in_=xdt[i][:, kc * 128:(kc + 1) * 128], identity=ident[:]),
                          reads=[("xdt", i), "c_ident_bf"], writes=[psk(bt)])
                evac(xdT[j][:], psb.rearrange("p (k t) -> p k t", k=8), reads=[psk(bt)], writes=[("xdT", j)])
                bg = 2 + j
                for slot, (wt, nm) in enumerate(((wg[i], "wg"), (wg[i], "wg"), (wu[i], "wu"), (wu[i], "wu"))):
                    n2 = slot % 2
                    for kc in range(8):
                        kb.op("pe", lambda e: e.matmul(ps[bg][:, slot * 128:(slot + 1) * 128], lhsT=wt[:, kc * 256 + n2 * 128:kc * 256 + (n2 + 1) * 128],
                                                       rhs=xdT[j][:, kc, :], start=(kc == 0), stop=(kc == 7)),
                              reads=[("wcomb", i), ("xdT", j)], writes=[psk(bg)])
                kb.op("act", lambda e: e.activation(out=sg[j][:], in_=ps[bg][:, 0:256], func=AF.Silu), reads=[psk(bg)], writes=[("sg", j)])
                kb.op("dve", lambda e: e.tensor_tensor(out=actT[j][:], in0=ps[bg][:, 256:512], in1=sg[j][:], op=ALU.mult),
                      reads=[psk(bg), ("sg", j)], writes=[("actT", j)])
                for hf in range(2):
                    bd = 4 + (2 * b_ + hf) % 4
                    for n2 in range(2):
                        kb.op("pe", lambda e: e.matmul(ps[bd][:, 0:512], lhsT=actT[j][:, n2 * 128:(n2 + 1) * 128],
                                                       rhs=wd[i][:, n2 * 1024 + hf * 512:n2 * 1024 + (hf + 1) * 512], start=(n2 == 0), stop=(n2 == 1)),
                              reads=[("actT", j), ("wcomb", i)], writes=[psk(bd)])
                    evac(ydt[j][:, hf * 512:(hf + 1) * 512], ps[bd][:, 0:512], reads=[psk(bd)], writes=[("ydt", j, hf)])
                kb.dma("act", lambda e: e.dma_start(out=yd_d[b_ * 128:(b_ + 1) * 128, :], in_=ydt[j][:]),
                       reads=[("ydt", j, 0), ("ydt", j, 1)], writes=[("yd_d", b_ % 2)], sem=f"ydt{j}")

            prefetch(0)
            if NB > 1:
                prefetch(1)
            for b_ in range(NB):
                if b_ + 2 < NB:
                    prefetch(b_ + 2)
                compute(b_)

        def stage_G(desti, W12):
            NBUF = 3
            gf = kb.sbuf("gf", [128, 1024], F32)
            kb.dma("sp", lambda e: e.dma_start(out=gf[:], in_=bcast_rows(g_fin_d, 1024)), writes=["gf"], sem="gf")
            g0 = [kb.sbuf(f"g0_{i}", [128, 1024], F32) for i in range(NBUF)]
            g1 = [kb.sbuf(f"g1_{i}", [128, 1024], F32) for i in range(NBUF)]
            xx = [kb.sbuf(f"xx_{i}", [128, 1024], F32) for i in range(NBUF)]
            oo = [kb.sbuf(f"oo_{i}", [128, 1024], F32) for i in range(2)]
            rg = [kb.sbuf(f"rg_{i}", [128, 4], F32) for i in range(2)]

            def prefetch(tt):
                i = tt % NBUF
                for k, gt in ((0, g0[i]), (1, g1[i])):
                    kb.dma("pool", lambda e: e.indirect_dma_start(out=gt[:], out_offset=None, in_=yd_d,
                                                                  in_offset=bass.IndirectOffsetOnAxis(ap=desti[:, 2 * tt + k:2 * tt + k + 1], axis=0)),
                           reads=YD_KEYS + ["desti"], writes=[("gg", k, i)], sem=f"gg{k}{i}")
                kb.dma("sp", lambda e: e.dma_start(out=xx[i][:], in_=x1_d[tt * 128:(tt + 1) * 128, :]), reads=[("x1_d", tt)], writes=[("xx", i)], sem=f"xx{i}")

            def compute(tt):
                i = tt % NBUF
                j = tt % 2
                kb.op("dve", lambda e: e.scalar_tensor_tensor(out=xx[i][:], in0=g0[i][:], scalar=W12[:, tt, 0:1], in1=xx[i][:], op0=ALU.mult, op1=ALU.add),
                      reads=[("gg", 0, i), ("xx", i), ("W12", tt)], writes=[("xx", i)])
                kb.op("dve", lambda e: e.scalar_tensor_tensor(out=xx[i][:], in0=g1[i][:], scalar=W12[:, tt, 1:2], in1=xx[i][:], op0=ALU.mult, op1=ALU.add),
                      reads=[("gg", 1, i), ("xx", i), ("W12", tt)], writes=[("xx", i)])
                kb.op("act", lambda e: e.activation(out=junk[:], in_=xx[i][:], func=AF.Square, accum_out=rg[j][:, 0:1]), reads=[("xx", i)], writes=["junk", ("rg", j)])
                kb.op("act", lambda e: e.activation(out=rg[j][:, 1:2], in_=rg[j][:, 0:1], func=AF.Ln, bias=EPS, scale=1.0 / 1024), reads=[("rg", j)], writes=[("rg", j)])
                kb.op("act", lambda e: e.activation(out=rg[j][:, 2:3], in_=rg[j][:, 1:2], func=AF.Exp, scale=-0.5), reads=[("rg", j)], writes=[("rg", j)])
                kb.op("dve", lambda e: e.scalar_tensor_tensor(out=oo[j][:], in0=xx[i][:], scalar=rg[j][:, 2:3], in1=gf[:], op0=ALU.mult, op1=ALU.mult),
                      reads=[("xx", i), ("rg", j), "gf"], writes=[("oo", j)])
                kb.dma("act", lambda e: e.dma_start(out=out_d[tt * 128:(tt + 1) * 128, :], in_=oo[j][:]), reads=[("oo", j)], writes=[("out_d", tt)], sem=f"oo{j}")

            prefetch(0)
            if NT > 1:
                prefetch(1)
            for tt in range(NT):
                if tt + 2 < NT:
                    prefetch(tt + 2)
                compute(tt)

        with kb.scope():
            stage_A()
        zfill = kb.sbuf("zfill", [128, 2048], BF16)
        kb.op("pool", lambda e: e.memset(zfill[:], 0.0), writes=["zfill"])
        for c in range(NROWS // 256):
            bg_ops.append(lambda c=c: kb.dma(
                "sp", lambda e: e.dma_start(out=xd_d[c * 256:(c + 1) * 256, :].rearrange("(p j) d -> p (j d)", j=2), in_=zfill[:]),
                reads=["zfill"], writes=[("xd_d", c % 4)], sem="zf"))
        for k_, (nm, src) in enumerate((("wg", wg_d), ("wu", wu_d), ("wd", wd_d))):
            for c in range(16):
                bg_ops.append(lambda k_=k_, src=src, c=c: kb.dma(
                    "pool", lambda e: e.dma_start(out=wall_d[c * 256:(c + 1) * 256, k_ * 2048:(k_ + 1) * 2048], in_=src[c * 256:(c + 1) * 256, :]),
                    writes=[("wall", (k_ * 16 + c) % 4)], sem=f"pc{(k_ * 16 + c) % 4}"))
        with kb.scope():
            stage_B1()
        with kb.scope():
            stage_B2()
        if dbg:
            for nm, T, keys, nch in (("dbg_oa", OaT, [("OaT", h, q) for h in range(8) for q in range(NQ)], 4),
                                     ("dbg_ob", ObT, [("ObT", h, q) for h in range(4) for q in range(NQ)], 4)):
                if nm[4:] in dbg:
                    o = dout(nm, [128, nch, S])
                    tmp = kb.sbuf(nm + "_t", [128, nch, S], F32)
                    kb.op("dve", lambda e: e.tensor_copy(out=tmp[:], in_=T[:]), reads=keys, writes=[nm])
                    kb.dma("sp", lambda e: e.dma_start(out=o, in_=tmp[:]), reads=[nm], writes=[nm + "_d"], sem=nm)
        while bg_ops:
            bg_ops.pop(0)()
        PB[0] = 4
        with kb.scope():
            stage_C()
        st_main.close()
        with contextlib.ExitStack() as st2:
            kb.stack = st2
            h2all = kb.sbuf("h2all", [128, NT, 1024], BF16)
            M32 = kb.sbuf("M32", [128, NT, 32], F32)
            OH = kb.sbuf("OH", [128, NT, 2, 32], F32)
            W12 = kb.sbuf("W12", [128, NT, 2], F32)
            desti = kb.sbuf("desti", [128, 2 * NT], I32)
            widx = kb.sbuf("widx", [128, NB], I32)
            with kb.scope():
                stage_D(h2all, M32, OH, W12)
            with kb.scope():
                stage_E(h2all, M32, OH, W12, desti, widx)
            with kb.scope():
                stage_F(widx)
            with kb.scope():
                stage_G(desti, W12)
        print("build: instr", kb.cnt, "waits", kb.n_wait, "sems", len(kb.sems))
    return nc


def _core_inputs(inputs, b, S, shared):
    d = dict(shared)
    d["x"] = np.ascontiguousarray(inputs["x"][b, :S], dtype=np.float32)
    return d


def _shared_inputs(inputs, S):
    f = lambda a: np.ascontiguousarray(np.asarray(a, dtype=np.float32))
    sh = {
        "w_in": f(inputs["w_in"][0]),
        "g_norm_mix": f(inputs["g_norm_mix"]).reshape(1, 1024),
        "g_subln": f(inputs["g_subln"]).reshape(1, 128),
        "w_up_a": f(inputs["w_up_a"][0]), "w_up_b": f(inputs["w_up_b"][0]), "w_out": f(inputs["w_out"][0]),
        "g_norm_ffn": f(inputs["g_norm_ffn"]).reshape(1, 1024),
        "w_router_group": f(inputs["w_router_group"][0]), "w_router_expert": f(inputs["w_router_expert"][0]),
        "b_router_group": f(inputs["b_router_group"]).reshape(1, 4), "b_router_expert": f(inputs["b_router_expert"]).reshape(1, 32),
        "g_norm_final": f(inputs["g_norm_final"]).reshape(1, 1024),
    }
    for k in ("lambda_q1", "lambda_k1", "lambda_q2", "lambda_k2"):
        sh[k] = f(inputs[k]).reshape(1, 64)
    wg = np.asarray(inputs["w_expert_gate"][0], dtype=np.float32)
    wu = np.asarray(inputs["w_expert_up"][0], dtype=np.float32)
    wd = np.asarray(inputs["w_expert_down"][0], dtype=np.float32)
    sh["wg_l"] = np.ascontiguousarray(wg.reshape(32, 8, 128, 256).transpose(0, 2, 1, 3)).reshape(4096, 2048)
    sh["wu_l"] = np.ascontiguousarray(wu.reshape(32, 8, 128, 256).transpose(0, 2, 1, 3)).reshape(4096, 2048)
    sh["wd_l"] = np.ascontiguousarray(wd.reshape(32, 2, 128, 1024).transpose(0, 2, 1, 3)).reshape(4096, 2048)
    sh.update(host_consts(S))
    return sh


_NC_CACHE = {}


def kernel(**inputs):
    S = SEQ
    B = inputs["x"].shape[0]
    if S not in _NC_CACHE:
        _NC_CACHE[S] = build(S)
    nc = _NC_CACHE[S]
    shared = _shared_inputs(inputs, S)
    in_maps = [_core_inputs(inputs, b, S, shared) for b in range(B)]
    res = run_bass_kernel_spmd(nc, in_maps, core_ids=list(range(B)))
    return np.stack([np.asarray(r["out"], dtype=np.float32) for r in res.results], axis=0)
```
